# Optimizing a Trainium2 kernel written in Bass

```python
import math
import jax, jax.numpy as jnp
from jax import lax
import numpy as np

D_MODEL = 1024
BATCH = 16
SEQ = 2048
DEPTH = 4

CHUNK = 64
HEAD_DIM = 64
POOL_WINDOWS = (2, 4, 8, 16)
POOL_WIDTH = D_MODEL // 4
POOL_GROUP = POOL_WIDTH // len(POOL_WINDOWS)
DSA_WIDTH = 3 * D_MODEL // 8
DSA_HEADS = DSA_WIDTH // HEAD_DIM
FOX_WIDTH = 3 * D_MODEL // 8
FOX_HEADS = FOX_WIDTH // HEAD_DIM
MIX_WIDTH = POOL_WIDTH + DSA_WIDTH + FOX_WIDTH
IDX_HEADS = 8
IDX_DIM = 32
DSA_TOPK_MAX = 256
DSA_QBLOCK = 32
FOX_QBLOCK = 128
RMS_EPS = 1e-6
IN_SIZES = (
    POOL_WIDTH, POOL_WIDTH,
    DSA_WIDTH, DSA_WIDTH, DSA_WIDTH, DSA_WIDTH,
    IDX_HEADS * IDX_DIM, IDX_DIM, IDX_HEADS,
    FOX_WIDTH, FOX_WIDTH, FOX_WIDTH, FOX_WIDTH,
    FOX_HEADS,
)
IN_COLS = 2 * POOL_WIDTH + 4 * DSA_WIDTH + IDX_HEADS * IDX_DIM + IDX_DIM + IDX_HEADS + 4 * FOX_WIDTH + FOX_HEADS

kernel_name = 'hybrid_pool_dsa_fox_chunk_causal'


def rmsnorm(x, g):
    xf = x.astype(jnp.float32)
    y = xf * lax.rsqrt(jnp.mean(xf * xf, axis=-1, keepdims=True) + RMS_EPS)
    return (y * g.astype(jnp.float32)).astype(x.dtype)


def split_columns(z):
    parts = []
    off = 0
    for n in IN_SIZES:
        parts.append(z[..., off:off + n])
        off += n
    return parts


def pool_mixer(v, w_grp, scale):
    B, L, _ = v.shape
    vf = v.astype(jnp.float32)
    cs = jnp.pad(jnp.cumsum(vf, axis=1), ((0, 0), (1, 0), (0, 0)))
    t = jnp.arange(L)
    outs = []
    for gi, w in enumerate(POOL_WINDOWS):
        sl = slice(gi * POOL_GROUP, (gi + 1) * POOL_GROUP)
        c = cs[:, :, sl]
        lo = jnp.maximum(t + 1 - w, 0)
        win_sum = c[:, t + 1] - c[:, lo]
        cnt = (t + 1 - lo).astype(jnp.float32)[None, :, None]
        outs.append(win_sum / cnt - vf[:, :, sl])
    p = jnp.stack(outs, axis=2).astype(v.dtype)
    y = jnp.einsum('blgc,gcd->blgd', p, w_grp).reshape(B, L, POOL_WIDTH)
    return y * scale


def dsa_mixer(q, k, v, iq, ik, iw, topk):
    B, L, H, dh = q.shape
    n_blocks = L // DSA_QBLOCK
    key_pos = jnp.arange(L)
    sm_scale = 1.0 / math.sqrt(dh)
    gather = jax.vmap(lambda arr, idx: arr[idx])

    def block(bi):
        start = bi * DSA_QBLOCK
        qb = lax.dynamic_slice_in_dim(q, start, DSA_QBLOCK, axis=1)
        iqb = lax.dynamic_slice_in_dim(iq, start, DSA_QBLOCK, axis=1)
        iwb = lax.dynamic_slice_in_dim(iw, start, DSA_QBLOCK, axis=1)
        qpos = start + jnp.arange(DSA_QBLOCK)
        limit = (qpos // CHUNK + 1) * CHUNK
        dots = jnp.einsum('bqhd,bsd->bqhs', iqb, ik).astype(jnp.float32)
        score = jnp.einsum('bqh,bqhs->bqs', iwb.astype(jnp.float32), jax.nn.relu(dots))
        admissible = key_pos[None, None, :] < limit[None, :, None]
        score = jnp.where(admissible, score, -jnp.inf)
        _, sel = lax.top_k(score, topk)
        valid = sel < limit[None, :, None]
        kg = gather(k, sel)
        vg = gather(v, sel)
        s = jnp.einsum('bqhd,bqkhd->bqhk', qb, kg).astype(jnp.float32) * sm_scale
        s = jnp.where(valid[:, :, None, :], s, -jnp.inf)
        p = jax.nn.softmax(s, axis=-1).astype(v.dtype)
        return jnp.einsum('bqhk,bqkhd->bqhd', p, vg)

    o = lax.map(block, jnp.arange(n_blocks))
    return jnp.transpose(o, (1, 0, 2, 3, 4)).reshape(B, L, H, dh)


def fox_mixer(q, k, v, log_f):
    B, L, H, dh = q.shape
    sm_scale = 1.0 / math.sqrt(dh)
    F = jnp.transpose(jnp.cumsum(log_f, axis=1), (0, 2, 1))
    outs = []
    for bi in range(L // FOX_QBLOCK):
        s0 = bi * FOX_QBLOCK
        e = s0 + FOX_QBLOCK
        s = jnp.einsum('bqhd,bkhd->bhqk', q[:, s0:e], k[:, :e]).astype(jnp.float32) * sm_scale
        s = s + F[:, :, s0:e, None] - F[:, :, None, :e]
        mask = jnp.arange(s0, e)[:, None] >= jnp.arange(e)[None, :]
        s = jnp.where(mask[None, None], s, -jnp.inf)
        p = jax.nn.softmax(s, axis=-1).astype(v.dtype)
        outs.append(jnp.einsum('bhqk,bkhd->bqhd', p, v[:, :e]))
    return jnp.concatenate(outs, axis=1)


def setup_inputs(seed: int = 0) -> dict:
    key = jax.random.key(seed)
    ks = jax.random.split(key, 9)
    x = jax.random.normal(ks[0], (BATCH, SEQ, D_MODEL), jnp.float32)
    w_in = jax.random.normal(ks[1], (DEPTH, D_MODEL, IN_COLS), jnp.float32) * D_MODEL ** -0.5
    norm_g = 1.0 + 0.02 * jax.random.normal(ks[2], (DEPTH, D_MODEL), jnp.float32)
    pool_w = jax.random.normal(ks[3], (DEPTH, len(POOL_WINDOWS), POOL_GROUP, POOL_GROUP), jnp.float32) * POOL_GROUP ** -0.5
    pool_scale = 1.0 + 0.1 * jax.random.normal(ks[4], (DEPTH, POOL_WIDTH), jnp.float32)
    fox_bf = 3.0 + 0.5 * jax.random.normal(ks[5], (DEPTH, FOX_HEADS), jnp.float32)
    w_out = jax.random.normal(ks[6], (DEPTH, MIX_WIDTH, D_MODEL), jnp.float32) * MIX_WIDTH ** -0.5
    final_g = 1.0 + 0.02 * jax.random.normal(ks[7], (D_MODEL,), jnp.float32)
    return {'x': x, 'w_in': w_in, 'norm_g': norm_g, 'pool_w': pool_w, 'pool_scale': pool_scale,
            'fox_bf': fox_bf, 'w_out': w_out, 'final_g': final_g}


def reference(x, w_in, norm_g, pool_w, pool_scale, fox_bf, w_out, final_g):
    B, L, _ = x.shape
    topk = min(DSA_TOPK_MAX, L // 4)
    idx_w_scale = IDX_HEADS ** -0.5
    for layer in range(DEPTH):
        h = rmsnorm(x, norm_g[layer])
        z = jnp.einsum('bld,dc->blc', h, w_in[layer])
        (pv, pg, dq, dk, dv, dg, iq, ik, iw, fq, fk, fv, fg, ff) = split_columns(z)
        a_out = pool_mixer(pv, pool_w[layer], pool_scale[layer]) * jax.nn.silu(pg)
        b_o = dsa_mixer(dq.reshape(B, L, DSA_HEADS, HEAD_DIM), dk.reshape(B, L, DSA_HEADS, HEAD_DIM),
                        dv.reshape(B, L, DSA_HEADS, HEAD_DIM), iq.reshape(B, L, IDX_HEADS, IDX_DIM),
                        ik, iw * idx_w_scale, topk)
        b_out = b_o.reshape(B, L, DSA_WIDTH) * jax.nn.silu(dg)
        log_f = jax.nn.log_sigmoid(ff.astype(jnp.float32) + fox_bf[layer].astype(jnp.float32))
        c_o = fox_mixer(fq.reshape(B, L, FOX_HEADS, HEAD_DIM), fk.reshape(B, L, FOX_HEADS, HEAD_DIM),
                        fv.reshape(B, L, FOX_HEADS, HEAD_DIM), log_f)
        c_out = c_o.reshape(B, L, FOX_WIDTH) * jax.nn.silu(fg)
        mixed = jnp.concatenate([a_out, b_out, c_out], axis=-1)
        x = x + jnp.einsum('blc,cd->bld', mixed, w_out[layer])
    return rmsnorm(x, final_g)
```

```python
import contextlib
import numpy as np
import concourse.bass as bass
import concourse.mybir as mybir
from concourse.bass_utils import run_bass_kernel_spmd

F32 = mybir.dt.float32
BF16 = mybir.dt.bfloat16
ALU = mybir.AluOpType
AF = mybir.ActivationFunctionType
AX = mybir.AxisListType

L_SEQ = 2048
D = 1024
NT = 16
IN_COLS = 3886
NEG = -30000.0
NSTEP = 14
TOPK = 256

C_PV, C_PG, C_DQ, C_DK, C_DV, C_DG = 0, 256, 512, 896, 1280, 1664
C_IQ, C_IK, C_IW, C_FQ, C_FK, C_FV, C_FG, C_FF = 2048, 2304, 2336, 2344, 2728, 3112, 3496, 3880

SEM_SPAN = 3000
SAME_ENG_WINDOW = 5


class Op:
    __slots__ = ("id", "eng", "fn", "deps", "dma", "sem_key", "dcount", "gsig",
                 "eidx", "signals")

    def __init__(self, id, eng, fn, dma, sem_key):
        self.id = id
        self.eng = eng
        self.fn = fn
        self.deps = set()
        self.dma = dma
        self.sem_key = sem_key
        self.dcount = 0
        self.gsig = 0
        self.eidx = 0
        self.signals = False


class Sched:
    ENGS = ("pe", "act", "dve", "pool", "sp")

    def __init__(self):
        self.ops = []
        self.last_writer = {}
        self.readers = {}
        self.eng_count = {e: 0 for e in self.ENGS}
        self.dma_counts = {}
        self.last_dma = {}

    def add(self, eng, fn, reads=(), writes=(), dma=False, sem_key=None, extra_deps=()):
        op = Op(len(self.ops), eng, fn, dma, sem_key)
        if dma:
            assert sem_key is not None
            self.dma_counts[sem_key] = self.dma_counts.get(sem_key, 0) + 1
            op.dcount = self.dma_counts[sem_key]
            self.last_dma[sem_key] = op.id
        op.eidx = self.eng_count[eng]
        self.eng_count[eng] += 1
        for k in list(reads) + list(writes):
            w = self.last_writer.get(k)
            if w is not None:
                op.deps.add(w)
        for k in writes:
            for r in self.readers.get(k, ()):
                op.deps.add(r)
        for k in reads:
            self.readers.setdefault(k, []).append(op.id)
        for k in writes:
            self.last_writer[k] = op.id
            self.readers[k] = []
        for d in extra_deps:
            op.deps.add(d)
        op.deps.discard(op.id)
        self.ops.append(op)
        return op

    def barrier(self):
        last = {}
        for op in self.ops:
            if not op.dma and op.fn is not None:
                last[op.eng] = op.id
        deps = list(last.values()) + list(self.last_dma.values())
        for e in self.ENGS:
            self.add(e, None, extra_deps=deps)

    def emit(self, nc):
        ops = self.ops
        for op in ops:
            for d in op.deps:
                p = ops[d]
                if p.dma or p.fn is None:
                    continue
                if p.eng == op.eng:
                    if op.eng == "pe":
                        continue
                    if op.eidx - p.eidx > SAME_ENG_WINDOW:
                        continue
                p.signals = True
        gs = {e: 0 for e in self.ENGS}
        for op in ops:
            if op.signals and not op.dma:
                gs[op.eng] += 1
                op.gsig = gs[op.eng]
        stack = []
        sems = {}
        for e in self.ENGS:
            for i in range((gs[e] + SEM_SPAN - 1) // SEM_SPAN):
                cm = nc.semaphore(f"s_{e}_{i}")
                sems[(e, i)] = cm.__enter__()
                stack.append(cm)
        dsems = {}
        for k in self.dma_counts:
            cm = nc.semaphore(f"d_{len(dsems)}")
            dsems[k] = cm.__enter__()
            stack.append(cm)
        self.n_sems = len(stack)
        per_eng = {e: [op for op in ops if op.eng == e] for e in self.ENGS}

        def run_engine(e, eng):
            waited = {x: 0 for x in self.ENGS}
            dwaited = {}
            for op in per_eng[e]:
                need = {}
                dneed = {}
                for d in op.deps:
                    p = ops[d]
                    if p.fn is None:
                        continue
                    if p.dma:
                        if dwaited.get(p.sem_key, 0) < p.dcount:
                            dneed[p.sem_key] = max(dneed.get(p.sem_key, 0), p.dcount)
                        continue
                    if p.eng == e:
                        if e == "pe" or op.eidx - p.eidx > SAME_ENG_WINDOW:
                            continue
                    if p.gsig > waited[p.eng]:
                        need[p.eng] = max(need.get(p.eng, 0), p.gsig)
                for pe_, g in need.items():
                    si = (g - 1) // SEM_SPAN
                    eng.wait_ge(sems[(pe_, si)], g - si * SEM_SPAN)
                    waited[pe_] = g
                for k, c in dneed.items():
                    eng.wait_ge(dsems[k], 16 * c)
                    dwaited[k] = c
                if op.fn is None:
                    continue
                ins = op.fn(eng)
                if op.dma:
                    ins.then_inc(dsems[op.sem_key], 16)
                elif op.signals:
                    si = (op.gsig - 1) // SEM_SPAN
                    ins.then_inc(sems[(op.eng, si)], 1)

        with nc.Block() as block:
            @block.tensor
            def _(eng):
                run_engine("pe", eng)

            @block.scalar
            def _(eng):
                run_engine("act", eng)

            @block.vector
            def _(eng):
                run_engine("dve", eng)

            @block.gpsimd
            def _(eng):
                run_engine("pool", eng)

            @block.sync
            def _(eng):
                run_engine("sp", eng)
        for cm in reversed(stack):
            cm.__exit__(None, None, None)


def build(n_seq=2, n_layers=4, lvl=9):
    nc = bass.Bass("TRN2", target_bir_lowering=False)
    x_d = nc.dram_tensor("x", [n_seq, L_SEQ, D], F32, kind="ExternalInput").ap()
    win_d = nc.dram_tensor("w_in", [4, D, IN_COLS], F32, kind="ExternalInput").ap()
    ng_d = nc.dram_tensor("norm_g", [4, D], F32, kind="ExternalInput").ap()
    pw_d = nc.dram_tensor("pool_w", [4, 4, 64, 64], F32, kind="ExternalInput").ap()
    psc_d = nc.dram_tensor("pool_scale", [4, 256], F32, kind="ExternalInput").ap()
    fbf_d = nc.dram_tensor("fox_bf", [4, 6], F32, kind="ExternalInput").ap()
    wout_d = nc.dram_tensor("w_out", [4, D, D], F32, kind="ExternalInput").ap()
    fg_d = nc.dram_tensor("final_g", [D], F32, kind="ExternalInput").ap()
    y_d = nc.dram_tensor("y", [n_seq, L_SEQ, D], F32, kind="ExternalOutput").ap()

    S = Sched()
    A = S.add
    es = contextlib.ExitStack()

    def sb(name, shape, dt):
        return es.enter_context(nc.sbuf_tensor(name, shape, dt))

    def pst(name, shape, dt):
        return es.enter_context(nc.psum_tensor(name, shape, dt))

    X = sb("X", [128, NT, D], F32)
    HT = sb("HT", [128, NT * 8 * 128], BF16)
    MIXT = sb("MIXT", [128, 3, L_SEQ], BF16)
    QT = sb("QT", [128, 3, L_SEQ], BF16)
    KT = sb("KT", [128, 3, L_SEQ], BF16)
    VT = sb("VT", [128, NT, 384], BF16)
    WB = [sb("WB0", [128, 8, 392], BF16), sb("WB1", [128, 8, 392], BF16)]
    WOB = sb("WOB", [128, 3, D], BF16)
    WFF = sb("WFF", [128, 8, 128], BF16)
    SCR = sb("SCR", [128, 24576], mybir.dt.uint8)
    PT = [sb(f"PT{i}", [128, 512], BF16) for i in range(4)]
    RB = [sb(f"RB{i}", [128, 512], BF16) for i in range(3)]
    RD = sb("RD", [128, 512], F32)
    TMPF = sb("TMPF", [128, 512], F32)
    ident = sb("ident", [128, 128], BF16)
    tri = sb("tri", [128, 128], BF16)
    Utri = sb("Utri", [128, 128], F32)
    onesf = sb("onesf", [128, 128], F32)
    ones64 = sb("ones64", [128, 64], BF16)
    dmaskf = sb("dmaskf", [128, 128], F32)
    dmaskb = sb("dmaskb", [128, 128], BF16)
    halves = sb("halves", [128, NSTEP + 1], F32)
    invw = sb("invw", [128, 2], F32)
    invcnt = sb("invcnt", [128, 2, 16], F32)
    SS = sb("SS", [128, NT], F32)
    RSTD = sb("RSTD", [128, NT], F32)
    PSCL = sb("PSCL", [128, 2], F32)
    POOLW = sb("POOLW", [128, 2, 128], BF16)
    FBF = sb("FBF", [128, 6], F32)
    IW = sb("IW", [128, NT, 8], F32)
    FF = sb("FF", [128, NT, 6], F32)
    TOT = sb("TOT", [128, NT, 6], F32)
    OFFS = sb("OFFS", [128, NT + 1, 6], F32)
    GG = sb("GG", [128, NT, 6], F32)
    DW = sb("DW", [128, 8, 128], BF16)
    BS = sb("BS", [128, 8 + NSTEP + 1], F32)

    def view(t, dt, byte_off, n_elem):
        ap = t[:, :]
        apb = ap.bitcast(dt)
        esz = mybir.dt.size(dt)
        tsz = mybir.dt.size(t.dtype)
        e0 = byte_off // esz
        return apb[:, e0:e0 + n_elem]

    HB = [view(SCR, BF16, 16384, 1024), view(SCR, BF16, 18432, 1024)]
    GB = view(SCR, F32, 20480, 1024)
    JUNKN = view(SCR, BF16, 8192, 1024)
    VBUF = view(SCR, F32, 0, 528)
    T1 = view(SCR, F32, 2112, 528)
    T2 = view(SCR, F32, 4224, 528)
    PTP = view(SCR, BF16, 8192, 4096)
    BIASALL = view(SCR, F32, 16384, 16 * 16 * 6)
    IQT = view(SCR, BF16, 0, 3 * 2048)
    IKT = view(SCR, BF16, 12288, 2048)
    MB = [view(HT, BF16, 4096 * q, 2048) for q in range(4)]
    SCORE = view(HT, F32, 16384, 2048)
    YST = [view(HT, F32, 0, 1024), view(HT, F32, 4096, 1024)]

    def ht(i, k):
        o = (i * 8 + k) * 128
        return HT[:, o:o + 128]

    HT4 = HT[:, :].rearrange("p (i k t) -> p i k t", i=NT, k=8)

    PTR = pst("PTR", [128, 1024], BF16)
    PA = [pst("PA0", [128, 512], F32), pst("PA1", [128, 512], F32)]
    PS = [pst("PS0", [128, 512], F32), pst("PS1", [128, 512], F32)]
    PO = [pst("PO0", [128, 512], F32), pst("PO1", [128, 512], F32)]
    PD = [pst("PD0", [128, 512], F32)]
    rot = {"pa": 0, "ps": 0, "po": 0, "pt": 0, "rb": 0, "wb": 0, "hb": 0}

    def nxt(name, n):
        v = rot[name]
        rot[name] = (v + 1) % n
        return v

    A("pool", lambda e: e.memset(TMPF[:, 0:128], 0.0), writes=["TMPF"])
    A("pool", lambda e: e.affine_select(out=TMPF[:, 0:128], in_=TMPF[:, 0:128], pattern=[[-1, 128]],
                                        compare_op=ALU.not_equal, fill=1.0, base=0,
                                        channel_multiplier=1), reads=["TMPF"], writes=["TMPF"])
    A("dve", lambda e: e.tensor_copy(out=ident[:], in_=TMPF[:, 0:128]), reads=["TMPF"], writes=["ident"])
    A("pool", lambda e: e.memset(TMPF[:, 0:128], 0.0), writes=["TMPF"])
    A("pool", lambda e: e.affine_select(out=TMPF[:, 0:128], in_=TMPF[:, 0:128], pattern=[[1, 128]],
                                        compare_op=ALU.is_ge, fill=NEG, base=0,
                                        channel_multiplier=-1), reads=["TMPF"], writes=["TMPF"])
    A("dve", lambda e: e.tensor_copy(out=tri[:], in_=TMPF[:, 0:128]), reads=["TMPF"], writes=["tri"])
    A("pool", lambda e: e.memset(Utri[:], 1.0), writes=["Utri"])
    A("pool", lambda e: e.affine_select(out=Utri[:], in_=Utri[:], pattern=[[1, 128]],
                                        compare_op=ALU.is_ge, fill=0.0, base=0,
                                        channel_multiplier=-1), reads=["Utri"], writes=["Utri"])
    A("pool", lambda e: e.memset(onesf[:], 1.0), writes=["onesf"])
    A("pool", lambda e: e.memset(ones64[:], 1.0), writes=["ones64"])
    A("pool", lambda e: e.memset(dmaskf[:], 0.0), writes=["dmaskf"])
    A("pool", lambda e: e.memset(dmaskf[0:64, 64:128], -1.0e30), reads=["dmaskf"], writes=["dmaskf"])
    A("pool", lambda e: e.memset(dmaskb[:], 0.0), writes=["dmaskb"])
    A("pool", lambda e: e.memset(dmaskb[0:64, 64:128], NEG), reads=["dmaskb"], writes=["dmaskb"])
    for k in range(NSTEP + 1):
        A("pool", lambda e, k=k: e.memset(halves[:, k:k + 1], 0.5 ** (k + 1)), writes=["halves"])
    for j, (wa, wb_) in enumerate(((2, 4), (8, 16))):
        A("pool", lambda e, j=j, wa=wa: e.memset(invw[0:64, j:j + 1], 1.0 / wa), writes=["invw"])
        A("pool", lambda e, j=j, wb_=wb_: e.memset(invw[64:128, j:j + 1], 1.0 / wb_), writes=["invw"])
        for t in range(16):
            A("pool", lambda e, j=j, t=t, wa=wa: e.memset(invcnt[0:64, j, t:t + 1], 1.0 / min(t + 1, wa)),
              writes=["invcnt"])
            A("pool", lambda e, j=j, t=t, wb_=wb_: e.memset(invcnt[64:128, j, t:t + 1], 1.0 / min(t + 1, wb_)),
              writes=["invcnt"])
    A("pool", lambda e: e.memset(OFFS[:, 0, :], 0.0), writes=["OFFS"])

    def load_w(l, pieces):
        b = nxt("wb", 2)
        for (c0, n, s0) in pieces:
            A("pool", lambda e, b=b, c0=c0, n=n, s0=s0: e.dma_start(
                out=WB[b][:, :, s0:s0 + n],
                in_=win_d[l].rearrange("(k p) c -> p k c", p=128)[:, :, c0:c0 + n]),
              writes=[f"WB{b}"], dma=True, sem_key=f"WB{b}")
        return b

    def proj_cm(b, col0, M, evac):
        for c in range(4):
            pa = nxt("pa", 2)
            for k in range(8):
                A("pe", lambda e, pa=pa, k=k, c=c: e.matmul(
                    PA[pa][0:M, :], lhsT=WB[b][:, k, col0:col0 + M],
                    rhs=HT4[:, 4 * c:4 * c + 4, k, :], start=(k == 0), stop=(k == 7)),
                  reads=[f"WB{b}"] + [f"HT{i}" for i in range(4 * c, 4 * c + 4)], writes=[f"PA{pa}"])
            evac(c, PA[pa], f"PA{pa}")

    def proj_tm(b, N, evac):
        for i in range(NT):
            pa = nxt("pa", 2)
            for k in range(8):
                A("pe", lambda e, pa=pa, k=k, i=i: e.matmul(
                    PA[pa][:, 0:N], lhsT=ht(i, k), rhs=WB[b][:, k, 0:N],
                    start=(k == 0), stop=(k == 7)),
                  reads=[f"WB{b}", f"HT{i}"], writes=[f"PA{pa}"])
            evac(i, PA[pa], f"PA{pa}")

    cnt = {"alt": 0}

    def copy_alt(out, in_, reads, writes, scale=None):
        cnt["alt"] += 1
        if cnt["alt"] % 2 == 0:
            if scale is None:
                A("act", lambda e: e.activation(out=out, in_=in_, func=AF.Copy), reads=reads, writes=writes)
            else:
                A("act", lambda e: e.activation(out=out, in_=in_, func=AF.Copy, scale=scale),
                  reads=reads, writes=writes)
        else:
            if scale is None:
                A("dve", lambda e: e.tensor_copy(out=out, in_=in_), reads=reads, writes=writes)
            else:
                A("dve", lambda e: e.tensor_scalar(out=out, in0=in_, scalar1=scale, scalar2=None,
                                                   op0=ALU.mult), reads=reads, writes=writes)

    def out_proj(l, row0, nch):
        A("pool", lambda e: e.dma_start(
            out=WOB[:, 0:nch, :],
            in_=wout_d[l, row0:row0 + nch * 128, :].rearrange("(q p) c -> p q c", p=128)),
          writes=["WOB"], dma=True, sem_key="WOB")
        for i in range(NT):
            for n in range(2):
                pa = nxt("pa", 2)
                for q in range(nch):
                    A("pe", lambda e, pa=pa, q=q, i=i, n=n: e.matmul(
                        PA[pa][:, :], lhsT=MIXT[:, q, i * 128:(i + 1) * 128],
                        rhs=WOB[:, q, n * 512:(n + 1) * 512], start=(q == 0), stop=(q == nch - 1)),
                      reads=["WOB", f"MIXT{q}"], writes=[f"PA{pa}"])
                A("dve", lambda e, pa=pa, i=i, n=n: e.tensor_tensor(
                    out=X[:, i, n * 512:(n + 1) * 512], in0=X[:, i, n * 512:(n + 1) * 512],
                    in1=PA[pa][:, :], op=ALU.add),
                  reads=[f"PA{pa}", f"X{i}"], writes=[f"X{i}"])

    def rms_stats():
        for i in range(NT):
            A("act", lambda e, i=i: e.activation(out=JUNKN, in_=X[:, i, :], func=AF.Square,
                                                 accum_out=SS[:, i:i + 1]),
              reads=[f"X{i}"], writes=["JUNKN", "SS"])
        A("act", lambda e: e.activation(out=RSTD[:], in_=SS[:], func=AF.Sqrt, bias=EPS_T[:, 0:1], scale=1.0 / D),
          reads=["SS"], writes=["RSTD"])
        A("dve", lambda e: e.reciprocal(out=RSTD[:], in_=RSTD[:]), reads=["RSTD"], writes=["RSTD"])

    EPS_T = sb("EPS_T", [128, 1], F32)
    A("pool", lambda e: e.memset(EPS_T[:], 1e-6), writes=["EPS_T"])
    C255 = sb("C255", [128, 1], F32)
    A("pool", lambda e: e.memset(C255[:], TOPK - 0.5), writes=["C255"])
    CNEG = sb("CNEG", [128, 1], F32)
    A("pool", lambda e: e.memset(CNEG[:], NEG), writes=["CNEG"])

    def attention(kind, biasfn=None):
        for c in range(4):
            if kind == "dsa":
                dsa_masks(c)
            for p in range(3):
                po = nxt("po", 2)
                pd = 0
                nj = 4 * c + 4
                for j in range(nj):
                    i_lo = max(4 * c, j)
                    off = (i_lo - 4 * c) * 128
                    t0 = 4 * c * 128
                    for hh in range(2):
                        h = 2 * p + hh
                        r0 = 64 * hh
                        ps = nxt("ps", 2)
                        kslc = KT[r0:r0 + 64, p, j * 128:(j + 1) * 128]
                        psk = f"PS{ps}"
                        if kind == "fox":
                            if j >= 4 * c:
                                A("pe", lambda e, ps=ps, off=off: e.matmul(
                                    PS[ps][:, off:off + 128], lhsT=ident[:], rhs=tri[:], start=True, stop=False),
                                  reads=["ident", "tri"], writes=[psk])
                                A("pe", lambda e, ps=ps, off=off, kslc=kslc, r0=r0, p=p, t0=t0: e.matmul(
                                    PS[ps][:, off:off + 128], lhsT=kslc,
                                    rhs=QT[r0:r0 + 64, p, t0 + off:t0 + off + 128], start=False, stop=True),
                                  reads=[f"KT{p}", f"QT{p}"], writes=[psk])
                                if off + 128 < 512:
                                    A("pe", lambda e, ps=ps, off=off, kslc=kslc, r0=r0, p=p, t0=t0: e.matmul(
                                        PS[ps][:, off + 128:512], lhsT=kslc,
                                        rhs=QT[r0:r0 + 64, p, t0 + off + 128:t0 + 512], start=True, stop=True),
                                      reads=[f"KT{p}", f"QT{p}"], writes=[psk])
                            else:
                                A("pe", lambda e, ps=ps, kslc=kslc, r0=r0, p=p, t0=t0: e.matmul(
                                    PS[ps][:, 0:512], lhsT=kslc, rhs=QT[r0:r0 + 64, p, t0:t0 + 512],
                                    start=True, stop=True),
                                  reads=[f"KT{p}", f"QT{p}"], writes=[psk])
                            pt = nxt("pt", 4)
                            for i in range(i_lo, 4 * c + 4):
                                co = (i - 4 * c) * 128
                                A("act", lambda e, ps=ps, pt=pt, co=co, i=i, j=j, h=h: e.activation(
                                    out=PT[pt][:, co:co + 128], in_=PS[ps][:, co:co + 128], func=AF.Exp,
                                    bias=BIASALL[:, (i * 16 + j) * 6 + h:(i * 16 + j) * 6 + h + 1], scale=1.0),
                                  reads=[psk, "BIASALL"], writes=[f"PT{pt}"])
                        else:
                            for i in range(i_lo, 4 * c + 4):
                                co = (i - 4 * c) * 128
                                q = i - 4 * c
                                A("pe", lambda e, ps=ps, co=co, q=q, j=j: e.matmul(
                                    PS[ps][:, co:co + 128], lhsT=MB[q][:, j * 128:(j + 1) * 128], rhs=ident[:],
                                    start=True, stop=False),
                                  reads=[f"MB{q}", "ident"], writes=[psk])
                                A("pe", lambda e, ps=ps, co=co, kslc=kslc, r0=r0, p=p, t0=t0: e.matmul(
                                    PS[ps][:, co:co + 128], lhsT=kslc,
                                    rhs=QT[r0:r0 + 64, p, t0 + co:t0 + co + 128], start=False, stop=True),
                                  reads=[f"KT{p}", f"QT{p}"], writes=[psk])
                            pt = nxt("pt", 4)
                            A("act", lambda e, ps=ps, pt=pt, off=off: e.activation(
                                out=PT[pt][:, off:512], in_=PS[ps][:, off:512], func=AF.Exp),
                              reads=[psk], writes=[f"PT{pt}"])
                        A("pe", lambda e, po=po, pt=pt, off=off, j=j, h=h, r0=r0, nj=nj: e.matmul(
                            PO[po][r0:r0 + 64, off:512], lhsT=VT[:, j, h * 64:(h + 1) * 64], rhs=PT[pt][:, off:512],
                            start=(j == 0), stop=(j == nj - 1), tile_position=(0, r0)),
                          reads=[f"PT{pt}", "VT"], writes=[f"PO{po}"])
                        A("pe", lambda e, pd=pd, pt=pt, off=off, j=j, r0=r0, nj=nj: e.matmul(
                            PD[pd][r0:r0 + 64, off:512], lhsT=ones64[:], rhs=PT[pt][:, off:512],
                            start=(j == 0), stop=(j == nj - 1), tile_position=(0, r0)),
                          reads=[f"PT{pt}", "ones64"], writes=[f"PD{pd}"])
                csl = slice(4 * c * 128, (4 * c + 4) * 128)
                A("dve", lambda e, pd=pd: e.reciprocal(out=RD[:], in_=PD[pd][:, :]),
                  reads=[f"PD{pd}"], writes=["RD"])
                A("dve", lambda e, po=po: e.tensor_tensor(out=TMPF[:], in0=PO[po][:, :], in1=RD[:], op=ALU.mult),
                  reads=[f"PO{po}", "RD"], writes=["TMPF"])
                A("dve", lambda e, p=p, csl=csl: e.tensor_tensor(out=MIXT[:, p, csl], in0=TMPF[:],
                                                                in1=MIXT[:, p, csl], op=ALU.mult),
                  reads=["TMPF", f"MIXT{p}"], writes=[f"MIXT{p}"])

    def dsa_masks(c):
        for q in range(4):
            i = 4 * c + q
            n = (i + 1) * 128
            mk = f"MB{q}"
            if i < 2:
                if i == 1:
                    A("pool", lambda e, q=q: e.memset(MB[q][:, 0:128], 0.0), writes=[mk])
                A("pool", lambda e, q=q, n=n: e.tensor_copy(out=MB[q][:, n - 128:n], in_=dmaskb[:]),
                  reads=["dmaskb"], writes=[mk])
                continue
            for h in range(8):
                A("pool", lambda e, i=i, h=h: e.tensor_scalar(out=DW[:, h, :], in0=ident[:],
                                                             scalar1=IW[:, i, h:h + 1], scalar2=None,
                                                             op0=ALU.mult),
                  reads=["ident", "IW"], writes=[f"DW{h}"])
            nsc = (n + 511) // 512
            for sc in range(nsc):
                w = min(512, n - sc * 512)
                psc = nxt("ps", 2)
                for h in range(8):
                    ti, r = h // 3, (h % 3) * 32
                    pa = nxt("pa", 2)
                    rb = nxt("rb", 3)
                    A("pe", lambda e, pa=pa, ti=ti, r=r, i=i, sc=sc, w=w: e.matmul(
                        PA[pa][:, 0:w], lhsT=IQT[r:r + 32, ti * 2048 + i * 128:ti * 2048 + (i + 1) * 128],
                        rhs=IKT[r:r + 32, sc * 512:sc * 512 + w], start=True, stop=True),
                      reads=["IQT", "IKT"], writes=[f"PA{pa}"])
                    A("act", lambda e, pa=pa, rb=rb, w=w: e.activation(out=RB[rb][:, 0:w], in_=PA[pa][:, 0:w],
                                                                       func=AF.Relu),
                      reads=[f"PA{pa}"], writes=[f"RB{rb}"])
                    A("pe", lambda e, psc=psc, rb=rb, h=h, w=w: e.matmul(
                        PS[psc][:, 0:w], lhsT=DW[:, h, :], rhs=RB[rb][:, 0:w], start=(h == 0), stop=(h == 7)),
                      reads=[f"DW{h}", f"RB{rb}"], writes=[f"PS{psc}"])
                s0 = sc * 512
                if sc == nsc - 1:
                    if w > 128:
                        A("dve", lambda e, psc=psc, s0=s0, w=w: e.tensor_copy(
                            out=SCORE[:, s0:s0 + w - 128], in_=PS[psc][:, 0:w - 128]),
                          reads=[f"PS{psc}"], writes=["SCORE"])
                    A("dve", lambda e, psc=psc, s0=s0, w=w: e.tensor_tensor(
                        out=SCORE[:, s0 + w - 128:s0 + w], in0=PS[psc][:, w - 128:w], in1=dmaskf[:], op=ALU.add),
                      reads=[f"PS{psc}", "dmaskf"], writes=["SCORE"])
                else:
                    A("dve", lambda e, psc=psc, s0=s0, w=w: e.tensor_copy(
                        out=SCORE[:, s0:s0 + w], in_=PS[psc][:, 0:w]),
                      reads=[f"PS{psc}"], writes=["SCORE"])
            eng = "dve"
            mx, mn, rg, mid, cn, tp = (BS[:, 0:1], BS[:, 1:2], BS[:, 2:3], BS[:, 3:4], BS[:, 4:5], BS[:, 5:6])
            DL = BS[:, 8:8 + NSTEP + 1]
            A(eng, lambda e, n=n: e.tensor_reduce(out=mx, in_=SCORE[:, 0:n], axis=AX.X, op=ALU.max),
              reads=["SCORE"], writes=["BSmx"])
            A(eng, lambda e, n=n: e.tensor_reduce(out=mn, in_=SCORE[:, 0:n - 64], axis=AX.X, op=ALU.min),
              reads=["SCORE"], writes=["BSmn"])
            A(eng, lambda e: e.tensor_tensor(out=rg, in0=mx, in1=mn, op=ALU.subtract),
              reads=["BSmx", "BSmn"], writes=["BSrg"])
            A(eng, lambda e: e.tensor_scalar(out=DL, in0=halves[:], scalar1=rg, scalar2=None, op0=ALU.mult),
              reads=["BSrg", "halves"], writes=["BSdl"])
            A(eng, lambda e: e.tensor_tensor(out=mid, in0=mn, in1=BS[:, 8:9], op=ALU.add),
              reads=["BSmn", "BSdl"], writes=["BSmid"])
            for k in range(NSTEP):
                A(eng, lambda e, q=q, n=n: e.tensor_scalar(out=MB[q][:, 0:n], in0=SCORE[:, 0:n], scalar1=mid,
                                                           scalar2=None, op0=ALU.is_ge, op1=ALU.add,
                                                           accum_out=cn),
                  reads=["SCORE", "BSmid"], writes=[mk, "BScn"])
                A(eng, lambda e, k=k: e.tensor_scalar(out=tp, in0=cn, scalar1=C255[:, 0:1],
                                                      scalar2=BS[:, 8 + k:9 + k], op0=ALU.is_ge, op1=ALU.mult),
                  reads=["BScn", "BSdl"], writes=["BStp"])
                A(eng, lambda e, k=k: e.scalar_tensor_tensor(out=mid, in0=tp, scalar=BS[:, 9 + k:10 + k], in1=mid,
                                                             op0=ALU.subtract, op1=ALU.add),
                  reads=["BStp", "BSdl", "BSmid"], writes=["BSmid"])
            A(eng, lambda e: e.tensor_tensor(out=tp, in0=mid, in1=BS[:, 8 + NSTEP:9 + NSTEP], op=ALU.subtract),
              reads=["BSmid", "BSdl"], writes=["BStp"])
            A(eng, lambda e, q=q, n=n: e.tensor_scalar(out=MB[q][:, 0:n], in0=SCORE[:, 0:n], scalar1=tp,
                                                       scalar2=CNEG[:, 0:1], op0=ALU.is_lt, op1=ALU.mult),
              reads=["SCORE", "BStp"], writes=[mk])

    for s in range(n_seq):
        for qd in range(4):
            A("sp", lambda e, s=s, qd=qd: e.dma_start(
                out=X[:, 4 * qd:4 * qd + 4, :],
                in_=x_d[s].rearrange("(i p) d -> p i d", p=128)[:, 4 * qd:4 * qd + 4, :]),
              writes=[f"X{i}" for i in range(4 * qd, 4 * qd + 4)], dma=True, sem_key=f"XL{qd}")
        for l in range(n_layers):
            if lvl < 1:
                break
            A("sp", lambda e, l=l: e.dma_start(out=GB, in_=ng_d[l:l + 1, :].to_broadcast([128, D])),
              writes=["GB"], dma=True, sem_key="GB")
            for j in range(2):
                A("sp", lambda e, l=l, j=j: e.dma_start(
                    out=PSCL[:, j:j + 1], in_=psc_d[l, j * 128:(j + 1) * 128].rearrange("(p o) -> p o", o=1)),
                  writes=["PSCL"], dma=True, sem_key=f"PSCL{j}")
            A("sp", lambda e, l=l: e.dma_start(out=FBF[:], in_=fbf_d[l:l + 1, :].to_broadcast([128, 6])),
              writes=["FBF"], dma=True, sem_key="FBF")
            A("pool", lambda e: e.memset(POOLW[:], 0.0), writes=["POOLW"])
            for g in range(4):
                j, r0 = g // 2, (g % 2) * 64
                A("pool", lambda e, l=l, g=g, j=j, r0=r0: e.dma_start(
                    out=POOLW[r0:r0 + 64, j, r0:r0 + 64], in_=pw_d[l, g]),
                  reads=[], writes=["POOLW"], dma=True, sem_key=f"POOLW{g}")
            rms_stats()
            for i in range(NT):
                hb = nxt("hb", 2)
                A("dve", lambda e, i=i, hb=hb: e.scalar_tensor_tensor(
                    out=HB[hb], in0=X[:, i, :], scalar=RSTD[:, i:i + 1], in1=GB, op0=ALU.mult, op1=ALU.mult),
                  reads=[f"X{i}", "RSTD", "GB"], writes=[f"HB{hb}"])
                for k in range(8):
                    A("pe", lambda e, hb=hb, k=k: e.transpose(out=PTR[:, k * 128:(k + 1) * 128],
                                                              in_=HB[hb][:, k * 128:(k + 1) * 128],
                                                              identity=ident[:]),
                      reads=[f"HB{hb}", "ident"], writes=["PTR"])
                copy_alt(HT[:, i * 1024:(i + 1) * 1024], PTR[:, :], ["PTR"], [f"HT{i}"])

            if lvl < 2:
                S.barrier()
                continue
            b = load_w(l, [(C_PG, 256, 0)])
            for j in range(2):
                def ev_g(c, ps_ap, key, j=j):
                    A("act", lambda e: e.activation(out=MIXT[:, j, c * 512:(c + 1) * 512], in_=ps_ap[:, :],
                                                    func=AF.Silu), reads=[key], writes=[f"MIXT{j}"])
                proj_cm(b, j * 128, 128, ev_g)
            b = load_w(l, [(C_PV, 256, 0)])
            for j in range(2):
                srcA = (T1, T2)
                def ev_v(c, ps_ap, key, j=j):
                    if c == 0:
                        A("pool", lambda e: e.memset(VBUF[:, 0:16], 0.0), writes=["VB_h"])
                    else:
                        A("pool", lambda e: e.tensor_copy(out=VBUF[:, 0:16], in_=VBUF[:, 512:528]),
                          reads=["VB_m"], writes=["VB_h"])
                    A("act", lambda e: e.activation(out=VBUF[:, 16:528], in_=ps_ap[:, :], func=AF.Copy),
                      reads=[key, "VB_h"], writes=["VB_m"])
                    rk = ["VB_h", "VB_m"]
                    A("pool", lambda e: e.tensor_tensor(out=T1[:, 1:528], in0=VBUF[:, 1:528], in1=VBUF[:, 0:527],
                                                        op=ALU.add), reads=rk, writes=["T1"])
                    if j == 0:
                        A("pool", lambda e: e.tensor_tensor(out=T2[64:128, 3:528], in0=T1[64:128, 3:528],
                                                            in1=T1[64:128, 1:526], op=ALU.add),
                          reads=["T1"], writes=["T2"])
                        fa, fb = T1, T2
                    else:
                        A("pool", lambda e: e.tensor_tensor(out=T2[:, 3:528], in0=T1[:, 3:528], in1=T1[:, 1:526],
                                                            op=ALU.add), reads=["T1"], writes=["T2"])
                        A("pool", lambda e: e.tensor_tensor(out=T1[:, 7:528], in0=T2[:, 7:528], in1=T2[:, 3:524],
                                                            op=ALU.add), reads=["T2"], writes=["T1"])
                        A("pool", lambda e: e.tensor_tensor(out=T2[64:128, 15:528], in0=T1[64:128, 15:528],
                                                            in1=T1[64:128, 7:520], op=ALU.add),
                          reads=["T1"], writes=["T2"])
                        fa, fb = T1, T2
                    o0 = j * 2048 + c * 512
                    for (rs, src) in ((slice(0, 64), fa), (slice(64, 128), fb)):
                        A("pool", lambda e, rs=rs, src=src: e.tensor_scalar(
                            out=TMPF[rs, 0:512], in0=src[rs, 16:528], scalar1=invw[rs, j:j + 1], scalar2=None,
                            op0=ALU.mult),
                          reads=["T1", "T2", "invw"], writes=["TMPF"])
                        A("pool", lambda e, rs=rs: e.tensor_tensor(
                            out=PTP[rs, o0:o0 + 512], in0=TMPF[rs, 0:512], in1=VBUF[rs, 16:528], op=ALU.subtract),
                          reads=["TMPF", "VB_m"], writes=[f"PTP{j}"])
                        if c == 0:
                            A("pool", lambda e, rs=rs, src=src: e.tensor_tensor(
                                out=TMPF[rs, 0:16], in0=src[rs, 16:32], in1=invcnt[rs, j, :], op=ALU.mult),
                              reads=["T1", "T2", "invcnt"], writes=["TMPF"])
                            A("pool", lambda e, rs=rs: e.tensor_tensor(
                                out=PTP[rs, o0:o0 + 16], in0=TMPF[rs, 0:16], in1=VBUF[rs, 16:32], op=ALU.subtract),
                              reads=["TMPF", "VB_m", f"PTP{j}"], writes=[f"PTP{j}"])
                proj_cm(b, j * 128, 128, ev_v)
            for j in range(2):
                for c in range(4):
                    pa = nxt("pa", 2)
                    A("pe", lambda e, pa=pa, j=j, c=c: e.matmul(
                        PA[pa][:, :], lhsT=POOLW[:, j, :], rhs=PTP[:, j * 2048 + c * 512:j * 2048 + (c + 1) * 512],
                        start=True, stop=True),
                      reads=["POOLW", f"PTP{j}"] + [f"POOLW{g}" for g in range(4)], writes=[f"PA{pa}"])
                    A("dve", lambda e, pa=pa, j=j, c=c: e.scalar_tensor_tensor(
                        out=MIXT[:, j, c * 512:(c + 1) * 512], in0=PA[pa][:, :], scalar=PSCL[:, j:j + 1],
                        in1=MIXT[:, j, c * 512:(c + 1) * 512], op0=ALU.mult, op1=ALU.mult),
                      reads=[f"PA{pa}", "PSCL", f"MIXT{j}"], writes=[f"MIXT{j}"])
            out_proj(l, 0, 2)

            if lvl < 3:
                S.barrier()
                continue
            def qk_proj(c_q, c_k):
                bq = load_w(l, [(c_q, 384, 0)])
                for p in range(3):
                    def ev(c, ps_ap, key, p=p):
                        copy_alt(QT[:, p, c * 512:(c + 1) * 512], ps_ap[:, :], [key], [f"QT{p}"], scale=0.125)
                    proj_cm(bq, p * 128, 128, ev)
                bk = load_w(l, [(c_k, 384, 0)])
                for p in range(3):
                    def ev(c, ps_ap, key, p=p):
                        copy_alt(KT[:, p, c * 512:(c + 1) * 512], ps_ap[:, :], [key], [f"KT{p}"])
                    proj_cm(bk, p * 128, 128, ev)

            def gate_proj(c_g):
                bg = load_w(l, [(c_g, 384, 0)])
                for p in range(3):
                    def ev(c, ps_ap, key, p=p):
                        A("act", lambda e: e.activation(out=MIXT[:, p, c * 512:(c + 1) * 512], in_=ps_ap[:, :],
                                                        func=AF.Silu), reads=[key], writes=[f"MIXT{p}"])
                    proj_cm(bg, p * 128, 128, ev)

            if lvl >= 3.01:
                qk_proj(C_FQ, C_FK)
            if lvl < 3.02:
                S.barrier()
                continue
            gate_proj(C_FG)
            if lvl < 3.03:
                S.barrier()
                continue
            bv = load_w(l, [(C_FV, 384, 0)])
            A("pool", lambda e, l=l: e.dma_start(
                out=WFF[:, :, :],
                in_=win_d[l].rearrange("(k p) c -> p k c", p=128)[:, :, IN_COLS - 128:IN_COLS]),
              writes=["WFF"], dma=True, sem_key="WFF")

            def ev_fv(i, ps_ap, key):
                A("dve", lambda e: e.tensor_copy(out=VT[:, i, :], in_=ps_ap[:, 0:384]), reads=[key], writes=["VT"])
                pa = nxt("pa", 2)
                for k in range(8):
                    A("pe", lambda e, pa=pa, k=k: e.matmul(PA[pa][:, 0:6], lhsT=ht(i, k), rhs=WFF[:, k, 122:128],
                                                           start=(k == 0), stop=(k == 7)),
                      reads=["WFF", f"HT{i}"], writes=[f"PA{pa}"])
                A("dve", lambda e, pa=pa: e.tensor_tensor(out=FF[:, i, :], in0=PA[pa][:, 0:6], in1=FBF[:], op=ALU.add),
                  reads=[f"PA{pa}", "FBF"], writes=["FF"])
            proj_tm(bv, 384, ev_fv)
            if lvl < 3.1:
                S.barrier()
                continue
            FFf = FF[:, :, :].rearrange("p i h -> p (i h)")
            A("act", lambda e: e.activation(out=FFf, in_=FFf, func=AF.Exp, scale=-1.0), reads=["FF"], writes=["FF"])
            A("act", lambda e: e.activation(out=FFf, in_=FFf, func=AF.Ln, bias=1.0, scale=1.0),
              reads=["FF"], writes=["FF"])
            pa = nxt("pa", 2)
            A("pe", lambda e, pa=pa: e.matmul(PA[pa][:, 0:96], lhsT=Utri[:], rhs=FFf, start=True, stop=True),
              reads=["Utri", "FF"], writes=[f"PA{pa}"])
            pa2 = nxt("pa", 2)
            A("pe", lambda e, pa2=pa2: e.matmul(PA[pa2][:, 0:96], lhsT=onesf[:], rhs=FFf, start=True, stop=True),
              reads=["onesf", "FF"], writes=[f"PA{pa2}"])
            A("dve", lambda e, pa2=pa2: e.tensor_copy(out=TOT[:, :, :].rearrange("p i h -> p (i h)"),
                                                      in_=PA[pa2][:, 0:96]), reads=[f"PA{pa2}"], writes=["TOT"])
            for i in range(NT):
                A("dve", lambda e, i=i: e.tensor_tensor(out=OFFS[:, i + 1, :], in0=OFFS[:, i, :], in1=TOT[:, i, :],
                                                        op=ALU.add), reads=["TOT", "OFFS"], writes=["OFFS"])
            A("dve", lambda e, pa=pa: e.tensor_tensor(
                out=GG[:, :, :].rearrange("p i h -> p (i h)"), in0=PA[pa][:, 0:96],
                in1=OFFS[:, 0:NT, :].rearrange("p i h -> p (i h)"), op=ALU.add),
              reads=[f"PA{pa}", "OFFS"], writes=["GG"])
            for i in range(NT):
                A("dve", lambda e, i=i: e.tensor_tensor(
                    out=BIASALL[:, i * 96:(i + 1) * 96].rearrange("p (j h) -> p j h", j=16),
                    in0=GG[:, :, :],
                    in1=OFFS[:, i + 1:i + 2, :].to_broadcast([128, 16, 6]), op=ALU.subtract),
                  reads=["GG", "OFFS"], writes=["BIASALL"])
            if lvl < 3.2:
                S.barrier()
                continue
            attention("fox")
            out_proj(l, 640, 3)

            if lvl < 4:
                S.barrier()
                continue
            S.barrier()
            qk_proj(C_DQ, C_DK)
            gate_proj(C_DG)
            bi = load_w(l, [(C_IQ, 256, 0)])
            A("pool", lambda e, l=l: e.dma_start(
                out=WFF[:, :, :],
                in_=win_d[l].rearrange("(k p) c -> p k c", p=128)[:, :, C_IK:C_IK + 128]),
              writes=["WFF"], dma=True, sem_key="WFF")
            for rr in range(3):
                A("dve", lambda e, rr=rr, bi=bi: e.tensor_copy(out=WB[bi][:, :, 256 + 32 * rr:288 + 32 * rr],
                                                               in_=WFF[:, :, 0:32]),
                  reads=["WFF"], writes=[f"WB{bi}"])
            for ti, (c0, m) in enumerate(((0, 96), (96, 96), (192, 64))):
                def ev(c, ps_ap, key, ti=ti, m=m):
                    copy_alt(IQT[0:m, ti * 2048 + c * 512:ti * 2048 + (c + 1) * 512], ps_ap[0:m, :], [key], ["IQT"])
                proj_cm(bi, c0, m, ev)

            def ev_ik(c, ps_ap, key):
                copy_alt(IKT[0:96, c * 512:(c + 1) * 512], ps_ap[0:96, :], [key], ["IKT"])
            proj_cm(bi, 256, 96, ev_ik)
            bv = load_w(l, [(C_DV, 384, 0)])

            def ev_dv(i, ps_ap, key):
                A("dve", lambda e: e.tensor_copy(out=VT[:, i, :], in_=ps_ap[:, 0:384]), reads=[key], writes=["VT"])
                pa = nxt("pa", 2)
                for k in range(8):
                    A("pe", lambda e, pa=pa, k=k: e.matmul(PA[pa][:, 0:8], lhsT=ht(i, k), rhs=WFF[:, k, 32:40],
                                                           start=(k == 0), stop=(k == 7)),
                      reads=["WFF", f"HT{i}"], writes=[f"PA{pa}"])
                A("dve", lambda e, pa=pa: e.tensor_scalar(out=IW[:, i, :], in0=PA[pa][:, 0:8], scalar1=8.0 ** -0.5,
                                                          scalar2=None, op0=ALU.mult), reads=[f"PA{pa}"], writes=["IW"])
            proj_tm(bv, 384, ev_dv)
            S.barrier()
            if lvl < 4.1:
                continue
            attention("dsa")
            out_proj(l, 256, 3)
            S.barrier()

        A("sp", lambda e: e.dma_start(out=GB, in_=fg_d[None, :].to_broadcast([128, D])),
          writes=["GB"], dma=True, sem_key="GB")
        rms_stats()
        for i in range(NT):
            yb = i % 2
            A("dve", lambda e, i=i, yb=yb: e.scalar_tensor_tensor(
                out=YST[yb], in0=X[:, i, :], scalar=RSTD[:, i:i + 1], in1=GB, op0=ALU.mult, op1=ALU.mult),
              reads=[f"X{i}", "RSTD", "GB"], writes=[f"YST{yb}"])
            A("sp", lambda e, s=s, i=i, yb=yb: e.dma_start(out=y_d[s, i * 128:(i + 1) * 128, :], in_=YST[yb]),
              reads=[f"YST{yb}"], writes=[], dma=True, sem_key=f"YST{yb}")
        S.barrier()
    S.barrier()
    S.emit(nc)
    es.close()
    return nc


_NC_CACHE = {}


def kernel(x, w_in, norm_g, pool_w, pool_scale, fox_bf, w_out, final_g):
    n_cores = 8
    x = np.ascontiguousarray(np.asarray(x, dtype=np.float32))
    per = x.shape[0] // n_cores
    if "nc" not in _NC_CACHE:
        _NC_CACHE["nc"] = build(n_seq=per, n_layers=4)
    nc = _NC_CACHE["nc"]
    shared = {
        "w_in": np.ascontiguousarray(np.asarray(w_in, dtype=np.float32)),
        "norm_g": np.ascontiguousarray(np.asarray(norm_g, dtype=np.float32)),
        "pool_w": np.ascontiguousarray(np.asarray(pool_w, dtype=np.float32)),
        "pool_scale": np.ascontiguousarray(np.asarray(pool_scale, dtype=np.float32)),
        "fox_bf": np.ascontiguousarray(np.asarray(fox_bf, dtype=np.float32)),
        "w_out": np.ascontiguousarray(np.asarray(w_out, dtype=np.float32)),
        "final_g": np.ascontiguousarray(np.asarray(final_g, dtype=np.float32)),
    }
    in_maps = []
    for c in range(n_cores):
        m = dict(shared)
        m["x"] = np.ascontiguousarray(x[c * per:(c + 1) * per])
        in_maps.append(m)
    res = run_bass_kernel_spmd(nc, in_maps, core_ids=list(range(n_cores)))
    return np.concatenate([np.asarray(r["y"]) for r in res.results], axis=0).astype(np.float32)
```

```python
import contextlib
import numpy as np
import concourse.bass as bass
import concourse.mybir as mybir
from concourse.bass_utils import run_bass_kernel_spmd

F32 = mybir.dt.float32
BF16 = mybir.dt.bfloat16
ALU = mybir.AluOpType
AF = mybir.ActivationFunctionType
AX = mybir.AxisListType

L_SEQ = 2048
D = 1024
NT = 16
IN_COLS = 3886
NEG = -30000.0
NSTEP = 14
TOPK = 256

C_PV, C_PG, C_DQ, C_DK, C_DV, C_DG = 0, 256, 512, 896, 1280, 1664
C_IQ, C_IK, C_IW, C_FQ, C_FK, C_FV, C_FG, C_FF = 2048, 2304, 2336, 2344, 2728, 3112, 3496, 3880

SEM_SPAN = 3000
SAME_ENG_WINDOW = 5


class Op:
    __slots__ = ("id", "eng", "fn", "deps", "dma", "sem_key", "dcount", "gsig",
                 "eidx", "signals")

    def __init__(self, id, eng, fn, dma, sem_key):
        self.id = id
        self.eng = eng
        self.fn = fn
        self.deps = set()
        self.dma = dma
        self.sem_key = sem_key
        self.dcount = 0
        self.gsig = 0
        self.eidx = 0
        self.signals = False


class Sched:
    ENGS = ("pe", "act", "dve", "pool", "sp")

    def __init__(self):
        self.ops = []
        self.last_writer = {}
        self.readers = {}
        self.eng_count = {e: 0 for e in self.ENGS}
        self.dma_counts = {}
        self.last_dma = {}

    def add(self, eng, fn, reads=(), writes=(), dma=False, sem_key=None, extra_deps=()):
        op = Op(len(self.ops), eng, fn, dma, sem_key)
        if dma:
            assert sem_key is not None
            self.dma_counts[sem_key] = self.dma_counts.get(sem_key, 0) + 1
            op.dcount = self.dma_counts[sem_key]
            self.last_dma[sem_key] = op.id
        op.eidx = self.eng_count[eng]
        self.eng_count[eng] += 1
        for k in list(reads) + list(writes):
            w = self.last_writer.get(k)
            if w is not None:
                op.deps.add(w)
        for k in writes:
            for r in self.readers.get(k, ()):
                op.deps.add(r)
        for k in reads:
            self.readers.setdefault(k, []).append(op.id)
        for k in writes:
            self.last_writer[k] = op.id
            self.readers[k] = []
        for d in extra_deps:
            op.deps.add(d)
        op.deps.discard(op.id)
        self.ops.append(op)
        return op

    def barrier(self):
        last = {}
        for op in self.ops:
            if not op.dma and op.fn is not None:
                last[op.eng] = op.id
        deps = list(last.values()) + list(self.last_dma.values())
        for e in self.ENGS:
            self.add(e, None, extra_deps=deps)

    def emit(self, nc):
        ops = self.ops
        for op in ops:
            for d in op.deps:
                p = ops[d]
                if p.dma or p.fn is None:
                    continue
                if p.eng == op.eng:
                    if op.eng == "pe":
                        continue
                    if op.eidx - p.eidx > SAME_ENG_WINDOW:
                        continue
                p.signals = True
        gs = {e: 0 for e in self.ENGS}
        for op in ops:
            if op.signals and not op.dma:
                gs[op.eng] += 1
                op.gsig = gs[op.eng]
        stack = []
        sems = {}
        for e in self.ENGS:
            for i in range((gs[e] + SEM_SPAN - 1) // SEM_SPAN):
                cm = nc.semaphore(f"s_{e}_{i}")
                sems[(e, i)] = cm.__enter__()
                stack.append(cm)
        dsems = {}
        for k in self.dma_counts:
            cm = nc.semaphore(f"d_{len(dsems)}")
            dsems[k] = cm.__enter__()
            stack.append(cm)
        self.n_sems = len(stack)
        per_eng = {e: [op for op in ops if op.eng == e] for e in self.ENGS}

        def run_engine(e, eng):
            waited = {x: 0 for x in self.ENGS}
            dwaited = {}
            for op in per_eng[e]:
                need = {}
                dneed = {}
                for d in op.deps:
                    p = ops[d]
                    if p.fn is None:
                        continue
                    if p.dma:
                        if dwaited.get(p.sem_key, 0) < p.dcount:
                            dneed[p.sem_key] = max(dneed.get(p.sem_key, 0), p.dcount)
                        continue
                    if p.eng == e:
                        if e == "pe" or op.eidx - p.eidx > SAME_ENG_WINDOW:
                            continue
                    if p.gsig > waited[p.eng]:
                        need[p.eng] = max(need.get(p.eng, 0), p.gsig)
                for pe_, g in need.items():
                    si = (g - 1) // SEM_SPAN
                    eng.wait_ge(sems[(pe_, si)], g - si * SEM_SPAN)
                    waited[pe_] = g
                for k, c in dneed.items():
                    eng.wait_ge(dsems[k], 16 * c)
                    dwaited[k] = c
                if op.fn is None:
                    continue
                ins = op.fn(eng)
                if op.dma:
                    ins.then_inc(dsems[op.sem_key], 16)
                elif op.signals:
                    si = (op.gsig - 1) // SEM_SPAN
                    ins.then_inc(sems[(op.eng, si)], 1)

        with nc.Block() as block:
            @block.tensor
            def _(eng):
                run_engine("pe", eng)

            @block.scalar
            def _(eng):
                run_engine("act", eng)

            @block.vector
            def _(eng):
                run_engine("dve", eng)

            @block.gpsimd
            def _(eng):
                run_engine("pool", eng)

            @block.sync
            def _(eng):
                run_engine("sp", eng)
        for cm in reversed(stack):
            cm.__exit__(None, None, None)


def build(n_seq=2, n_layers=4, lvl=9):
    nc = bass.Bass("TRN2", target_bir_lowering=False)
    x_d = nc.dram_tensor("x", [n_seq, L_SEQ, D], F32, kind="ExternalInput").ap()
    win_d = nc.dram_tensor("w_in", [4, D, IN_COLS], F32, kind="ExternalInput").ap()
    ng_d = nc.dram_tensor("norm_g", [4, D], F32, kind="ExternalInput").ap()
    pw_d = nc.dram_tensor("pool_w", [4, 4, 64, 64], F32, kind="ExternalInput").ap()
    psc_d = nc.dram_tensor("pool_scale", [4, 256], F32, kind="ExternalInput").ap()
    fbf_d = nc.dram_tensor("fox_bf", [4, 6], F32, kind="ExternalInput").ap()
    wout_d = nc.dram_tensor("w_out", [4, D, D], F32, kind="ExternalInput").ap()
    fg_d = nc.dram_tensor("final_g", [D], F32, kind="ExternalInput").ap()
    y_d = nc.dram_tensor("y", [n_seq, L_SEQ, D], F32, kind="ExternalOutput").ap()

    S = Sched()
    A = S.add
    es = contextlib.ExitStack()

    def sb(name, shape, dt):
        return es.enter_context(nc.sbuf_tensor(name, shape, dt))

    def pst(name, shape, dt):
        return es.enter_context(nc.psum_tensor(name, shape, dt))

    X = sb("X", [128, NT, D], F32)
    HT = sb("HT", [128, NT * 8 * 128], BF16)
    MIXT = sb("MIXT", [128, 3, L_SEQ], BF16)
    QT = sb("QT", [128, 3, L_SEQ], BF16)
    KT = sb("KT", [128, 3, L_SEQ], BF16)
    VT = sb("VT", [128, NT, 384], BF16)
    WB = [sb("WB0", [128, 8, 392], BF16), sb("WB1", [128, 8, 392], BF16)]
    WOB = sb("WOB", [128, 3, D], BF16)
    WFF = sb("WFF", [128, 8, 128], BF16)
    SCR = sb("SCR", [128, 24576], mybir.dt.uint8)
    PT = [sb(f"PT{i}", [128, 512], BF16) for i in range(4)]
    RB = [sb(f"RB{i}", [128, 512], BF16) for i in range(3)]
    RD = sb("RD", [128, 512], F32)
    TMPF = sb("TMPF", [128, 512], F32)
    ident = sb("ident", [128, 128], BF16)
    tri = sb("tri", [128, 128], BF16)
    Utri = sb("Utri", [128, 128], F32)
    onesf = sb("onesf", [128, 128], F32)
    ones64 = sb("ones64", [128, 64], BF16)
    dmaskf = sb("dmaskf", [128, 128], F32)
    dmaskb = sb("dmaskb", [128, 128], BF16)
    halves = sb("halves", [128, NSTEP + 1], F32)
    invw = sb("invw", [128, 2], F32)
    invcnt = sb("invcnt", [128, 2, 16], F32)
    SS = sb("SS", [128, NT], F32)
    RSTD = sb("RSTD", [128, NT], F32)
    PSCL = sb("PSCL", [128, 2], F32)
    POOLW = sb("POOLW", [128, 2, 128], BF16)
    FBF = sb("FBF", [128, 6], F32)
    IW = sb("IW", [128, NT, 8], F32)
    FF = sb("FF", [128, NT, 6], F32)
    TOT = sb("TOT", [128, NT, 6], F32)
    OFFS = sb("OFFS", [128, NT + 1, 6], F32)
    GG = sb("GG", [128, NT, 6], F32)
    DW = sb("DW", [128, 8, 128], BF16)
    BS = sb("BS", [128, 8 + NSTEP + 1], F32)

    def view(t, dt, byte_off, n_elem):
        ap = t[:, :]
        apb = ap.bitcast(dt)
        esz = mybir.dt.size(dt)
        tsz = mybir.dt.size(t.dtype)
        e0 = byte_off // esz
        return apb[:, e0:e0 + n_elem]

    HB = [view(SCR, BF16, 16384, 1024), view(SCR, BF16, 18432, 1024)]
    GB = view(SCR, F32, 20480, 1024)
    JUNKN = view(SCR, BF16, 8192, 1024)
    VBUF = view(SCR, F32, 0, 528)
    T1 = view(SCR, F32, 2112, 528)
    T2 = view(SCR, F32, 4224, 528)
    PTP = view(SCR, BF16, 8192, 4096)
    BIASALL = view(SCR, F32, 16384, 16 * 16 * 6)
    IQT = view(SCR, BF16, 0, 3 * 2048)
    IKT = view(SCR, BF16, 12288, 2048)
    SCORE = view(SCR, F32, 16384, 2048)
    YST = [view(HT, F32, 0, 1024), view(HT, F32, 4096, 1024)]

    def ht(i, k):
        o = (i * 8 + k) * 128
        return HT[:, o:o + 128]

    HT4 = HT[:, :].rearrange("p (i k t) -> p i k t", i=NT, k=8)

    PTR = pst("PTR", [128, 1024], BF16)
    PA = [pst("PA0", [128, 512], F32), pst("PA1", [128, 512], F32)]
    PS = [pst("PS0", [128, 512], F32), pst("PS1", [128, 512], F32)]
    PO = [pst("PO0", [128, 512], F32), pst("PO1", [128, 512], F32)]
    PD = [pst("PD0", [128, 512], F32)]
    rot = {"pa": 0, "ps": 0, "po": 0, "pt": 0, "rb": 0, "wb": 0, "hb": 0}

    def nxt(name, n):
        v = rot[name]
        rot[name] = (v + 1) % n
        return v

    A("pool", lambda e: e.memset(TMPF[:, 0:128], 0.0), writes=["TMPF"])
    A("pool", lambda e: e.affine_select(out=TMPF[:, 0:128], in_=TMPF[:, 0:128], pattern=[[-1, 128]],
                                        compare_op=ALU.not_equal, fill=1.0, base=0,
                                        channel_multiplier=1), reads=["TMPF"], writes=["TMPF"])
    A("dve", lambda e: e.tensor_copy(out=ident[:], in_=TMPF[:, 0:128]), reads=["TMPF"], writes=["ident"])
    A("pool", lambda e: e.memset(TMPF[:, 0:128], 0.0), writes=["TMPF"])
    A("pool", lambda e: e.affine_select(out=TMPF[:, 0:128], in_=TMPF[:, 0:128], pattern=[[1, 128]],
                                        compare_op=ALU.is_ge, fill=NEG, base=0,
                                        channel_multiplier=-1), reads=["TMPF"], writes=["TMPF"])
    A("dve", lambda e: e.tensor_copy(out=tri[:], in_=TMPF[:, 0:128]), reads=["TMPF"], writes=["tri"])
    A("pool", lambda e: e.memset(Utri[:], 1.0), writes=["Utri"])
    A("pool", lambda e: e.affine_select(out=Utri[:], in_=Utri[:], pattern=[[1, 128]],
                                        compare_op=ALU.is_ge, fill=0.0, base=0,
                                        channel_multiplier=-1), reads=["Utri"], writes=["Utri"])
    A("pool", lambda e: e.memset(onesf[:], 1.0), writes=["onesf"])
    A("pool", lambda e: e.memset(ones64[:], 1.0), writes=["ones64"])
    A("pool", lambda e: e.memset(dmaskf[:], 0.0), writes=["dmaskf"])
    A("pool", lambda e: e.memset(dmaskf[0:64, 64:128], -1.0e30), reads=["dmaskf"], writes=["dmaskf"])
    A("pool", lambda e: e.memset(dmaskb[:], 0.0), writes=["dmaskb"])
    A("pool", lambda e: e.memset(dmaskb[0:64, 64:128], NEG), reads=["dmaskb"], writes=["dmaskb"])
    for k in range(NSTEP + 1):
        A("pool", lambda e, k=k: e.memset(halves[:, k:k + 1], 0.5 ** (k + 1)), writes=["halves"])
    for j, (wa, wb_) in enumerate(((2, 4), (8, 16))):
        A("pool", lambda e, j=j, wa=wa: e.memset(invw[0:64, j:j + 1], 1.0 / wa), writes=["invw"])
        A("pool", lambda e, j=j, wb_=wb_: e.memset(invw[64:128, j:j + 1], 1.0 / wb_), writes=["invw"])
        for t in range(16):
            A("pool", lambda e, j=j, t=t, wa=wa: e.memset(invcnt[0:64, j, t:t + 1], 1.0 / min(t + 1, wa)),
              writes=["invcnt"])
            A("pool", lambda e, j=j, t=t, wb_=wb_: e.memset(invcnt[64:128, j, t:t + 1], 1.0 / min(t + 1, wb_)),
              writes=["invcnt"])
    A("pool", lambda e: e.memset(OFFS[:, 0, :], 0.0), writes=["OFFS"])

    def load_w(l, pieces):
        b = nxt("wb", 2)
        for (c0, n, s0) in pieces:
            A("pool", lambda e, b=b, c0=c0, n=n, s0=s0: e.dma_start(
                out=WB[b][:, :, s0:s0 + n],
                in_=win_d[l].rearrange("(k p) c -> p k c", p=128)[:, :, c0:c0 + n]),
              writes=[f"WB{b}"], dma=True, sem_key=f"WB{b}")
        return b

    def proj_cm(b, col0, M, evac):
        for c in range(4):
            pa = nxt("pa", 2)
            for k in range(8):
                A("pe", lambda e, pa=pa, k=k, c=c: e.matmul(
                    PA[pa][0:M, :], lhsT=WB[b][:, k, col0:col0 + M],
                    rhs=HT4[:, 4 * c:4 * c + 4, k, :], start=(k == 0), stop=(k == 7)),
                  reads=[f"WB{b}"] + [f"HT{i}" for i in range(4 * c, 4 * c + 4)], writes=[f"PA{pa}"])
            evac(c, PA[pa], f"PA{pa}")

    def proj_tm(b, N, evac):
        for i in range(NT):
            pa = nxt("pa", 2)
            for k in range(8):
                A("pe", lambda e, pa=pa, k=k, i=i: e.matmul(
                    PA[pa][:, 0:N], lhsT=ht(i, k), rhs=WB[b][:, k, 0:N],
                    start=(k == 0), stop=(k == 7)),
                  reads=[f"WB{b}", f"HT{i}"], writes=[f"PA{pa}"])
            evac(i, PA[pa], f"PA{pa}")

    cnt = {"alt": 0}

    def copy_alt(out, in_, reads, writes, scale=None):
        cnt["alt"] += 1
        if cnt["alt"] % 2 == 0:
            if scale is None:
                A("act", lambda e: e.activation(out=out, in_=in_, func=AF.Copy), reads=reads, writes=writes)
            else:
                A("act", lambda e: e.activation(out=out, in_=in_, func=AF.Copy, scale=scale),
                  reads=reads, writes=writes)
        else:
            if scale is None:
                A("dve", lambda e: e.tensor_copy(out=out, in_=in_), reads=reads, writes=writes)
            else:
                A("dve", lambda e: e.tensor_scalar(out=out, in0=in_, scalar1=scale, scalar2=None,
                                                   op0=ALU.mult), reads=reads, writes=writes)

    def out_proj(l, row0, nch):
        A("pool", lambda e: e.dma_start(
            out=WOB[:, 0:nch, :],
            in_=wout_d[l, row0:row0 + nch * 128, :].rearrange("(q p) c -> p q c", p=128)),
          writes=["WOB"], dma=True, sem_key="WOB")
        for i in range(NT):
            for n in range(2):
                pa = nxt("pa", 2)
                for q in range(nch):
                    A("pe", lambda e, pa=pa, q=q, i=i, n=n: e.matmul(
                        PA[pa][:, :], lhsT=MIXT[:, q, i * 128:(i + 1) * 128],
                        rhs=WOB[:, q, n * 512:(n + 1) * 512], start=(q == 0), stop=(q == nch - 1)),
                      reads=["WOB", f"MIXT{q}"], writes=[f"PA{pa}"])
                A("dve", lambda e, pa=pa, i=i, n=n: e.tensor_tensor(
                    out=X[:, i, n * 512:(n + 1) * 512], in0=X[:, i, n * 512:(n + 1) * 512],
                    in1=PA[pa][:, :], op=ALU.add),
                  reads=[f"PA{pa}", f"X{i}"], writes=[f"X{i}"])

    def rms_stats():
        for i in range(NT):
            A("act", lambda e, i=i: e.activation(out=JUNKN, in_=X[:, i, :], func=AF.Square,
                                                 accum_out=SS[:, i:i + 1]),
              reads=[f"X{i}"], writes=["JUNKN", "SS"])
        A("act", lambda e: e.activation(out=RSTD[:], in_=SS[:], func=AF.Sqrt, bias=EPS_T[:, 0:1], scale=1.0 / D),
          reads=["SS"], writes=["RSTD"])
        A("dve", lambda e: e.reciprocal(out=RSTD[:], in_=RSTD[:]), reads=["RSTD"], writes=["RSTD"])

    EPS_T = sb("EPS_T", [128, 1], F32)
    A("pool", lambda e: e.memset(EPS_T[:], 1e-6), writes=["EPS_T"])
    C255 = sb("C255", [128, 1], F32)
    A("pool", lambda e: e.memset(C255[:], TOPK - 0.5), writes=["C255"])
    CNEG = sb("CNEG", [128, 1], F32)
    A("pool", lambda e: e.memset(CNEG[:], NEG), writes=["CNEG"])


    def mb_view(c, q):
        base = (c % 2) * 16384
        o = 0
        for qq in range(q):
            o += (4 * c + qq + 1) * 128
        n = (4 * c + q + 1) * 128
        return view(HT, BF16, base + 2 * o, n)

    PSC = PTR[:, :].bitcast(F32)

    def attention(kind):
        if kind == "dsa":
            for q in range(4):
                dsa_mask_tile(0, q)
        for c in range(4):
            attn_chunk(kind, c)

    def attn_chunk(kind, c):
        nj = 4 * c + 4
        t0 = 4 * c * 128
        po_of = {}
        pd = 0
        pre = {0: (0, 1), 1: (2,), 2: (3,)}

        def qk(p, j, hh):
            h = 2 * p + hh
            r0 = 64 * hh
            i_lo = max(4 * c, j)
            off = (i_lo - 4 * c) * 128
            ps = nxt("ps", 2)
            psk = f"PS{ps}"
            pt = nxt("pt", 4)
            kslc = KT[r0:r0 + 64, p, j * 128:(j + 1) * 128]
            if kind == "fox":
                if j >= 4 * c:
                    A("pe", lambda e: e.matmul(PS[ps][:, off:off + 128], lhsT=ident[:], rhs=tri[:],
                                               start=True, stop=False),
                      reads=["ident", "tri"], writes=[psk])
                    A("pe", lambda e: e.matmul(PS[ps][:, off:off + 128], lhsT=kslc,
                                               rhs=QT[r0:r0 + 64, p, t0 + off:t0 + off + 128],
                                               start=False, stop=True),
                      reads=[f"KT{p}", f"QT{p}"], writes=[psk])
                    if off + 128 < 512:
                        A("pe", lambda e: e.matmul(PS[ps][:, off + 128:512], lhsT=kslc,
                                                   rhs=QT[r0:r0 + 64, p, t0 + off + 128:t0 + 512],
                                                   start=True, stop=True),
                          reads=[f"KT{p}", f"QT{p}"], writes=[psk])
                else:
                    A("pe", lambda e: e.matmul(PS[ps][:, 0:512], lhsT=kslc, rhs=QT[r0:r0 + 64, p, t0:t0 + 512],
                                               start=True, stop=True),
                      reads=[f"KT{p}", f"QT{p}"], writes=[psk])
                i = i_lo
                while i < 4 * c + 4:
                    wd = 2 if i % 2 == 0 else 1
                    co = (i - 4 * c) * 128
                    bcol = ((i | 1) * 16 + j) * 6 + h
                    A("act", lambda e, co=co, wd=wd, bcol=bcol: e.activation(
                        out=PT[pt][:, co:co + 128 * wd], in_=PS[ps][:, co:co + 128 * wd], func=AF.Exp,
                        bias=BIASALL[:, bcol:bcol + 1], scale=1.0),
                      reads=[psk, "BIASALL"], writes=[f"PT{pt}"])
                    i += wd
            else:
                for i in range(i_lo, 4 * c + 4):
                    co = (i - 4 * c) * 128
                    q = i - 4 * c
                    mbv = mb_view(c, q)
                    A("pe", lambda e, co=co, mbv=mbv: e.matmul(
                        PS[ps][:, co:co + 128], lhsT=mbv[:, j * 128:(j + 1) * 128], rhs=ident[:],
                        start=True, stop=False),
                      reads=[f"MB{c % 2}_{q}", "ident"], writes=[psk])
                    A("pe", lambda e, co=co: e.matmul(
                        PS[ps][:, co:co + 128], lhsT=kslc,
                        rhs=QT[r0:r0 + 64, p, t0 + co:t0 + co + 128], start=False, stop=True),
                      reads=[f"KT{p}", f"QT{p}"], writes=[psk])
                A("act", lambda e: e.activation(out=PT[pt][:, off:512], in_=PS[ps][:, off:512], func=AF.Exp),
                  reads=[psk], writes=[f"PT{pt}"])
            return (p, j, hh, pt, off)

        def pv(blk):
            p, j, hh, pt, off = blk
            h = 2 * p + hh
            r0 = 64 * hh
            if p not in po_of:
                po_of[p] = nxt("po", 2)
            po = po_of[p]
            A("pe", lambda e: e.matmul(
                PO[po][r0:r0 + 64, off:512], lhsT=VT[:, j, h * 64:(h + 1) * 64], rhs=PT[pt][:, off:512],
                start=(j == 0), stop=(j == nj - 1), tile_position=(0, r0)),
              reads=[f"PT{pt}", "VT"], writes=[f"PO{po}"])
            A("pe", lambda e: e.matmul(
                PD[pd][r0:r0 + 64, off:512], lhsT=ones64[:], rhs=PT[pt][:, off:512],
                start=(j == 0), stop=(j == nj - 1), tile_position=(0, r0)),
              reads=[f"PT{pt}", "ones64"], writes=[f"PD{pd}"])
            if j == nj - 1 and hh == 1:
                csl = slice(4 * c * 128, (4 * c + 4) * 128)
                A("dve", lambda e: e.reciprocal(out=RD[:], in_=PD[pd][:, :]), reads=[f"PD{pd}"], writes=["RD"])
                A("dve", lambda e: e.tensor_tensor(out=TMPF[:], in0=PO[po][:, :], in1=RD[:], op=ALU.mult),
                  reads=[f"PO{po}", "RD"], writes=["TMPF"])
                A("dve", lambda e: e.tensor_tensor(out=MIXT[:, p, csl], in0=TMPF[:], in1=MIXT[:, p, csl],
                                                   op=ALU.mult),
                  reads=["TMPF", f"MIXT{p}"], writes=[f"MIXT{p}"])

        pend = None
        for p in range(3):
            if kind == "dsa" and c < 3:
                for q in pre[p]:
                    dsa_mask_tile(c + 1, q)
            for j in range(nj):
                for hh in range(2):
                    cur = qk(p, j, hh)
                    if pend is not None:
                        pv(pend)
                    pend = cur
        pv(pend)

    def dsa_mask_tile(c, q):
        i = 4 * c + q
        n = (i + 1) * 128
        mk = f"MB{c % 2}_{q}"
        mbv = mb_view(c, q)
        if i < 2:
            if i == 1:
                A("pool", lambda e: e.memset(mbv[:, 0:128], 0.0), writes=[mk])
            A("pool", lambda e: e.tensor_copy(out=mbv[:, n - 128:n], in_=dmaskb[:]),
              reads=["dmaskb"], writes=[mk])
            return
        A("dve", lambda e: e.tensor_tensor(
            out=DW[:, :, :], in0=ident[:].unsqueeze(1).to_broadcast([128, 8, 128]),
            in1=IW[:, i, :].unsqueeze(2).to_broadcast([128, 8, 128]), op=ALU.mult),
          reads=["ident", "IW"], writes=["DW"])
        nsc = (n + 511) // 512
        for sc in range(nsc):
            w = min(512, n - sc * 512)
            for h in range(8):
                ti, r = h // 3, (h % 3) * 32
                pa = nxt("pa", 2)
                rb = nxt("rb", 3)
                A("pe", lambda e, pa=pa, ti=ti, r=r, w=w, sc=sc: e.matmul(
                    PA[pa][:, 0:w], lhsT=IQT[r:r + 32, ti * 2048 + i * 128:ti * 2048 + (i + 1) * 128],
                    rhs=IKT[r:r + 32, sc * 512:sc * 512 + w], start=True, stop=True),
                  reads=["IQT", "IKT"], writes=[f"PA{pa}"])
                A("act", lambda e, pa=pa, rb=rb, w=w: e.activation(out=RB[rb][:, 0:w], in_=PA[pa][:, 0:w],
                                                                   func=AF.Relu),
                  reads=[f"PA{pa}"], writes=[f"RB{rb}"])
                A("pe", lambda e, rb=rb, h=h, w=w: e.matmul(
                    PSC[:, 0:w], lhsT=DW[:, h, :], rhs=RB[rb][:, 0:w], start=(h == 0), stop=(h == 7)),
                  reads=["DW", f"RB{rb}"], writes=["PTR"])
            s0 = sc * 512
            if sc == nsc - 1:
                if w > 128:
                    A("dve", lambda e, s0=s0, w=w: e.tensor_copy(
                        out=SCORE[:, s0:s0 + w - 128], in_=PSC[:, 0:w - 128]),
                      reads=["PTR"], writes=["SCORE"])
                A("dve", lambda e, s0=s0, w=w: e.tensor_tensor(
                    out=SCORE[:, s0 + w - 128:s0 + w], in0=PSC[:, w - 128:w], in1=dmaskf[:], op=ALU.add),
                  reads=["PTR", "dmaskf"], writes=["SCORE"])
            else:
                A("dve", lambda e, s0=s0, w=w: e.tensor_copy(out=SCORE[:, s0:s0 + w], in_=PSC[:, 0:w]),
                  reads=["PTR"], writes=["SCORE"])
        eng = "dve"
        mx, mn, rg, mid, cn, tp = (BS[:, 0:1], BS[:, 1:2], BS[:, 2:3], BS[:, 3:4], BS[:, 4:5], BS[:, 5:6])
        DL = BS[:, 8:8 + NSTEP + 1]
        A(eng, lambda e: e.tensor_reduce(out=mx, in_=SCORE[:, 0:n], axis=AX.X, op=ALU.max),
          reads=["SCORE"], writes=["BSmx"])
        A(eng, lambda e: e.tensor_reduce(out=mn, in_=SCORE[:, 0:n - 64], axis=AX.X, op=ALU.min),
          reads=["SCORE"], writes=["BSmn"])
        A(eng, lambda e: e.tensor_tensor(out=rg, in0=mx, in1=mn, op=ALU.subtract),
          reads=["BSmx", "BSmn"], writes=["BSrg"])
        A(eng, lambda e: e.tensor_scalar(out=DL, in0=halves[:], scalar1=rg, scalar2=None, op0=ALU.mult),
          reads=["BSrg", "halves"], writes=["BSdl"])
        A(eng, lambda e: e.tensor_tensor(out=mid, in0=mn, in1=BS[:, 8:9], op=ALU.add),
          reads=["BSmn", "BSdl"], writes=["BSmid"])
        for k in range(NSTEP):
            A(eng, lambda e: e.tensor_scalar(out=mbv[:, 0:n], in0=SCORE[:, 0:n], scalar1=mid,
                                             scalar2=None, op0=ALU.is_ge, op1=ALU.add, accum_out=cn),
              reads=["SCORE", "BSmid"], writes=[mk, "BScn"])
            A(eng, lambda e, k=k: e.tensor_scalar(out=tp, in0=cn, scalar1=C255[:, 0:1],
                                                  scalar2=BS[:, 8 + k:9 + k], op0=ALU.is_ge, op1=ALU.mult),
              reads=["BScn", "BSdl"], writes=["BStp"])
            A(eng, lambda e, k=k: e.scalar_tensor_tensor(out=mid, in0=tp, scalar=BS[:, 9 + k:10 + k], in1=mid,
                                                         op0=ALU.subtract, op1=ALU.add),
              reads=["BStp", "BSdl", "BSmid"], writes=["BSmid"])
        A(eng, lambda e: e.tensor_tensor(out=tp, in0=mid, in1=BS[:, 8 + NSTEP:9 + NSTEP], op=ALU.subtract),
          reads=["BSmid", "BSdl"], writes=["BStp"])
        A(eng, lambda e: e.tensor_scalar(out=mbv[:, 0:n], in0=SCORE[:, 0:n], scalar1=tp,
                                         scalar2=CNEG[:, 0:1], op0=ALU.is_lt, op1=ALU.mult),
          reads=["SCORE", "BStp"], writes=[mk])

    for s in range(n_seq):
        for qd in range(4):
            A("sp", lambda e, s=s, qd=qd: e.dma_start(
                out=X[:, 4 * qd:4 * qd + 4, :],
                in_=x_d[s].rearrange("(i p) d -> p i d", p=128)[:, 4 * qd:4 * qd + 4, :]),
              writes=[f"X{i}" for i in range(4 * qd, 4 * qd + 4)], dma=True, sem_key=f"XL{qd}")
        for l in range(n_layers):
            if lvl < 1:
                break
            A("sp", lambda e, l=l: e.dma_start(out=GB, in_=ng_d[l:l + 1, :].to_broadcast([128, D])),
              writes=["GB"], dma=True, sem_key="GB")
            for j in range(2):
                A("sp", lambda e, l=l, j=j: e.dma_start(
                    out=PSCL[:, j:j + 1], in_=psc_d[l, j * 128:(j + 1) * 128].rearrange("(p o) -> p o", o=1)),
                  writes=["PSCL"], dma=True, sem_key=f"PSCL{j}")
            A("sp", lambda e, l=l: e.dma_start(out=FBF[:], in_=fbf_d[l:l + 1, :].to_broadcast([128, 6])),
              writes=["FBF"], dma=True, sem_key="FBF")
            A("pool", lambda e: e.memset(POOLW[:], 0.0), writes=["POOLW"])
            for g in range(4):
                j, r0 = g // 2, (g % 2) * 64
                A("pool", lambda e, l=l, g=g, j=j, r0=r0: e.dma_start(
                    out=POOLW[r0:r0 + 64, j, r0:r0 + 64], in_=pw_d[l, g]),
                  reads=[], writes=["POOLW"], dma=True, sem_key=f"POOLW{g}")
            rms_stats()
            for i in range(NT):
                hb = nxt("hb", 2)
                A("dve", lambda e, i=i, hb=hb: e.scalar_tensor_tensor(
                    out=HB[hb], in0=X[:, i, :], scalar=RSTD[:, i:i + 1], in1=GB, op0=ALU.mult, op1=ALU.mult),
                  reads=[f"X{i}", "RSTD", "GB"], writes=[f"HB{hb}"])
                for k in range(8):
                    A("pe", lambda e, hb=hb, k=k: e.transpose(out=PTR[:, k * 128:(k + 1) * 128],
                                                              in_=HB[hb][:, k * 128:(k + 1) * 128],
                                                              identity=ident[:]),
                      reads=[f"HB{hb}", "ident"], writes=["PTR"])
                copy_alt(HT[:, i * 1024:(i + 1) * 1024], PTR[:, :], ["PTR"], [f"HT{i}"])

            if lvl < 2:
                S.barrier()
                continue
            b = load_w(l, [(C_PG, 256, 0)])
            for j in range(2):
                def ev_g(c, ps_ap, key, j=j):
                    A("act", lambda e: e.activation(out=MIXT[:, j, c * 512:(c + 1) * 512], in_=ps_ap[:, :],
                                                    func=AF.Silu), reads=[key], writes=[f"MIXT{j}"])
                proj_cm(b, j * 128, 128, ev_g)
            b = load_w(l, [(C_PV, 256, 0)])
            for j in range(2):
                srcA = (T1, T2)
                def ev_v(c, ps_ap, key, j=j):
                    if c == 0:
                        A("pool", lambda e: e.memset(VBUF[:, 0:16], 0.0), writes=["VB_h"])
                    else:
                        A("pool", lambda e: e.tensor_copy(out=VBUF[:, 0:16], in_=VBUF[:, 512:528]),
                          reads=["VB_m"], writes=["VB_h"])
                    A("act", lambda e: e.activation(out=VBUF[:, 16:528], in_=ps_ap[:, :], func=AF.Copy),
                      reads=[key, "VB_h"], writes=["VB_m"])
                    rk = ["VB_h", "VB_m"]
                    A("pool", lambda e: e.tensor_tensor(out=T1[:, 1:528], in0=VBUF[:, 1:528], in1=VBUF[:, 0:527],
                                                        op=ALU.add), reads=rk, writes=["T1"])
                    if j == 0:
                        A("pool", lambda e: e.tensor_tensor(out=T2[64:128, 3:528], in0=T1[64:128, 3:528],
                                                            in1=T1[64:128, 1:526], op=ALU.add),
                          reads=["T1"], writes=["T2"])
                        fa, fb = T1, T2
                    else:
                        A("pool", lambda e: e.tensor_tensor(out=T2[:, 3:528], in0=T1[:, 3:528], in1=T1[:, 1:526],
                                                            op=ALU.add), reads=["T1"], writes=["T2"])
                        A("pool", lambda e: e.tensor_tensor(out=T1[:, 7:528], in0=T2[:, 7:528], in1=T2[:, 3:524],
                                                            op=ALU.add), reads=["T2"], writes=["T1"])
                        A("pool", lambda e: e.tensor_tensor(out=T2[64:128, 15:528], in0=T1[64:128, 15:528],
                                                            in1=T1[64:128, 7:520], op=ALU.add),
                          reads=["T1"], writes=["T2"])
                        fa, fb = T1, T2
                    o0 = j * 2048 + c * 512
                    for (rs, src) in ((slice(0, 64), fa), (slice(64, 128), fb)):
                        A("pool", lambda e, rs=rs, src=src: e.tensor_scalar(
                            out=TMPF[rs, 0:512], in0=src[rs, 16:528], scalar1=invw[rs, j:j + 1], scalar2=None,
                            op0=ALU.mult),
                          reads=["T1", "T2", "invw"], writes=["TMPF"])
                        A("pool", lambda e, rs=rs: e.tensor_tensor(
                            out=PTP[rs, o0:o0 + 512], in0=TMPF[rs, 0:512], in1=VBUF[rs, 16:528], op=ALU.subtract),
                          reads=["TMPF", "VB_m"], writes=[f"PTP{j}"])
                        if c == 0:
                            A("pool", lambda e, rs=rs, src=src: e.tensor_tensor(
                                out=TMPF[rs, 0:16], in0=src[rs, 16:32], in1=invcnt[rs, j, :], op=ALU.mult),
                              reads=["T1", "T2", "invcnt"], writes=["TMPF"])
                            A("pool", lambda e, rs=rs: e.tensor_tensor(
                                out=PTP[rs, o0:o0 + 16], in0=TMPF[rs, 0:16], in1=VBUF[rs, 16:32], op=ALU.subtract),
                              reads=["TMPF", "VB_m", f"PTP{j}"], writes=[f"PTP{j}"])
                proj_cm(b, j * 128, 128, ev_v)
            for j in range(2):
                for c in range(4):
                    pa = nxt("pa", 2)
                    A("pe", lambda e, pa=pa, j=j, c=c: e.matmul(
                        PA[pa][:, :], lhsT=POOLW[:, j, :], rhs=PTP[:, j * 2048 + c * 512:j * 2048 + (c + 1) * 512],
                        start=True, stop=True),
                      reads=["POOLW", f"PTP{j}"] + [f"POOLW{g}" for g in range(4)], writes=[f"PA{pa}"])
                    A("dve", lambda e, pa=pa, j=j, c=c: e.scalar_tensor_tensor(
                        out=MIXT[:, j, c * 512:(c + 1) * 512], in0=PA[pa][:, :], scalar=PSCL[:, j:j + 1],
                        in1=MIXT[:, j, c * 512:(c + 1) * 512], op0=ALU.mult, op1=ALU.mult),
                      reads=[f"PA{pa}", "PSCL", f"MIXT{j}"], writes=[f"MIXT{j}"])
            out_proj(l, 0, 2)

            if lvl < 3:
                S.barrier()
                continue
            def qk_proj(c_q, c_k):
                bq = load_w(l, [(c_q, 384, 0)])
                for p in range(3):
                    def ev(c, ps_ap, key, p=p):
                        copy_alt(QT[:, p, c * 512:(c + 1) * 512], ps_ap[:, :], [key], [f"QT{p}"], scale=0.125)
                    proj_cm(bq, p * 128, 128, ev)
                bk = load_w(l, [(c_k, 384, 0)])
                for p in range(3):
                    def ev(c, ps_ap, key, p=p):
                        copy_alt(KT[:, p, c * 512:(c + 1) * 512], ps_ap[:, :], [key], [f"KT{p}"])
                    proj_cm(bk, p * 128, 128, ev)

            def gate_proj(c_g):
                bg = load_w(l, [(c_g, 384, 0)])
                for p in range(3):
                    def ev(c, ps_ap, key, p=p):
                        A("act", lambda e: e.activation(out=MIXT[:, p, c * 512:(c + 1) * 512], in_=ps_ap[:, :],
                                                        func=AF.Silu), reads=[key], writes=[f"MIXT{p}"])
                    proj_cm(bg, p * 128, 128, ev)

            if lvl >= 3.01:
                qk_proj(C_FQ, C_FK)
            if lvl < 3.02:
                S.barrier()
                continue
            gate_proj(C_FG)
            if lvl < 3.03:
                S.barrier()
                continue
            bv = load_w(l, [(C_FV, 384, 0)])
            A("pool", lambda e, l=l: e.dma_start(
                out=WFF[:, :, :],
                in_=win_d[l].rearrange("(k p) c -> p k c", p=128)[:, :, IN_COLS - 128:IN_COLS]),
              writes=["WFF"], dma=True, sem_key="WFF")

            def ev_fv(i, ps_ap, key):
                A("dve", lambda e: e.tensor_copy(out=VT[:, i, :], in_=ps_ap[:, 0:384]), reads=[key], writes=["VT"])
                pa = nxt("pa", 2)
                for k in range(8):
                    A("pe", lambda e, pa=pa, k=k: e.matmul(PA[pa][:, 0:6], lhsT=ht(i, k), rhs=WFF[:, k, 122:128],
                                                           start=(k == 0), stop=(k == 7)),
                      reads=["WFF", f"HT{i}"], writes=[f"PA{pa}"])
                A("dve", lambda e, pa=pa: e.tensor_tensor(out=FF[:, i, :], in0=PA[pa][:, 0:6], in1=FBF[:], op=ALU.add),
                  reads=[f"PA{pa}", "FBF"], writes=["FF"])
            proj_tm(bv, 384, ev_fv)
            if lvl < 3.1:
                S.barrier()
                continue
            FFf = FF[:, :, :].rearrange("p i h -> p (i h)")
            A("act", lambda e: e.activation(out=FFf, in_=FFf, func=AF.Exp, scale=-1.0), reads=["FF"], writes=["FF"])
            A("act", lambda e: e.activation(out=FFf, in_=FFf, func=AF.Ln, bias=1.0, scale=1.0),
              reads=["FF"], writes=["FF"])
            pa = nxt("pa", 2)
            A("pe", lambda e, pa=pa: e.matmul(PA[pa][:, 0:96], lhsT=Utri[:], rhs=FFf, start=True, stop=True),
              reads=["Utri", "FF"], writes=[f"PA{pa}"])
            pa2 = nxt("pa", 2)
            A("pe", lambda e, pa2=pa2: e.matmul(PA[pa2][:, 0:96], lhsT=onesf[:], rhs=FFf, start=True, stop=True),
              reads=["onesf", "FF"], writes=[f"PA{pa2}"])
            A("dve", lambda e, pa2=pa2: e.tensor_copy(out=TOT[:, :, :].rearrange("p i h -> p (i h)"),
                                                      in_=PA[pa2][:, 0:96]), reads=[f"PA{pa2}"], writes=["TOT"])
            for i in range(NT):
                A("dve", lambda e, i=i: e.tensor_tensor(out=OFFS[:, i + 1, :], in0=OFFS[:, i, :], in1=TOT[:, i, :],
                                                        op=ALU.add), reads=["TOT", "OFFS"], writes=["OFFS"])
            A("dve", lambda e, pa=pa: e.tensor_tensor(
                out=GG[:, :, :].rearrange("p i h -> p (i h)"), in0=PA[pa][:, 0:96],
                in1=OFFS[:, 0:NT, :].rearrange("p i h -> p (i h)"), op=ALU.add),
              reads=[f"PA{pa}", "OFFS"], writes=["GG"])
            for i in range(1, NT, 2):
                A("dve", lambda e, i=i: e.tensor_tensor(
                    out=BIASALL[:, i * 96:(i + 1) * 96].rearrange("p (j h) -> p j h", j=16),
                    in0=GG[:, :, :],
                    in1=OFFS[:, i + 1:i + 2, :].to_broadcast([128, 16, 6]), op=ALU.subtract),
                  reads=["GG", "OFFS"], writes=["BIASALL"])
            if lvl < 3.2:
                S.barrier()
                continue
            attention("fox")
            out_proj(l, 640, 3)

            if lvl < 4:
                S.barrier()
                continue
            S.barrier()
            qk_proj(C_DQ, C_DK)
            gate_proj(C_DG)
            bi = load_w(l, [(C_IQ, 256, 0)])
            A("pool", lambda e, l=l: e.dma_start(
                out=WFF[:, :, :],
                in_=win_d[l].rearrange("(k p) c -> p k c", p=128)[:, :, C_IK:C_IK + 128]),
              writes=["WFF"], dma=True, sem_key="WFF")
            for rr in range(3):
                A("dve", lambda e, rr=rr, bi=bi: e.tensor_copy(out=WB[bi][:, :, 256 + 32 * rr:288 + 32 * rr],
                                                               in_=WFF[:, :, 0:32]),
                  reads=["WFF"], writes=[f"WB{bi}"])
            for ti, (c0, m) in enumerate(((0, 96), (96, 96), (192, 64))):
                def ev(c, ps_ap, key, ti=ti, m=m):
                    copy_alt(IQT[0:m, ti * 2048 + c * 512:ti * 2048 + (c + 1) * 512], ps_ap[0:m, :], [key], ["IQT"])
                proj_cm(bi, c0, m, ev)

            def ev_ik(c, ps_ap, key):
                copy_alt(IKT[0:96, c * 512:(c + 1) * 512], ps_ap[0:96, :], [key], ["IKT"])
            proj_cm(bi, 256, 96, ev_ik)
            bv = load_w(l, [(C_DV, 384, 0)])

            def ev_dv(i, ps_ap, key):
                A("dve", lambda e: e.tensor_copy(out=VT[:, i, :], in_=ps_ap[:, 0:384]), reads=[key], writes=["VT"])
                pa = nxt("pa", 2)
                for k in range(8):
                    A("pe", lambda e, pa=pa, k=k: e.matmul(PA[pa][:, 0:8], lhsT=ht(i, k), rhs=WFF[:, k, 32:40],
                                                           start=(k == 0), stop=(k == 7)),
                      reads=["WFF", f"HT{i}"], writes=[f"PA{pa}"])
                A("dve", lambda e, pa=pa: e.tensor_scalar(out=IW[:, i, :], in0=PA[pa][:, 0:8], scalar1=8.0 ** -0.5,
                                                          scalar2=None, op0=ALU.mult), reads=[f"PA{pa}"], writes=["IW"])
            proj_tm(bv, 384, ev_dv)
            S.barrier()
            if lvl < 4.1:
                continue
            attention("dsa")
            out_proj(l, 256, 3)
            S.barrier()

        A("sp", lambda e: e.dma_start(out=GB, in_=fg_d[None, :].to_broadcast([128, D])),
          writes=["GB"], dma=True, sem_key="GB")
        rms_stats()
        for i in range(NT):
            yb = i % 2
            A("dve", lambda e, i=i, yb=yb: e.scalar_tensor_tensor(
                out=YST[yb], in0=X[:, i, :], scalar=RSTD[:, i:i + 1], in1=GB, op0=ALU.mult, op1=ALU.mult),
              reads=[f"X{i}", "RSTD", "GB"], writes=[f"YST{yb}"])
            A("sp", lambda e, s=s, i=i, yb=yb: e.dma_start(out=y_d[s, i * 128:(i + 1) * 128, :], in_=YST[yb]),
              reads=[f"YST{yb}"], writes=[], dma=True, sem_key=f"YST{yb}")
        S.barrier()
    S.barrier()
    S.emit(nc)
    es.close()
    return nc


_NC_CACHE = {}


def kernel(x, w_in, norm_g, pool_w, pool_scale, fox_bf, w_out, final_g):
    n_cores = 8
    x = np.ascontiguousarray(np.asarray(x, dtype=np.float32))
    per = x.shape[0] // n_cores
    if "nc" not in _NC_CACHE:
        _NC_CACHE["nc"] = build(n_seq=per, n_layers=4)
    nc = _NC_CACHE["nc"]
    shared = {
        "w_in": np.ascontiguousarray(np.asarray(w_in, dtype=np.float32)),
        "norm_g": np.ascontiguousarray(np.asarray(norm_g, dtype=np.float32)),
        "pool_w": np.ascontiguousarray(np.asarray(pool_w, dtype=np.float32)),
        "pool_scale": np.ascontiguousarray(np.asarray(pool_scale, dtype=np.float32)),
        "fox_bf": np.ascontiguousarray(np.asarray(fox_bf, dtype=np.float32)),
        "w_out": np.ascontiguousarray(np.asarray(w_out, dtype=np.float32)),
        "final_g": np.ascontiguousarray(np.asarray(final_g, dtype=np.float32)),
    }
    in_maps = []
    for c in range(n_cores):
        m = dict(shared)
        m["x"] = np.ascontiguousarray(x[c * per:(c + 1) * per])
        in_maps.append(m)
    res = run_bass_kernel_spmd(nc, in_maps, core_ids=list(range(n_cores)))
    return np.concatenate([np.asarray(r["y"]) for r in res.results], axis=0).astype(np.float32)
```

```python
import contextlib
import numpy as np
import concourse.bass as bass
import concourse.mybir as mybir
from concourse.bass_utils import run_bass_kernel_spmd

F32 = mybir.dt.float32
BF16 = mybir.dt.bfloat16
ALU = mybir.AluOpType
AF = mybir.ActivationFunctionType
AX = mybir.AxisListType

L_SEQ = 2048
D = 1024
NT = 16
IN_COLS = 3886
NEG = -30000.0
NSTEP = 14
TOPK = 256

C_PV, C_PG, C_DQ, C_DK, C_DV, C_DG = 0, 256, 512, 896, 1280, 1664
C_IQ, C_IK, C_IW, C_FQ, C_FK, C_FV, C_FG, C_FF = 2048, 2304, 2336, 2344, 2728, 3112, 3496, 3880

SEM_SPAN = 3000
SAME_ENG_WINDOW = 5


class Op:
    __slots__ = ("id", "eng", "fn", "deps", "dma", "sem_key", "dcount", "gsig",
                 "eidx", "signals")

    def __init__(self, id, eng, fn, dma, sem_key):
        self.id = id
        self.eng = eng
        self.fn = fn
        self.deps = set()
        self.dma = dma
        self.sem_key = sem_key
        self.dcount = 0
        self.gsig = 0
        self.eidx = 0
        self.signals = False


class Sched:
    ENGS = ("pe", "act", "dve", "pool", "sp")

    def __init__(self):
        self.ops = []
        self.last_writer = {}
        self.readers = {}
        self.eng_count = {e: 0 for e in self.ENGS}
        self.dma_counts = {}
        self.last_dma = {}

    def add(self, eng, fn, reads=(), writes=(), dma=False, sem_key=None, extra_deps=()):
        op = Op(len(self.ops), eng, fn, dma, sem_key)
        if dma:
            assert sem_key is not None
            self.dma_counts[sem_key] = self.dma_counts.get(sem_key, 0) + 1
            op.dcount = self.dma_counts[sem_key]
            self.last_dma[sem_key] = op.id
        op.eidx = self.eng_count[eng]
        self.eng_count[eng] += 1
        for k in list(reads) + list(writes):
            w = self.last_writer.get(k)
            if w is not None:
                op.deps.add(w)
        for k in writes:
            for r in self.readers.get(k, ()):
                op.deps.add(r)
        for k in reads:
            self.readers.setdefault(k, []).append(op.id)
        for k in writes:
            self.last_writer[k] = op.id
            self.readers[k] = []
        for d in extra_deps:
            op.deps.add(d)
        op.deps.discard(op.id)
        self.ops.append(op)
        return op

    def barrier(self):
        last = {}
        for op in self.ops:
            if not op.dma and op.fn is not None:
                last[op.eng] = op.id
        deps = list(last.values()) + list(self.last_dma.values())
        for e in self.ENGS:
            self.add(e, None, extra_deps=deps)

    def emit(self, nc):
        ops = self.ops
        for op in ops:
            for d in op.deps:
                p = ops[d]
                if p.dma or p.fn is None:
                    continue
                if p.eng == op.eng:
                    if op.eng == "pe":
                        continue
                    if op.eidx - p.eidx > SAME_ENG_WINDOW:
                        continue
                p.signals = True
        gs = {e: 0 for e in self.ENGS}
        for op in ops:
            if op.signals and not op.dma:
                gs[op.eng] += 1
                op.gsig = gs[op.eng]
        stack = []
        sems = {}
        for e in self.ENGS:
            for i in range((gs[e] + SEM_SPAN - 1) // SEM_SPAN):
                cm = nc.semaphore(f"s_{e}_{i}")
                sems[(e, i)] = cm.__enter__()
                stack.append(cm)
        dsems = {}
        for k in self.dma_counts:
            cm = nc.semaphore(f"d_{len(dsems)}")
            dsems[k] = cm.__enter__()
            stack.append(cm)
        self.n_sems = len(stack)
        per_eng = {e: [op for op in ops if op.eng == e] for e in self.ENGS}

        def run_engine(e, eng):
            waited = {x: 0 for x in self.ENGS}
            dwaited = {}
            for op in per_eng[e]:
                need = {}
                dneed = {}
                for d in op.deps:
                    p = ops[d]
                    if p.fn is None:
                        continue
                    if p.dma:
                        if dwaited.get(p.sem_key, 0) < p.dcount:
                            dneed[p.sem_key] = max(dneed.get(p.sem_key, 0), p.dcount)
                        continue
                    if p.eng == e:
                        if e == "pe" or op.eidx - p.eidx > SAME_ENG_WINDOW:
                            continue
                    if p.gsig > waited[p.eng]:
                        need[p.eng] = max(need.get(p.eng, 0), p.gsig)
                for pe_, g in need.items():
                    si = (g - 1) // SEM_SPAN
                    eng.wait_ge(sems[(pe_, si)], g - si * SEM_SPAN)
                    waited[pe_] = g
                for k, c in dneed.items():
                    eng.wait_ge(dsems[k], 16 * c)
                    dwaited[k] = c
                if op.fn is None:
                    continue
                ins = op.fn(eng)
                if op.dma:
                    ins.then_inc(dsems[op.sem_key], 16)
                elif op.signals:
                    si = (op.gsig - 1) // SEM_SPAN
                    ins.then_inc(sems[(op.eng, si)], 1)

        with nc.Block() as block:
            @block.tensor
            def _(eng):
                run_engine("pe", eng)

            @block.scalar
            def _(eng):
                run_engine("act", eng)

            @block.vector
            def _(eng):
                run_engine("dve", eng)

            @block.gpsimd
            def _(eng):
                run_engine("pool", eng)

            @block.sync
            def _(eng):
                run_engine("sp", eng)
        for cm in reversed(stack):
            cm.__exit__(None, None, None)


def build(n_seq=2, n_layers=4, lvl=9):
    nc = bass.Bass("TRN2", target_bir_lowering=False)
    x_d = nc.dram_tensor("x", [n_seq, L_SEQ, D], F32, kind="ExternalInput").ap()
    win_d = nc.dram_tensor("w_in", [4, D, IN_COLS], F32, kind="ExternalInput").ap()
    ng_d = nc.dram_tensor("norm_g", [4, D], F32, kind="ExternalInput").ap()
    pw_d = nc.dram_tensor("pool_w", [4, 4, 64, 64], F32, kind="ExternalInput").ap()
    psc_d = nc.dram_tensor("pool_scale", [4, 256], F32, kind="ExternalInput").ap()
    fbf_d = nc.dram_tensor("fox_bf", [4, 6], F32, kind="ExternalInput").ap()
    wout_d = nc.dram_tensor("w_out", [4, D, D], F32, kind="ExternalInput").ap()
    fg_d = nc.dram_tensor("final_g", [D], F32, kind="ExternalInput").ap()
    y_d = nc.dram_tensor("y", [n_seq, L_SEQ, D], F32, kind="ExternalOutput").ap()

    S = Sched()
    A = S.add
    es = contextlib.ExitStack()

    def sb(name, shape, dt):
        return es.enter_context(nc.sbuf_tensor(name, shape, dt))

    def pst(name, shape, dt):
        return es.enter_context(nc.psum_tensor(name, shape, dt))

    X = sb("X", [128, NT, D], F32)
    HT = sb("HT", [128, NT * 8 * 128], BF16)
    MIXT = sb("MIXT", [128, 3, L_SEQ], BF16)
    QT = sb("QT", [128, 3, L_SEQ], BF16)
    KT = sb("KT", [128, 3, L_SEQ], BF16)
    VT = sb("VT", [128, NT, 384], BF16)
    WBT = sb("WBT", [128, 2, 8, 392], BF16)
    WB = [WBT[:, 0, :, :], WBT[:, 1, :, :]]
    WOB = sb("WOB", [128, 3, D], BF16)
    WFF = sb("WFF", [128, 8, 128], BF16)
    SCR = sb("SCR", [128, 24576], mybir.dt.uint8)
    PT = [sb(f"PT{i}", [128, 512], BF16) for i in range(4)]
    RB = [sb(f"RB{i}", [128, 512], BF16) for i in range(3)]
    RD = sb("RD", [128, 512], F32)
    TMPF = sb("TMPF", [128, 512], F32)
    ident = sb("ident", [128, 128], BF16)
    tri = sb("tri", [128, 128], BF16)
    Utri = sb("Utri", [128, 128], F32)
    onesf = sb("onesf", [128, 128], F32)
    ones64 = sb("ones64", [128, 64], BF16)
    dmaskf = sb("dmaskf", [128, 128], F32)
    dmaskb = sb("dmaskb", [128, 128], BF16)
    dmaskg = sb("dmaskg", [128, 128], BF16)
    halves = sb("halves", [128, NSTEP + 1], F32)
    invw = sb("invw", [128, 2], F32)
    invcnt = sb("invcnt", [128, 2, 16], F32)
    SS = sb("SS", [128, NT], F32)
    RSTD = sb("RSTD", [128, NT], F32)
    PSCL = sb("PSCL", [128, 2], F32)
    POOLW = sb("POOLW", [128, 2, 128], BF16)
    FBF = sb("FBF", [128, 6], F32)
    IW = sb("IW", [128, NT, 8], F32)
    FF = sb("FF", [128, NT, 6], F32)
    TOT = sb("TOT", [128, NT, 6], F32)
    OFFS = sb("OFFS", [128, NT + 1, 6], F32)
    GG = sb("GG", [128, NT, 6], F32)
    DW = sb("DW", [128, 8, 128], BF16)
    BS = sb("BS", [128, 8 + NSTEP + 1], F32)

    def view(t, dt, byte_off, n_elem):
        ap = t[:, :]
        apb = ap.bitcast(dt)
        esz = mybir.dt.size(dt)
        tsz = mybir.dt.size(t.dtype)
        e0 = byte_off // esz
        return apb[:, e0:e0 + n_elem]

    HB = [view(SCR, BF16, 16384, 1024), view(SCR, BF16, 18432, 1024)]
    GB = view(SCR, F32, 20480, 1024)
    JUNKN = view(SCR, BF16, 8192, 1024)
    VBUF = view(SCR, F32, 0, 528)
    T1 = view(SCR, F32, 2112, 528)
    T2 = view(SCR, F32, 4224, 528)
    PTP = view(SCR, BF16, 8192, 4096)
    BIASALL = view(SCR, F32, 16384, 16 * 16 * 6)
    IQT = view(SCR, BF16, 0, 3 * 2048)
    IKT = view(SCR, BF16, 12288, 2048)
    SCORES = [view(SCR, F32, 16384, 2048), WBT[:, :, :, :].rearrange('p a k c -> p (a k c)').bitcast(F32)[:, 0:2048]]
    YST = [view(HT, F32, 0, 1024), view(HT, F32, 4096, 1024)]

    def ht(i, k):
        o = (i * 8 + k) * 128
        return HT[:, o:o + 128]

    HT4 = HT[:, :].rearrange("p (i k t) -> p i k t", i=NT, k=8)

    PTR = pst("PTR", [128, 1024], BF16)
    PA = [pst("PA0", [128, 512], F32), pst("PA1", [128, 512], F32)]
    PS = [pst("PS0", [128, 512], F32), pst("PS1", [128, 512], F32)]
    PO = [pst("PO0", [128, 512], F32), pst("PO1", [128, 512], F32)]
    PD = [pst("PD0", [128, 512], F32)]
    rot = {"pa": 0, "ps": 0, "po": 0, "pt": 0, "rb": 0, "wb": 0, "hb": 0}

    def nxt(name, n):
        v = rot[name]
        rot[name] = (v + 1) % n
        return v

    A("pool", lambda e: e.memset(TMPF[:, 0:128], 0.0), writes=["TMPF"])
    A("pool", lambda e: e.affine_select(out=TMPF[:, 0:128], in_=TMPF[:, 0:128], pattern=[[-1, 128]],
                                        compare_op=ALU.not_equal, fill=1.0, base=0,
                                        channel_multiplier=1), reads=["TMPF"], writes=["TMPF"])
    A("dve", lambda e: e.tensor_copy(out=ident[:], in_=TMPF[:, 0:128]), reads=["TMPF"], writes=["ident"])
    A("pool", lambda e: e.memset(TMPF[:, 0:128], 0.0), writes=["TMPF"])
    A("pool", lambda e: e.affine_select(out=TMPF[:, 0:128], in_=TMPF[:, 0:128], pattern=[[1, 128]],
                                        compare_op=ALU.is_ge, fill=NEG, base=0,
                                        channel_multiplier=-1), reads=["TMPF"], writes=["TMPF"])
    A("dve", lambda e: e.tensor_copy(out=tri[:], in_=TMPF[:, 0:128]), reads=["TMPF"], writes=["tri"])
    A("pool", lambda e: e.memset(Utri[:], 1.0), writes=["Utri"])
    A("pool", lambda e: e.affine_select(out=Utri[:], in_=Utri[:], pattern=[[1, 128]],
                                        compare_op=ALU.is_ge, fill=0.0, base=0,
                                        channel_multiplier=-1), reads=["Utri"], writes=["Utri"])
    A("pool", lambda e: e.memset(onesf[:], 1.0), writes=["onesf"])
    A("pool", lambda e: e.memset(ones64[:], 1.0), writes=["ones64"])
    A("pool", lambda e: e.memset(dmaskf[:], 0.0), writes=["dmaskf"])
    A("pool", lambda e: e.memset(dmaskf[0:64, 64:128], -1.0e30), reads=["dmaskf"], writes=["dmaskf"])
    A("pool", lambda e: e.memset(dmaskg[:], 0.0), writes=["dmaskg"])
    A("pool", lambda e: e.memset(dmaskg[0:64, 64:128], -1.0e30), reads=["dmaskg"], writes=["dmaskg"])
    A("pool", lambda e: e.memset(dmaskb[:], 0.0), writes=["dmaskb"])
    A("pool", lambda e: e.memset(dmaskb[0:64, 64:128], NEG), reads=["dmaskb"], writes=["dmaskb"])
    for k in range(NSTEP + 1):
        A("pool", lambda e, k=k: e.memset(halves[:, k:k + 1], 0.5 ** (k + 1)), writes=["halves"])
    for j, (wa, wb_) in enumerate(((2, 4), (8, 16))):
        A("pool", lambda e, j=j, wa=wa: e.memset(invw[0:64, j:j + 1], 1.0 / wa), writes=["invw"])
        A("pool", lambda e, j=j, wb_=wb_: e.memset(invw[64:128, j:j + 1], 1.0 / wb_), writes=["invw"])
        for t in range(16):
            A("pool", lambda e, j=j, t=t, wa=wa: e.memset(invcnt[0:64, j, t:t + 1], 1.0 / min(t + 1, wa)),
              writes=["invcnt"])
            A("pool", lambda e, j=j, t=t, wb_=wb_: e.memset(invcnt[64:128, j, t:t + 1], 1.0 / min(t + 1, wb_)),
              writes=["invcnt"])
    A("pool", lambda e: e.memset(OFFS[:, 0, :], 0.0), writes=["OFFS"])

    def load_w(l, pieces):
        b = nxt("wb", 2)
        for (c0, n, s0) in pieces:
            A("pool", lambda e, b=b, c0=c0, n=n, s0=s0: e.dma_start(
                out=WB[b][:, :, s0:s0 + n],
                in_=win_d[l].rearrange("(k p) c -> p k c", p=128)[:, :, c0:c0 + n]),
              writes=[f"WB{b}"], dma=True, sem_key=f"WB{b}")
        return b

    def proj_cm(b, col0, M, evac):
        for c in range(4):
            pa = nxt("pa", 2)
            for k in range(8):
                A("pe", lambda e, pa=pa, k=k, c=c: e.matmul(
                    PA[pa][0:M, :], lhsT=WB[b][:, k, col0:col0 + M],
                    rhs=HT4[:, 4 * c:4 * c + 4, k, :], start=(k == 0), stop=(k == 7)),
                  reads=[f"WB{b}"] + [f"HT{i}" for i in range(4 * c, 4 * c + 4)], writes=[f"PA{pa}"])
            evac(c, PA[pa], f"PA{pa}")

    def proj_tm(b, N, evac):
        for i in range(NT):
            pa = nxt("pa", 2)
            for k in range(8):
                A("pe", lambda e, pa=pa, k=k, i=i: e.matmul(
                    PA[pa][:, 0:N], lhsT=ht(i, k), rhs=WB[b][:, k, 0:N],
                    start=(k == 0), stop=(k == 7)),
                  reads=[f"WB{b}", f"HT{i}"], writes=[f"PA{pa}"])
            evac(i, PA[pa], f"PA{pa}")

    cnt = {"alt": 0}

    def copy_alt(out, in_, reads, writes, scale=None):
        cnt["alt"] += 1
        if cnt["alt"] % 2 == 0:
            if scale is None:
                A("act", lambda e: e.activation(out=out, in_=in_, func=AF.Copy), reads=reads, writes=writes)
            else:
                A("act", lambda e: e.activation(out=out, in_=in_, func=AF.Copy, scale=scale),
                  reads=reads, writes=writes)
        else:
            if scale is None:
                A("dve", lambda e: e.tensor_copy(out=out, in_=in_), reads=reads, writes=writes)
            else:
                A("dve", lambda e: e.tensor_scalar(out=out, in0=in_, scalar1=scale, scalar2=None,
                                                   op0=ALU.mult), reads=reads, writes=writes)

    def out_proj(l, row0, nch):
        A("pool", lambda e: e.dma_start(
            out=WOB[:, 0:nch, :],
            in_=wout_d[l, row0:row0 + nch * 128, :].rearrange("(q p) c -> p q c", p=128)),
          writes=["WOB"], dma=True, sem_key="WOB")
        for i in range(NT):
            for n in range(2):
                pa = nxt("pa", 2)
                for q in range(nch):
                    A("pe", lambda e, pa=pa, q=q, i=i, n=n: e.matmul(
                        PA[pa][:, :], lhsT=MIXT[:, q, i * 128:(i + 1) * 128],
                        rhs=WOB[:, q, n * 512:(n + 1) * 512], start=(q == 0), stop=(q == nch - 1)),
                      reads=["WOB", f"MIXT{q}"], writes=[f"PA{pa}"])
                A("dve", lambda e, pa=pa, i=i, n=n: e.tensor_tensor(
                    out=X[:, i, n * 512:(n + 1) * 512], in0=X[:, i, n * 512:(n + 1) * 512],
                    in1=PA[pa][:, :], op=ALU.add),
                  reads=[f"PA{pa}", f"X{i}"], writes=[f"X{i}"])

    def rms_stats():
        for i in range(NT):
            A("act", lambda e, i=i: e.activation(out=JUNKN, in_=X[:, i, :], func=AF.Square,
                                                 accum_out=SS[:, i:i + 1]),
              reads=[f"X{i}"], writes=["JUNKN", "SS"])
        A("act", lambda e: e.activation(out=RSTD[:], in_=SS[:], func=AF.Sqrt, bias=EPS_T[:, 0:1], scale=1.0 / D),
          reads=["SS"], writes=["RSTD"])
        A("dve", lambda e: e.reciprocal(out=RSTD[:], in_=RSTD[:]), reads=["RSTD"], writes=["RSTD"])

    EPS_T = sb("EPS_T", [128, 1], F32)
    A("pool", lambda e: e.memset(EPS_T[:], 1e-6), writes=["EPS_T"])
    C255 = sb("C255", [128, 1], F32)
    A("pool", lambda e: e.memset(C255[:], TOPK - 0.5), writes=["C255"])
    CNEG = sb("CNEG", [128, 1], F32)
    A("pool", lambda e: e.memset(CNEG[:], NEG), writes=["CNEG"])


    def mb_view(c, q):
        base = (c % 2) * 16384
        o = 0
        for qq in range(q):
            o += (4 * c + qq + 1) * 128
        n = (4 * c + q + 1) * 128
        return view(HT, BF16, base + 2 * o, n)

    PSC = PTR[:, :].bitcast(F32)

    def attention(kind):
        if kind == "dsa":
            for slot in range(3):
                mask_jobs(0, slot)
        for c in range(4):
            attn_chunk(kind, c)

    def attn_chunk(kind, c):
        nj = 4 * c + 4
        t0 = 4 * c * 128
        po_of = {}
        pd = 0
        pre = {0: (0, 1), 1: (2,), 2: (3,)}

        def qk(p, j, hh):
            h = 2 * p + hh
            r0 = 64 * hh
            i_lo = max(4 * c, j)
            off = (i_lo - 4 * c) * 128
            ps = nxt("ps", 2)
            psk = f"PS{ps}"
            pt = nxt("pt", 4)
            kslc = KT[r0:r0 + 64, p, j * 128:(j + 1) * 128]
            if kind == "fox":
                if j >= 4 * c:
                    A("pe", lambda e: e.matmul(PS[ps][:, off:off + 128], lhsT=ident[:], rhs=tri[:],
                                               start=True, stop=False),
                      reads=["ident", "tri"], writes=[psk])
                    A("pe", lambda e: e.matmul(PS[ps][:, off:off + 128], lhsT=kslc,
                                               rhs=QT[r0:r0 + 64, p, t0 + off:t0 + off + 128],
                                               start=False, stop=True),
                      reads=[f"KT{p}", f"QT{p}"], writes=[psk])
                    if off + 128 < 512:
                        A("pe", lambda e: e.matmul(PS[ps][:, off + 128:512], lhsT=kslc,
                                                   rhs=QT[r0:r0 + 64, p, t0 + off + 128:t0 + 512],
                                                   start=True, stop=True),
                          reads=[f"KT{p}", f"QT{p}"], writes=[psk])
                else:
                    A("pe", lambda e: e.matmul(PS[ps][:, 0:512], lhsT=kslc, rhs=QT[r0:r0 + 64, p, t0:t0 + 512],
                                               start=True, stop=True),
                      reads=[f"KT{p}", f"QT{p}"], writes=[psk])
                i = i_lo
                while i < 4 * c + 4:
                    wd = 2 if i % 2 == 0 else 1
                    co = (i - 4 * c) * 128
                    bcol = ((i | 1) * 16 + j) * 6 + h
                    A("act", lambda e, co=co, wd=wd, bcol=bcol: e.activation(
                        out=PT[pt][:, co:co + 128 * wd], in_=PS[ps][:, co:co + 128 * wd], func=AF.Exp,
                        bias=BIASALL[:, bcol:bcol + 1], scale=1.0),
                      reads=[psk, "BIASALL"], writes=[f"PT{pt}"])
                    i += wd
            else:
                for i in range(i_lo, 4 * c + 4):
                    co = (i - 4 * c) * 128
                    q = i - 4 * c
                    mbv = mb_view(c, q)
                    A("pe", lambda e, co=co, mbv=mbv: e.matmul(
                        PS[ps][:, co:co + 128], lhsT=mbv[:, j * 128:(j + 1) * 128], rhs=ident[:],
                        start=True, stop=False),
                      reads=[f"MB{c % 2}_{q}", "ident"], writes=[psk])
                    A("pe", lambda e, co=co: e.matmul(
                        PS[ps][:, co:co + 128], lhsT=kslc,
                        rhs=QT[r0:r0 + 64, p, t0 + co:t0 + co + 128], start=False, stop=True),
                      reads=[f"KT{p}", f"QT{p}"], writes=[psk])
                A("act", lambda e: e.activation(out=PT[pt][:, off:512], in_=PS[ps][:, off:512], func=AF.Exp),
                  reads=[psk], writes=[f"PT{pt}"])
            return (p, j, hh, pt, off)

        def pv(blk):
            p, j, hh, pt, off = blk
            h = 2 * p + hh
            r0 = 64 * hh
            if p not in po_of:
                po_of[p] = nxt("po", 2)
            po = po_of[p]
            A("pe", lambda e: e.matmul(
                PO[po][r0:r0 + 64, off:512], lhsT=VT[:, j, h * 64:(h + 1) * 64], rhs=PT[pt][:, off:512],
                start=(j == 0), stop=(j == nj - 1), tile_position=(0, r0)),
              reads=[f"PT{pt}", "VT"], writes=[f"PO{po}"])
            A("pe", lambda e: e.matmul(
                PD[pd][r0:r0 + 64, off:512], lhsT=ones64[:], rhs=PT[pt][:, off:512],
                start=(j == 0), stop=(j == nj - 1), tile_position=(0, r0)),
              reads=[f"PT{pt}", "ones64"], writes=[f"PD{pd}"])
            if j == nj - 1 and hh == 1:
                csl = slice(4 * c * 128, (4 * c + 4) * 128)
                A("dve", lambda e: e.reciprocal(out=RD[:], in_=PD[pd][:, :]), reads=[f"PD{pd}"], writes=["RD"])
                A("dve", lambda e: e.tensor_tensor(out=TMPF[:], in0=PO[po][:, :], in1=RD[:], op=ALU.mult),
                  reads=[f"PO{po}", "RD"], writes=["TMPF"])
                A("dve", lambda e: e.tensor_tensor(out=MIXT[:, p, csl], in0=TMPF[:], in1=MIXT[:, p, csl],
                                                   op=ALU.mult),
                  reads=["TMPF", f"MIXT{p}"], writes=[f"MIXT{p}"])

        pend = None
        for p in range(3):
            if kind == "dsa" and c < 3:
                mask_jobs(c + 1, p)
            for j in range(nj):
                for hh in range(2):
                    cur = qk(p, j, hh)
                    if pend is not None:
                        pv(pend)
                    pend = cur
        pv(pend)

    def dsa_idx_tile(c, q):
        i = 4 * c + q
        n = (i + 1) * 128
        mk = f"MB{c % 2}_{q}"
        mbv = mb_view(c, q)
        if i < 2:
            if i == 1:
                A("pool", lambda e: e.memset(mbv[:, 0:128], 0.0), writes=[mk])
            A("pool", lambda e: e.tensor_copy(out=mbv[:, n - 128:n], in_=dmaskb[:]),
              reads=["dmaskb"], writes=[mk])
            return
        SC = SCORES[q % 2]
        sk = f"SCORE{q % 2}"
        for h in range(8):
            A("act", lambda e, h=h: e.activation(out=DW[:, h, :], in_=ident[:], func=AF.Copy,
                                                 scale=IW[:, i, h:h + 1]),
              reads=["ident", "IW"], writes=["DW"])
        nsc = (n + 511) // 512
        for sc in range(nsc):
            w = min(512, n - sc * 512)
            last = (sc == nsc - 1)
            for h in range(8):
                ti, r = h // 3, (h % 3) * 32
                pa = nxt("pa", 2)
                rb = nxt("rb", 3)
                A("pe", lambda e, pa=pa, ti=ti, r=r, w=w, sc=sc: e.matmul(
                    PA[pa][:, 0:w], lhsT=IQT[r:r + 32, ti * 2048 + i * 128:ti * 2048 + (i + 1) * 128],
                    rhs=IKT[r:r + 32, sc * 512:sc * 512 + w], start=True, stop=True),
                  reads=["IQT", "IKT"], writes=[f"PA{pa}"])
                A("act", lambda e, pa=pa, rb=rb, w=w: e.activation(out=RB[rb][:, 0:w], in_=PA[pa][:, 0:w],
                                                                   func=AF.Relu),
                  reads=[f"PA{pa}"], writes=[f"RB{rb}"])
                A("pe", lambda e, rb=rb, h=h, w=w, last=last: e.matmul(
                    PSC[:, 0:w], lhsT=DW[:, h, :], rhs=RB[rb][:, 0:w], start=(h == 0),
                    stop=(h == 7 and not last)),
                  reads=["DW", f"RB{rb}"], writes=["PTR"])
            if last:
                A("pe", lambda e, w=w: e.matmul(PSC[:, w - 128:w], lhsT=ident[:], rhs=dmaskg[:],
                                                start=False, stop=True),
                  reads=["ident", "dmaskg"], writes=["PTR"])
            s0 = sc * 512
            A("act", lambda e, s0=s0, w=w: e.activation(out=SC[:, s0:s0 + w], in_=PSC[:, 0:w], func=AF.Copy),
              reads=["PTR"], writes=[sk])

    def dsa_bis_tile(c, q):
        i = 4 * c + q
        if i < 2:
            return
        n = (i + 1) * 128
        mk = f"MB{c % 2}_{q}"
        mbv = mb_view(c, q)
        SC = SCORES[q % 2]
        sk = f"SCORE{q % 2}"
        eng = "dve"
        mx, mn, rg, mid, cn, tp = (BS[:, 0:1], BS[:, 1:2], BS[:, 2:3], BS[:, 3:4], BS[:, 4:5], BS[:, 5:6])
        DL = BS[:, 8:8 + NSTEP + 1]
        A(eng, lambda e: e.tensor_reduce(out=mx, in_=SC[:, 0:n], axis=AX.X, op=ALU.max),
          reads=[sk], writes=["BSmx"])
        A(eng, lambda e: e.tensor_reduce(out=mn, in_=SC[:, 0:n - 64], axis=AX.X, op=ALU.min),
          reads=[sk], writes=["BSmn"])
        A(eng, lambda e: e.tensor_tensor(out=rg, in0=mx, in1=mn, op=ALU.subtract),
          reads=["BSmx", "BSmn"], writes=["BSrg"])
        A(eng, lambda e: e.tensor_scalar(out=DL, in0=halves[:], scalar1=rg, scalar2=None, op0=ALU.mult),
          reads=["BSrg", "halves"], writes=["BSdl"])
        A(eng, lambda e: e.tensor_tensor(out=mid, in0=mn, in1=BS[:, 8:9], op=ALU.add),
          reads=["BSmn", "BSdl"], writes=["BSmid"])
        for k in range(NSTEP):
            A(eng, lambda e: e.tensor_scalar(out=mbv[:, 0:n], in0=SC[:, 0:n], scalar1=mid,
                                             scalar2=None, op0=ALU.is_ge, op1=ALU.add, accum_out=cn),
              reads=[sk, "BSmid"], writes=[mk, "BScn"])
            A(eng, lambda e, k=k: e.tensor_scalar(out=tp, in0=cn, scalar1=C255[:, 0:1],
                                                  scalar2=BS[:, 8 + k:9 + k], op0=ALU.is_ge, op1=ALU.mult),
              reads=["BScn", "BSdl"], writes=["BStp"])
            A(eng, lambda e, k=k: e.scalar_tensor_tensor(out=mid, in0=tp, scalar=BS[:, 9 + k:10 + k], in1=mid,
                                                         op0=ALU.subtract, op1=ALU.add),
              reads=["BStp", "BSdl", "BSmid"], writes=["BSmid"])
        A(eng, lambda e: e.tensor_tensor(out=tp, in0=mid, in1=BS[:, 8 + NSTEP:9 + NSTEP], op=ALU.subtract),
          reads=["BSmid", "BSdl"], writes=["BStp"])
        A(eng, lambda e: e.tensor_scalar(out=mbv[:, 0:n], in0=SC[:, 0:n], scalar1=tp,
                                         scalar2=CNEG[:, 0:1], op0=ALU.is_lt, op1=ALU.mult),
          reads=[sk, "BStp"], writes=[mk])

    def mask_jobs(c, slot):
        if slot == 0:
            dsa_idx_tile(c, 0); dsa_idx_tile(c, 1); dsa_bis_tile(c, 0)
        elif slot == 1:
            dsa_idx_tile(c, 2); dsa_bis_tile(c, 1)
        else:
            dsa_idx_tile(c, 3); dsa_bis_tile(c, 2); dsa_bis_tile(c, 3)

    for s in range(n_seq):
        for qd in range(4):
            A("sp", lambda e, s=s, qd=qd: e.dma_start(
                out=X[:, 4 * qd:4 * qd + 4, :],
                in_=x_d[s].rearrange("(i p) d -> p i d", p=128)[:, 4 * qd:4 * qd + 4, :]),
              writes=[f"X{i}" for i in range(4 * qd, 4 * qd + 4)], dma=True, sem_key=f"XL{qd}")
        for l in range(n_layers):
            if lvl < 1:
                break
            A("sp", lambda e, l=l: e.dma_start(out=GB, in_=ng_d[l:l + 1, :].to_broadcast([128, D])),
              writes=["GB"], dma=True, sem_key="GB")
            for j in range(2):
                A("sp", lambda e, l=l, j=j: e.dma_start(
                    out=PSCL[:, j:j + 1], in_=psc_d[l, j * 128:(j + 1) * 128].rearrange("(p o) -> p o", o=1)),
                  writes=["PSCL"], dma=True, sem_key=f"PSCL{j}")
            A("sp", lambda e, l=l: e.dma_start(out=FBF[:], in_=fbf_d[l:l + 1, :].to_broadcast([128, 6])),
              writes=["FBF"], dma=True, sem_key="FBF")
            A("pool", lambda e: e.memset(POOLW[:], 0.0), writes=["POOLW"])
            for g in range(4):
                j, r0 = g // 2, (g % 2) * 64
                A("pool", lambda e, l=l, g=g, j=j, r0=r0: e.dma_start(
                    out=POOLW[r0:r0 + 64, j, r0:r0 + 64], in_=pw_d[l, g]),
                  reads=[], writes=["POOLW"], dma=True, sem_key=f"POOLW{g}")
            rms_stats()
            for i in range(NT):
                hb = nxt("hb", 2)
                A("dve", lambda e, i=i, hb=hb: e.scalar_tensor_tensor(
                    out=HB[hb], in0=X[:, i, :], scalar=RSTD[:, i:i + 1], in1=GB, op0=ALU.mult, op1=ALU.mult),
                  reads=[f"X{i}", "RSTD", "GB"], writes=[f"HB{hb}"])
                for k in range(8):
                    A("pe", lambda e, hb=hb, k=k: e.transpose(out=PTR[:, k * 128:(k + 1) * 128],
                                                              in_=HB[hb][:, k * 128:(k + 1) * 128],
                                                              identity=ident[:]),
                      reads=[f"HB{hb}", "ident"], writes=["PTR"])
                copy_alt(HT[:, i * 1024:(i + 1) * 1024], PTR[:, :], ["PTR"], [f"HT{i}"])

            if lvl < 2:
                S.barrier()
                continue
            b = load_w(l, [(C_PG, 256, 0)])
            for j in range(2):
                def ev_g(c, ps_ap, key, j=j):
                    A("act", lambda e: e.activation(out=MIXT[:, j, c * 512:(c + 1) * 512], in_=ps_ap[:, :],
                                                    func=AF.Silu), reads=[key], writes=[f"MIXT{j}"])
                proj_cm(b, j * 128, 128, ev_g)
            b = load_w(l, [(C_PV, 256, 0)])
            for j in range(2):
                srcA = (T1, T2)
                def ev_v(c, ps_ap, key, j=j):
                    if c == 0:
                        A("pool", lambda e: e.memset(VBUF[:, 0:16], 0.0), writes=["VB_h"])
                    else:
                        A("pool", lambda e: e.tensor_copy(out=VBUF[:, 0:16], in_=VBUF[:, 512:528]),
                          reads=["VB_m"], writes=["VB_h"])
                    A("act", lambda e: e.activation(out=VBUF[:, 16:528], in_=ps_ap[:, :], func=AF.Copy),
                      reads=[key, "VB_h"], writes=["VB_m"])
                    rk = ["VB_h", "VB_m"]
                    A("pool", lambda e: e.tensor_tensor(out=T1[:, 1:528], in0=VBUF[:, 1:528], in1=VBUF[:, 0:527],
                                                        op=ALU.add), reads=rk, writes=["T1"])
                    if j == 0:
                        A("pool", lambda e: e.tensor_tensor(out=T2[64:128, 3:528], in0=T1[64:128, 3:528],
                                                            in1=T1[64:128, 1:526], op=ALU.add),
                          reads=["T1"], writes=["T2"])
                        fa, fb = T1, T2
                    else:
                        A("pool", lambda e: e.tensor_tensor(out=T2[:, 3:528], in0=T1[:, 3:528], in1=T1[:, 1:526],
                                                            op=ALU.add), reads=["T1"], writes=["T2"])
                        A("pool", lambda e: e.tensor_tensor(out=T1[:, 7:528], in0=T2[:, 7:528], in1=T2[:, 3:524],
                                                            op=ALU.add), reads=["T2"], writes=["T1"])
                        A("pool", lambda e: e.tensor_tensor(out=T2[64:128, 15:528], in0=T1[64:128, 15:528],
                                                            in1=T1[64:128, 7:520], op=ALU.add),
                          reads=["T1"], writes=["T2"])
                        fa, fb = T1, T2
                    o0 = j * 2048 + c * 512
                    for (rs, src) in ((slice(0, 64), fa), (slice(64, 128), fb)):
                        A("pool", lambda e, rs=rs, src=src: e.tensor_scalar(
                            out=TMPF[rs, 0:512], in0=src[rs, 16:528], scalar1=invw[rs, j:j + 1], scalar2=None,
                            op0=ALU.mult),
                          reads=["T1", "T2", "invw"], writes=["TMPF"])
                        A("pool", lambda e, rs=rs: e.tensor_tensor(
                            out=PTP[rs, o0:o0 + 512], in0=TMPF[rs, 0:512], in1=VBUF[rs, 16:528], op=ALU.subtract),
                          reads=["TMPF", "VB_m"], writes=[f"PTP{j}"])
                        if c == 0:
                            A("pool", lambda e, rs=rs, src=src: e.tensor_tensor(
                                out=TMPF[rs, 0:16], in0=src[rs, 16:32], in1=invcnt[rs, j, :], op=ALU.mult),
                              reads=["T1", "T2", "invcnt"], writes=["TMPF"])
                            A("pool", lambda e, rs=rs: e.tensor_tensor(
                                out=PTP[rs, o0:o0 + 16], in0=TMPF[rs, 0:16], in1=VBUF[rs, 16:32], op=ALU.subtract),
                              reads=["TMPF", "VB_m", f"PTP{j}"], writes=[f"PTP{j}"])
                proj_cm(b, j * 128, 128, ev_v)
            for j in range(2):
                for c in range(4):
                    pa = nxt("pa", 2)
                    A("pe", lambda e, pa=pa, j=j, c=c: e.matmul(
                        PA[pa][:, :], lhsT=POOLW[:, j, :], rhs=PTP[:, j * 2048 + c * 512:j * 2048 + (c + 1) * 512],
                        start=True, stop=True),
                      reads=["POOLW", f"PTP{j}"] + [f"POOLW{g}" for g in range(4)], writes=[f"PA{pa}"])
                    A("dve", lambda e, pa=pa, j=j, c=c: e.scalar_tensor_tensor(
                        out=MIXT[:, j, c * 512:(c + 1) * 512], in0=PA[pa][:, :], scalar=PSCL[:, j:j + 1],
                        in1=MIXT[:, j, c * 512:(c + 1) * 512], op0=ALU.mult, op1=ALU.mult),
                      reads=[f"PA{pa}", "PSCL", f"MIXT{j}"], writes=[f"MIXT{j}"])
            out_proj(l, 0, 2)

            if lvl < 3:
                S.barrier()
                continue
            def qk_proj(c_q, c_k):
                bq = load_w(l, [(c_q, 384, 0)])
                for p in range(3):
                    def ev(c, ps_ap, key, p=p):
                        copy_alt(QT[:, p, c * 512:(c + 1) * 512], ps_ap[:, :], [key], [f"QT{p}"], scale=0.125)
                    proj_cm(bq, p * 128, 128, ev)
                bk = load_w(l, [(c_k, 384, 0)])
                for p in range(3):
                    def ev(c, ps_ap, key, p=p):
                        copy_alt(KT[:, p, c * 512:(c + 1) * 512], ps_ap[:, :], [key], [f"KT{p}"])
                    proj_cm(bk, p * 128, 128, ev)

            def gate_proj(c_g):
                bg = load_w(l, [(c_g, 384, 0)])
                for p in range(3):
                    def ev(c, ps_ap, key, p=p):
                        A("act", lambda e: e.activation(out=MIXT[:, p, c * 512:(c + 1) * 512], in_=ps_ap[:, :],
                                                        func=AF.Silu), reads=[key], writes=[f"MIXT{p}"])
                    proj_cm(bg, p * 128, 128, ev)

            if lvl >= 3.01:
                qk_proj(C_FQ, C_FK)
            if lvl < 3.02:
                S.barrier()
                continue
            gate_proj(C_FG)
            if lvl < 3.03:
                S.barrier()
                continue
            bv = load_w(l, [(C_FV, 384, 0)])
            A("pool", lambda e, l=l: e.dma_start(
                out=WFF[:, :, :],
                in_=win_d[l].rearrange("(k p) c -> p k c", p=128)[:, :, IN_COLS - 128:IN_COLS]),
              writes=["WFF"], dma=True, sem_key="WFF")

            def ev_fv(i, ps_ap, key):
                A("dve", lambda e: e.tensor_copy(out=VT[:, i, :], in_=ps_ap[:, 0:384]), reads=[key], writes=["VT"])
                pa = nxt("pa", 2)
                for k in range(8):
                    A("pe", lambda e, pa=pa, k=k: e.matmul(PA[pa][:, 0:6], lhsT=ht(i, k), rhs=WFF[:, k, 122:128],
                                                           start=(k == 0), stop=(k == 7)),
                      reads=["WFF", f"HT{i}"], writes=[f"PA{pa}"])
                A("dve", lambda e, pa=pa: e.tensor_tensor(out=FF[:, i, :], in0=PA[pa][:, 0:6], in1=FBF[:], op=ALU.add),
                  reads=[f"PA{pa}", "FBF"], writes=["FF"])
            proj_tm(bv, 384, ev_fv)
            if lvl < 3.1:
                S.barrier()
                continue
            FFf = FF[:, :, :].rearrange("p i h -> p (i h)")
            A("act", lambda e: e.activation(out=FFf, in_=FFf, func=AF.Exp, scale=-1.0), reads=["FF"], writes=["FF"])
            A("act", lambda e: e.activation(out=FFf, in_=FFf, func=AF.Ln, bias=1.0, scale=1.0),
              reads=["FF"], writes=["FF"])
            pa = nxt("pa", 2)
            A("pe", lambda e, pa=pa: e.matmul(PA[pa][:, 0:96], lhsT=Utri[:], rhs=FFf, start=True, stop=True),
              reads=["Utri", "FF"], writes=[f"PA{pa}"])
            pa2 = nxt("pa", 2)
            A("pe", lambda e, pa2=pa2: e.matmul(PA[pa2][:, 0:96], lhsT=onesf[:], rhs=FFf, start=True, stop=True),
              reads=["onesf", "FF"], writes=[f"PA{pa2}"])
            A("dve", lambda e, pa2=pa2: e.tensor_copy(out=TOT[:, :, :].rearrange("p i h -> p (i h)"),
                                                      in_=PA[pa2][:, 0:96]), reads=[f"PA{pa2}"], writes=["TOT"])
            for i in range(NT):
                A("dve", lambda e, i=i: e.tensor_tensor(out=OFFS[:, i + 1, :], in0=OFFS[:, i, :], in1=TOT[:, i, :],
                                                        op=ALU.add), reads=["TOT", "OFFS"], writes=["OFFS"])
            A("dve", lambda e, pa=pa: e.tensor_tensor(
                out=GG[:, :, :].rearrange("p i h -> p (i h)"), in0=PA[pa][:, 0:96],
                in1=OFFS[:, 0:NT, :].rearrange("p i h -> p (i h)"), op=ALU.add),
              reads=[f"PA{pa}", "OFFS"], writes=["GG"])
            for i in range(1, NT, 2):
                A("dve", lambda e, i=i: e.tensor_tensor(
                    out=BIASALL[:, i * 96:(i + 1) * 96].rearrange("p (j h) -> p j h", j=16),
                    in0=GG[:, :, :],
                    in1=OFFS[:, i + 1:i + 2, :].to_broadcast([128, 16, 6]), op=ALU.subtract),
                  reads=["GG", "OFFS"], writes=["BIASALL"])
            if lvl < 3.2:
                S.barrier()
                continue
            attention("fox")
            out_proj(l, 640, 3)

            if lvl < 4:
                S.barrier()
                continue
            S.barrier()
            qk_proj(C_DQ, C_DK)
            gate_proj(C_DG)
            bi = load_w(l, [(C_IQ, 256, 0)])
            A("pool", lambda e, l=l: e.dma_start(
                out=WFF[:, :, :],
                in_=win_d[l].rearrange("(k p) c -> p k c", p=128)[:, :, C_IK:C_IK + 128]),
              writes=["WFF"], dma=True, sem_key="WFF")
            for rr in range(3):
                A("dve", lambda e, rr=rr, bi=bi: e.tensor_copy(out=WB[bi][:, :, 256 + 32 * rr:288 + 32 * rr],
                                                               in_=WFF[:, :, 0:32]),
                  reads=["WFF"], writes=[f"WB{bi}"])
            for ti, (c0, m) in enumerate(((0, 96), (96, 96), (192, 64))):
                def ev(c, ps_ap, key, ti=ti, m=m):
                    copy_alt(IQT[0:m, ti * 2048 + c * 512:ti * 2048 + (c + 1) * 512], ps_ap[0:m, :], [key], ["IQT"])
                proj_cm(bi, c0, m, ev)

            def ev_ik(c, ps_ap, key):
                copy_alt(IKT[0:96, c * 512:(c + 1) * 512], ps_ap[0:96, :], [key], ["IKT"])
            proj_cm(bi, 256, 96, ev_ik)
            bv = load_w(l, [(C_DV, 384, 0)])

            def ev_dv(i, ps_ap, key):
                A("dve", lambda e: e.tensor_copy(out=VT[:, i, :], in_=ps_ap[:, 0:384]), reads=[key], writes=["VT"])
                pa = nxt("pa", 2)
                for k in range(8):
                    A("pe", lambda e, pa=pa, k=k: e.matmul(PA[pa][:, 0:8], lhsT=ht(i, k), rhs=WFF[:, k, 32:40],
                                                           start=(k == 0), stop=(k == 7)),
                      reads=["WFF", f"HT{i}"], writes=[f"PA{pa}"])
                A("dve", lambda e, pa=pa: e.tensor_scalar(out=IW[:, i, :], in0=PA[pa][:, 0:8], scalar1=8.0 ** -0.5,
                                                          scalar2=None, op0=ALU.mult), reads=[f"PA{pa}"], writes=["IW"])
            proj_tm(bv, 384, ev_dv)
            S.barrier()
            if lvl < 4.1:
                continue
            attention("dsa")
            out_proj(l, 256, 3)
            S.barrier()

        A("sp", lambda e: e.dma_start(out=GB, in_=fg_d[None, :].to_broadcast([128, D])),
          writes=["GB"], dma=True, sem_key="GB")
        rms_stats()
        for i in range(NT):
            yb = i % 2
            A("dve", lambda e, i=i, yb=yb: e.scalar_tensor_tensor(
                out=YST[yb], in0=X[:, i, :], scalar=RSTD[:, i:i + 1], in1=GB, op0=ALU.mult, op1=ALU.mult),
              reads=[f"X{i}", "RSTD", "GB"], writes=[f"YST{yb}"])
            A("sp", lambda e, s=s, i=i, yb=yb: e.dma_start(out=y_d[s, i * 128:(i + 1) * 128, :], in_=YST[yb]),
              reads=[f"YST{yb}"], writes=[], dma=True, sem_key=f"YST{yb}")
        S.barrier()
    S.barrier()
    S.emit(nc)
    es.close()
    return nc


_NC_CACHE = {}


def kernel(x, w_in, norm_g, pool_w, pool_scale, fox_bf, w_out, final_g):
    n_cores = 8
    x = np.ascontiguousarray(np.asarray(x, dtype=np.float32))
    per = x.shape[0] // n_cores
    if "nc" not in _NC_CACHE:
        _NC_CACHE["nc"] = build(n_seq=per, n_layers=4)
    nc = _NC_CACHE["nc"]
    shared = {
        "w_in": np.ascontiguousarray(np.asarray(w_in, dtype=np.float32)),
        "norm_g": np.ascontiguousarray(np.asarray(norm_g, dtype=np.float32)),
        "pool_w": np.ascontiguousarray(np.asarray(pool_w, dtype=np.float32)),
        "pool_scale": np.ascontiguousarray(np.asarray(pool_scale, dtype=np.float32)),
        "fox_bf": np.ascontiguousarray(np.asarray(fox_bf, dtype=np.float32)),
        "w_out": np.ascontiguousarray(np.asarray(w_out, dtype=np.float32)),
        "final_g": np.ascontiguousarray(np.asarray(final_g, dtype=np.float32)),
    }
    in_maps = []
    for c in range(n_cores):
        m = dict(shared)
        m["x"] = np.ascontiguousarray(x[c * per:(c + 1) * per])
        in_maps.append(m)
    res = run_bass_kernel_spmd(nc, in_maps, core_ids=list(range(n_cores)))
    return np.concatenate([np.asarray(r["y"]) for r in res.results], axis=0).astype(np.float32)
```

```python
import contextlib
import numpy as np
import concourse.bass as bass
import concourse.mybir as mybir
from concourse.bass_utils import run_bass_kernel_spmd

F32 = mybir.dt.float32
BF16 = mybir.dt.bfloat16
ALU = mybir.AluOpType
AF = mybir.ActivationFunctionType
AX = mybir.AxisListType

L_SEQ = 2048
D = 1024
NT = 16
IN_COLS = 3886
NEG = -30000.0
NSTEP = 14
TOPK = 256

C_PV, C_PG, C_DQ, C_DK, C_DV, C_DG = 0, 256, 512, 896, 1280, 1664
C_IQ, C_IK, C_IW, C_FQ, C_FK, C_FV, C_FG, C_FF = 2048, 2304, 2336, 2344, 2728, 3112, 3496, 3880

SEM_SPAN = 3000
SAME_ENG_WINDOW = 5


class Op:
    __slots__ = ("id", "eng", "fn", "deps", "dma", "sem_key", "dcount", "gsig",
                 "eidx", "signals")

    def __init__(self, id, eng, fn, dma, sem_key):
        self.id = id
        self.eng = eng
        self.fn = fn
        self.deps = set()
        self.dma = dma
        self.sem_key = sem_key
        self.dcount = 0
        self.gsig = 0
        self.eidx = 0
        self.signals = False


class Sched:
    ENGS = ("pe", "act", "dve", "pool", "sp")

    def __init__(self):
        self.ops = []
        self.last_writer = {}
        self.readers = {}
        self.eng_count = {e: 0 for e in self.ENGS}
        self.dma_counts = {}
        self.last_dma = {}

    def add(self, eng, fn, reads=(), writes=(), dma=False, sem_key=None, extra_deps=()):
        op = Op(len(self.ops), eng, fn, dma, sem_key)
        if dma:
            assert sem_key is not None
            self.dma_counts[sem_key] = self.dma_counts.get(sem_key, 0) + 1
            op.dcount = self.dma_counts[sem_key]
            self.last_dma[sem_key] = op.id
        op.eidx = self.eng_count[eng]
        self.eng_count[eng] += 1
        for k in list(reads) + list(writes):
            w = self.last_writer.get(k)
            if w is not None:
                op.deps.add(w)
        for k in writes:
            for r in self.readers.get(k, ()):
                op.deps.add(r)
        for k in reads:
            self.readers.setdefault(k, []).append(op.id)
        for k in writes:
            self.last_writer[k] = op.id
            self.readers[k] = []
        for d in extra_deps:
            op.deps.add(d)
        op.deps.discard(op.id)
        self.ops.append(op)
        return op

    def barrier(self):
        last = {}
        for op in self.ops:
            if not op.dma and op.fn is not None:
                last[op.eng] = op.id
        deps = list(last.values()) + list(self.last_dma.values())
        for e in self.ENGS:
            self.add(e, None, extra_deps=deps)

    def emit(self, nc):
        ops = self.ops
        for op in ops:
            for d in op.deps:
                p = ops[d]
                if p.dma or p.fn is None:
                    continue
                if p.eng == op.eng:
                    if op.eng == "pe":
                        continue
                    if op.eidx - p.eidx > SAME_ENG_WINDOW:
                        continue
                p.signals = True
        gs = {e: 0 for e in self.ENGS}
        for op in ops:
            if op.signals and not op.dma:
                gs[op.eng] += 1
                op.gsig = gs[op.eng]
        stack = []
        sems = {}
        for e in self.ENGS:
            for i in range((gs[e] + SEM_SPAN - 1) // SEM_SPAN):
                cm = nc.semaphore(f"s_{e}_{i}")
                sems[(e, i)] = cm.__enter__()
                stack.append(cm)
        dsems = {}
        for k in self.dma_counts:
            cm = nc.semaphore(f"d_{len(dsems)}")
            dsems[k] = cm.__enter__()
            stack.append(cm)
        self.n_sems = len(stack)
        per_eng = {e: [op for op in ops if op.eng == e] for e in self.ENGS}

        def run_engine(e, eng):
            waited = {x: 0 for x in self.ENGS}
            dwaited = {}
            for op in per_eng[e]:
                need = {}
                dneed = {}
                for d in op.deps:
                    p = ops[d]
                    if p.fn is None:
                        continue
                    if p.dma:
                        if dwaited.get(p.sem_key, 0) < p.dcount:
                            dneed[p.sem_key] = max(dneed.get(p.sem_key, 0), p.dcount)
                        continue
                    if p.eng == e:
                        if e == "pe" or op.eidx - p.eidx > SAME_ENG_WINDOW:
                            continue
                    if p.gsig > waited[p.eng]:
                        need[p.eng] = max(need.get(p.eng, 0), p.gsig)
                for pe_, g in need.items():
                    si = (g - 1) // SEM_SPAN
                    eng.wait_ge(sems[(pe_, si)], g - si * SEM_SPAN)
                    waited[pe_] = g
                for k, c in dneed.items():
                    eng.wait_ge(dsems[k], 16 * c)
                    dwaited[k] = c
                if op.fn is None:
                    continue
                ins = op.fn(eng)
                if op.dma:
                    ins.then_inc(dsems[op.sem_key], 16)
                elif op.signals:
                    si = (op.gsig - 1) // SEM_SPAN
                    ins.then_inc(sems[(op.eng, si)], 1)

        with nc.Block() as block:
            @block.tensor
            def _(eng):
                run_engine("pe", eng)

            @block.scalar
            def _(eng):
                run_engine("act", eng)

            @block.vector
            def _(eng):
                run_engine("dve", eng)

            @block.gpsimd
            def _(eng):
                run_engine("pool", eng)

            @block.sync
            def _(eng):
                run_engine("sp", eng)
        for cm in reversed(stack):
            cm.__exit__(None, None, None)


def build(n_seq=2, n_layers=4, lvl=9):
    nc = bass.Bass("TRN2", target_bir_lowering=False)
    x_d = nc.dram_tensor("x", [n_seq, L_SEQ, D], F32, kind="ExternalInput").ap()
    win_d = nc.dram_tensor("w_in", [4, D, IN_COLS], F32, kind="ExternalInput").ap()
    ng_d = nc.dram_tensor("norm_g", [4, D], F32, kind="ExternalInput").ap()
    pw_d = nc.dram_tensor("pool_w", [4, 4, 64, 64], F32, kind="ExternalInput").ap()
    psc_d = nc.dram_tensor("pool_scale", [4, 256], F32, kind="ExternalInput").ap()
    fbf_d = nc.dram_tensor("fox_bf", [4, 6], F32, kind="ExternalInput").ap()
    wout_d = nc.dram_tensor("w_out", [4, D, D], F32, kind="ExternalInput").ap()
    fg_d = nc.dram_tensor("final_g", [D], F32, kind="ExternalInput").ap()
    y_d = nc.dram_tensor("y", [n_seq, L_SEQ, D], F32, kind="ExternalOutput").ap()

    S = Sched()
    A = S.add
    es = contextlib.ExitStack()

    def sb(name, shape, dt):
        return es.enter_context(nc.sbuf_tensor(name, shape, dt))

    def pst(name, shape, dt):
        return es.enter_context(nc.psum_tensor(name, shape, dt))

    X = sb("X", [128, NT, D], F32)
    HT = sb("HT", [128, NT * 8 * 128], BF16)
    MIXT = sb("MIXT", [128, 3, L_SEQ], BF16)
    QT = sb("QT", [128, 3, L_SEQ], BF16)
    KT = sb("KT", [128, 3, L_SEQ], BF16)
    VT = sb("VT", [128, NT, 384], BF16)
    WBT = sb("WBT", [128, 2, 8, 392], BF16)
    WB = [WBT[:, 0, :, :], WBT[:, 1, :, :]]
    WOB = sb("WOB", [128, 3, D], BF16)
    WFF = sb("WFF", [128, 8, 128], BF16)
    SCR = sb("SCR", [128, 24576], mybir.dt.uint8)
    PT = [sb(f"PT{i}", [128, 512], BF16) for i in range(4)]
    RB = [sb(f"RB{i}", [128, 512], BF16) for i in range(3)]
    RD = sb("RD", [128, 512], F32)
    TMPF = sb("TMPF", [128, 512], F32)
    ident = sb("ident", [128, 128], BF16)
    tri = sb("tri", [128, 128], BF16)
    Utri = sb("Utri", [128, 128], F32)
    onesf = sb("onesf", [128, 128], F32)
    ones64 = sb("ones64", [128, 64], BF16)
    dmaskf = sb("dmaskf", [128, 128], F32)
    dmaskb = sb("dmaskb", [128, 128], BF16)
    dmaskg = sb("dmaskg", [128, 128], BF16)
    halves = sb("halves", [128, NSTEP + 1], F32)
    invw = sb("invw", [128, 2], F32)
    invcnt = sb("invcnt", [128, 2, 16], F32)
    SS = sb("SS", [128, NT], F32)
    RSTD = sb("RSTD", [128, NT], F32)
    PSCL = sb("PSCL", [128, 2], F32)
    POOLW = sb("POOLW", [128, 2, 128], BF16)
    FBF = sb("FBF", [128, 6], F32)
    IW = sb("IW", [128, NT, 8], F32)
    FF = sb("FF", [128, NT, 6], F32)
    TOT = sb("TOT", [128, NT, 6], F32)
    OFFS = sb("OFFS", [128, NT + 1, 6], F32)
    GG = sb("GG", [128, NT, 6], F32)
    DW = sb("DW", [128, 8, 128], BF16)
    BSS = [sb("BS0", [128, 8 + NSTEP + 1], F32), sb("BS1", [128, 8 + NSTEP + 1], F32)]

    def view(t, dt, byte_off, n_elem):
        ap = t[:, :]
        apb = ap.bitcast(dt)
        esz = mybir.dt.size(dt)
        tsz = mybir.dt.size(t.dtype)
        e0 = byte_off // esz
        return apb[:, e0:e0 + n_elem]

    HB = [view(SCR, BF16, 16384, 1024), view(SCR, BF16, 18432, 1024)]
    GB = view(SCR, F32, 20480, 1024)
    JUNKN = view(SCR, BF16, 8192, 1024)
    VBUF = view(SCR, F32, 0, 528)
    T1 = view(SCR, F32, 2112, 528)
    T2 = view(SCR, F32, 4224, 528)
    PTP = view(SCR, BF16, 8192, 4096)
    BIASALL = view(SCR, F32, 16384, 16 * 16 * 6)
    IQT = view(SCR, BF16, 0, 3 * 2048)
    IKT = view(SCR, BF16, 12288, 2048)
    SCORES = [view(SCR, F32, 16384, 2048), WBT[:, :, :, :].rearrange('p a k c -> p (a k c)').bitcast(F32)[:, 0:2048]]
    YST = [view(HT, F32, 0, 1024), view(HT, F32, 4096, 1024)]

    def ht(i, k):
        o = (i * 8 + k) * 128
        return HT[:, o:o + 128]

    HT4 = HT[:, :].rearrange("p (i k t) -> p i k t", i=NT, k=8)

    PTR = pst("PTR", [128, 1024], BF16)
    PA = [pst("PA0", [128, 512], F32), pst("PA1", [128, 512], F32)]
    PS = [pst("PS0", [128, 512], F32), pst("PS1", [128, 512], F32)]
    PO = [pst("PO0", [128, 512], F32), pst("PO1", [128, 512], F32)]
    PD = [pst("PD0", [128, 512], F32)]
    rot = {"pa": 0, "ps": 0, "po": 0, "pt": 0, "rb": 0, "wb": 0, "hb": 0}

    def nxt(name, n):
        v = rot[name]
        rot[name] = (v + 1) % n
        return v

    A("pool", lambda e: e.memset(TMPF[:, 0:128], 0.0), writes=["TMPF"])
    A("pool", lambda e: e.affine_select(out=TMPF[:, 0:128], in_=TMPF[:, 0:128], pattern=[[-1, 128]],
                                        compare_op=ALU.not_equal, fill=1.0, base=0,
                                        channel_multiplier=1), reads=["TMPF"], writes=["TMPF"])
    A("dve", lambda e: e.tensor_copy(out=ident[:], in_=TMPF[:, 0:128]), reads=["TMPF"], writes=["ident"])
    A("pool", lambda e: e.memset(TMPF[:, 0:128], 0.0), writes=["TMPF"])
    A("pool", lambda e: e.affine_select(out=TMPF[:, 0:128], in_=TMPF[:, 0:128], pattern=[[1, 128]],
                                        compare_op=ALU.is_ge, fill=NEG, base=0,
                                        channel_multiplier=-1), reads=["TMPF"], writes=["TMPF"])
    A("dve", lambda e: e.tensor_copy(out=tri[:], in_=TMPF[:, 0:128]), reads=["TMPF"], writes=["tri"])
    A("pool", lambda e: e.memset(Utri[:], 1.0), writes=["Utri"])
    A("pool", lambda e: e.affine_select(out=Utri[:], in_=Utri[:], pattern=[[1, 128]],
                                        compare_op=ALU.is_ge, fill=0.0, base=0,
                                        channel_multiplier=-1), reads=["Utri"], writes=["Utri"])
    A("pool", lambda e: e.memset(onesf[:], 1.0), writes=["onesf"])
    A("pool", lambda e: e.memset(ones64[:], 1.0), writes=["ones64"])
    A("pool", lambda e: e.memset(dmaskf[:], 0.0), writes=["dmaskf"])
    A("pool", lambda e: e.memset(dmaskf[0:64, 64:128], -1.0e30), reads=["dmaskf"], writes=["dmaskf"])
    A("pool", lambda e: e.memset(dmaskg[:], 0.0), writes=["dmaskg"])
    A("pool", lambda e: e.memset(dmaskg[0:64, 64:128], -1.0e30), reads=["dmaskg"], writes=["dmaskg"])
    A("pool", lambda e: e.memset(dmaskb[:], 0.0), writes=["dmaskb"])
    A("pool", lambda e: e.memset(dmaskb[0:64, 64:128], NEG), reads=["dmaskb"], writes=["dmaskb"])
    for k in range(NSTEP + 1):
        A("pool", lambda e, k=k: e.memset(halves[:, k:k + 1], 0.5 ** (k + 1)), writes=["halves"])
    for j, (wa, wb_) in enumerate(((2, 4), (8, 16))):
        A("pool", lambda e, j=j, wa=wa: e.memset(invw[0:64, j:j + 1], 1.0 / wa), writes=["invw"])
        A("pool", lambda e, j=j, wb_=wb_: e.memset(invw[64:128, j:j + 1], 1.0 / wb_), writes=["invw"])
        for t in range(16):
            A("pool", lambda e, j=j, t=t, wa=wa: e.memset(invcnt[0:64, j, t:t + 1], 1.0 / min(t + 1, wa)),
              writes=["invcnt"])
            A("pool", lambda e, j=j, t=t, wb_=wb_: e.memset(invcnt[64:128, j, t:t + 1], 1.0 / min(t + 1, wb_)),
              writes=["invcnt"])
    A("pool", lambda e: e.memset(OFFS[:, 0, :], 0.0), writes=["OFFS"])

    def load_w(l, pieces):
        b = nxt("wb", 2)
        for (c0, n, s0) in pieces:
            A("pool", lambda e, b=b, c0=c0, n=n, s0=s0: e.dma_start(
                out=WB[b][:, :, s0:s0 + n],
                in_=win_d[l].rearrange("(k p) c -> p k c", p=128)[:, :, c0:c0 + n]),
              writes=[f"WB{b}"], dma=True, sem_key=f"WB{b}")
        return b

    def proj_cm(b, col0, M, evac):
        for c in range(4):
            pa = nxt("pa", 2)
            for k in range(8):
                A("pe", lambda e, pa=pa, k=k, c=c: e.matmul(
                    PA[pa][0:M, :], lhsT=WB[b][:, k, col0:col0 + M],
                    rhs=HT4[:, 4 * c:4 * c + 4, k, :], start=(k == 0), stop=(k == 7)),
                  reads=[f"WB{b}"] + [f"HT{i}" for i in range(4 * c, 4 * c + 4)], writes=[f"PA{pa}"])
            evac(c, PA[pa], f"PA{pa}")

    def proj_tm(b, N, evac):
        for i in range(NT):
            pa = nxt("pa", 2)
            for k in range(8):
                A("pe", lambda e, pa=pa, k=k, i=i: e.matmul(
                    PA[pa][:, 0:N], lhsT=ht(i, k), rhs=WB[b][:, k, 0:N],
                    start=(k == 0), stop=(k == 7)),
                  reads=[f"WB{b}", f"HT{i}"], writes=[f"PA{pa}"])
            evac(i, PA[pa], f"PA{pa}")

    cnt = {"alt": 0}

    def copy_alt(out, in_, reads, writes, scale=None):
        cnt["alt"] += 1
        if cnt["alt"] % 2 == 0:
            if scale is None:
                A("act", lambda e: e.activation(out=out, in_=in_, func=AF.Copy), reads=reads, writes=writes)
            else:
                A("act", lambda e: e.activation(out=out, in_=in_, func=AF.Copy, scale=scale),
                  reads=reads, writes=writes)
        else:
            if scale is None:
                A("dve", lambda e: e.tensor_copy(out=out, in_=in_), reads=reads, writes=writes)
            else:
                A("dve", lambda e: e.tensor_scalar(out=out, in0=in_, scalar1=scale, scalar2=None,
                                                   op0=ALU.mult), reads=reads, writes=writes)

    def out_proj(l, row0, nch):
        A("pool", lambda e: e.dma_start(
            out=WOB[:, 0:nch, :],
            in_=wout_d[l, row0:row0 + nch * 128, :].rearrange("(q p) c -> p q c", p=128)),
          writes=["WOB"], dma=True, sem_key="WOB")
        for i in range(NT):
            for n in range(2):
                pa = nxt("pa", 2)
                for q in range(nch):
                    A("pe", lambda e, pa=pa, q=q, i=i, n=n: e.matmul(
                        PA[pa][:, :], lhsT=MIXT[:, q, i * 128:(i + 1) * 128],
                        rhs=WOB[:, q, n * 512:(n + 1) * 512], start=(q == 0), stop=(q == nch - 1)),
                      reads=["WOB", f"MIXT{q}"], writes=[f"PA{pa}"])
                A("dve", lambda e, pa=pa, i=i, n=n: e.tensor_tensor(
                    out=X[:, i, n * 512:(n + 1) * 512], in0=X[:, i, n * 512:(n + 1) * 512],
                    in1=PA[pa][:, :], op=ALU.add),
                  reads=[f"PA{pa}", f"X{i}"], writes=[f"X{i}"])

    def rms_stats():
        for i in range(NT):
            A("act", lambda e, i=i: e.activation(out=JUNKN, in_=X[:, i, :], func=AF.Square,
                                                 accum_out=SS[:, i:i + 1]),
              reads=[f"X{i}"], writes=["JUNKN", "SS"])
        A("act", lambda e: e.activation(out=RSTD[:], in_=SS[:], func=AF.Sqrt, bias=EPS_T[:, 0:1], scale=1.0 / D),
          reads=["SS"], writes=["RSTD"])
        A("dve", lambda e: e.reciprocal(out=RSTD[:], in_=RSTD[:]), reads=["RSTD"], writes=["RSTD"])

    EPS_T = sb("EPS_T", [128, 1], F32)
    A("pool", lambda e: e.memset(EPS_T[:], 1e-6), writes=["EPS_T"])
    C255 = sb("C255", [128, 1], F32)
    A("pool", lambda e: e.memset(C255[:], TOPK - 0.5), writes=["C255"])
    CNEG = sb("CNEG", [128, 1], F32)
    A("pool", lambda e: e.memset(CNEG[:], NEG), writes=["CNEG"])


    def mb_view(c, q):
        base = (c % 2) * 16384
        o = 0
        for qq in range(q):
            o += (4 * c + qq + 1) * 128
        n = (4 * c + q + 1) * 128
        return view(HT, BF16, base + 2 * o, n)

    PSC = PTR[:, :].bitcast(F32)

    def attention(kind):
        if kind == "dsa":
            for slot in range(3):
                mask_jobs(0, slot)
        for c in range(4):
            attn_chunk(kind, c)

    def attn_chunk(kind, c):
        nj = 4 * c + 4
        t0 = 4 * c * 128
        po_of = {}
        pd = 0
        pre = {0: (0, 1), 1: (2,), 2: (3,)}

        def qk(p, j, hh):
            h = 2 * p + hh
            r0 = 64 * hh
            i_lo = max(4 * c, j)
            off = (i_lo - 4 * c) * 128
            ps = nxt("ps", 2)
            psk = f"PS{ps}"
            pt = nxt("pt", 4)
            kslc = KT[r0:r0 + 64, p, j * 128:(j + 1) * 128]
            if kind == "fox":
                if j >= 4 * c:
                    A("pe", lambda e: e.matmul(PS[ps][:, off:off + 128], lhsT=ident[:], rhs=tri[:],
                                               start=True, stop=False),
                      reads=["ident", "tri"], writes=[psk])
                    A("pe", lambda e: e.matmul(PS[ps][:, off:off + 128], lhsT=kslc,
                                               rhs=QT[r0:r0 + 64, p, t0 + off:t0 + off + 128],
                                               start=False, stop=True),
                      reads=[f"KT{p}", f"QT{p}"], writes=[psk])
                    if off + 128 < 512:
                        A("pe", lambda e: e.matmul(PS[ps][:, off + 128:512], lhsT=kslc,
                                                   rhs=QT[r0:r0 + 64, p, t0 + off + 128:t0 + 512],
                                                   start=True, stop=True),
                          reads=[f"KT{p}", f"QT{p}"], writes=[psk])
                else:
                    A("pe", lambda e: e.matmul(PS[ps][:, 0:512], lhsT=kslc, rhs=QT[r0:r0 + 64, p, t0:t0 + 512],
                                               start=True, stop=True),
                      reads=[f"KT{p}", f"QT{p}"], writes=[psk])
                i = i_lo
                while i < 4 * c + 4:
                    wd = 2 if i % 2 == 0 else 1
                    co = (i - 4 * c) * 128
                    bcol = ((i | 1) * 16 + j) * 6 + h
                    A("act", lambda e, co=co, wd=wd, bcol=bcol: e.activation(
                        out=PT[pt][:, co:co + 128 * wd], in_=PS[ps][:, co:co + 128 * wd], func=AF.Exp,
                        bias=BIASALL[:, bcol:bcol + 1], scale=1.0),
                      reads=[psk, "BIASALL"], writes=[f"PT{pt}"])
                    i += wd
            else:
                A("pe", lambda e: e.matmul(
                    PS[ps][:, off:512], lhsT=kslc, rhs=QT[r0:r0 + 64, p, t0 + off:t0 + 512],
                    start=True, stop=False),
                  reads=[f"KT{p}", f"QT{p}"], writes=[psk])
                for i in range(i_lo, 4 * c + 4):
                    co = (i - 4 * c) * 128
                    q = i - 4 * c
                    mbv = mb_view(c, q)
                    A("pe", lambda e, co=co, mbv=mbv: e.matmul(
                        PS[ps][:, co:co + 128], lhsT=mbv[:, j * 128:(j + 1) * 128], rhs=ident[:],
                        start=False, stop=True),
                      reads=[f"MB{c % 2}_{q}", "ident"], writes=[psk])
                A("act", lambda e: e.activation(out=PT[pt][:, off:512], in_=PS[ps][:, off:512], func=AF.Exp),
                  reads=[psk], writes=[f"PT{pt}"])
            return (p, j, hh, pt, off)

        def pv(blk):
            p, j, hh, pt, off = blk
            h = 2 * p + hh
            r0 = 64 * hh
            if p not in po_of:
                po_of[p] = nxt("po", 2)
            po = po_of[p]
            A("pe", lambda e: e.matmul(
                PO[po][r0:r0 + 64, off:512], lhsT=VT[:, j, h * 64:(h + 1) * 64], rhs=PT[pt][:, off:512],
                start=(j == 0), stop=(j == nj - 1), tile_position=(0, r0)),
              reads=[f"PT{pt}", "VT"], writes=[f"PO{po}"])
            A("pe", lambda e: e.matmul(
                PD[pd][r0:r0 + 64, off:512], lhsT=ones64[:], rhs=PT[pt][:, off:512],
                start=(j == 0), stop=(j == nj - 1), tile_position=(0, r0)),
              reads=[f"PT{pt}", "ones64"], writes=[f"PD{pd}"])
            if j == nj - 1 and hh == 1:
                csl = slice(4 * c * 128, (4 * c + 4) * 128)
                A("dve", lambda e: e.reciprocal(out=RD[:], in_=PD[pd][:, :]), reads=[f"PD{pd}"], writes=["RD"])
                A("dve", lambda e: e.tensor_tensor(out=TMPF[:], in0=PO[po][:, :], in1=RD[:], op=ALU.mult),
                  reads=[f"PO{po}", "RD"], writes=["TMPF"])
                A("dve", lambda e: e.tensor_tensor(out=MIXT[:, p, csl], in0=TMPF[:], in1=MIXT[:, p, csl],
                                                   op=ALU.mult),
                  reads=["TMPF", f"MIXT{p}"], writes=[f"MIXT{p}"])

        pend = None
        for p in range(3):
            if kind == "dsa" and c < 3:
                mask_jobs(c + 1, p)
            for j in range(nj):
                for hh in range(2):
                    cur = qk(p, j, hh)
                    if pend is not None:
                        pv(pend)
                    pend = cur
        pv(pend)

    def dsa_idx_tile(c, q):
        i = 4 * c + q
        n = (i + 1) * 128
        mk = f"MB{c % 2}_{q}"
        mbv = mb_view(c, q)
        if i < 2:
            if i == 1:
                A("pool", lambda e: e.memset(mbv[:, 0:128], 0.0), writes=[mk])
            A("pool", lambda e: e.tensor_copy(out=mbv[:, n - 128:n], in_=dmaskb[:]),
              reads=["dmaskb"], writes=[mk])
            return
        SC = SCORES[q % 2]
        sk = f"SCORE{q % 2}"
        for h in range(8):
            A("act", lambda e, h=h: e.activation(out=DW[:, h, :], in_=ident[:], func=AF.Copy,
                                                 scale=IW[:, i, h:h + 1]),
              reads=["ident", "IW"], writes=["DW"])
        nsc = (n + 511) // 512
        for sc in range(nsc):
            w = min(512, n - sc * 512)
            last = (sc == nsc - 1)
            for h in range(8):
                ti, r = h // 3, (h % 3) * 32
                pa = nxt("pa", 2)
                rb = nxt("rb", 3)
                A("pe", lambda e, pa=pa, ti=ti, r=r, w=w, sc=sc: e.matmul(
                    PA[pa][:, 0:w], lhsT=IQT[r:r + 32, ti * 2048 + i * 128:ti * 2048 + (i + 1) * 128],
                    rhs=IKT[r:r + 32, sc * 512:sc * 512 + w], start=True, stop=True),
                  reads=["IQT", "IKT"], writes=[f"PA{pa}"])
                A("act", lambda e, pa=pa, rb=rb, w=w: e.activation(out=RB[rb][:, 0:w], in_=PA[pa][:, 0:w],
                                                                   func=AF.Relu),
                  reads=[f"PA{pa}"], writes=[f"RB{rb}"])
                A("pe", lambda e, rb=rb, h=h, w=w, last=last: e.matmul(
                    PSC[:, 0:w], lhsT=DW[:, h, :], rhs=RB[rb][:, 0:w], start=(h == 0),
                    stop=(h == 7 and not last)),
                  reads=["DW", f"RB{rb}"], writes=["PTR"])
            if last:
                A("pe", lambda e, w=w: e.matmul(PSC[:, w - 128:w], lhsT=ident[:], rhs=dmaskg[:],
                                                start=False, stop=True),
                  reads=["ident", "dmaskg"], writes=["PTR"])
            s0 = sc * 512
            A("act", lambda e, s0=s0, w=w: e.activation(out=SC[:, s0:s0 + w], in_=PSC[:, 0:w], func=AF.Copy),
              reads=["PTR"], writes=[sk])

    def dsa_bis_ops(c, q):
        i = 4 * c + q
        if i < 2:
            return []
        n = (i + 1) * 128
        mk = f"MB{c % 2}_{q}"
        mbv = mb_view(c, q)
        SC = SCORES[q % 2]
        sk = f"SCORE{q % 2}"
        BS = BSS[q % 2]
        u = f"b{q % 2}"
        mx, mn, rg, mid, cn, tp = (BS[:, 0:1], BS[:, 1:2], BS[:, 2:3], BS[:, 3:4], BS[:, 4:5], BS[:, 5:6])
        DL = BS[:, 8:8 + NSTEP + 1]
        ops = []
        O = lambda fn, reads, writes: ops.append((fn, reads, writes))
        O(lambda e: e.tensor_reduce(out=mx, in_=SC[:, 0:n], axis=AX.X, op=ALU.max), [sk], [u + "mx"])
        O(lambda e: e.tensor_reduce(out=mn, in_=SC[:, 0:n - 64], axis=AX.X, op=ALU.min), [sk], [u + "mn"])
        O(lambda e: e.tensor_tensor(out=rg, in0=mx, in1=mn, op=ALU.subtract), [u + "mx", u + "mn"], [u + "rg"])
        O(lambda e: e.tensor_scalar(out=DL, in0=halves[:], scalar1=rg, scalar2=None, op0=ALU.mult),
          [u + "rg", "halves"], [u + "dl"])
        O(lambda e: e.tensor_tensor(out=mid, in0=mn, in1=BS[:, 8:9], op=ALU.add), [u + "mn", u + "dl"], [u + "mid"])
        for k in range(NSTEP):
            O(lambda e: e.tensor_scalar(out=mbv[:, 0:n], in0=SC[:, 0:n], scalar1=mid, scalar2=None,
                                        op0=ALU.is_ge, op1=ALU.add, accum_out=cn),
              [sk, u + "mid"], [mk, u + "cn"])
            O(lambda e, k=k: e.tensor_scalar(out=tp, in0=cn, scalar1=C255[:, 0:1], scalar2=BS[:, 8 + k:9 + k],
                                             op0=ALU.is_ge, op1=ALU.mult),
              [u + "cn", u + "dl"], [u + "tp"])
            O(lambda e, k=k: e.scalar_tensor_tensor(out=mid, in0=tp, scalar=BS[:, 9 + k:10 + k], in1=mid,
                                                    op0=ALU.subtract, op1=ALU.add),
              [u + "tp", u + "dl", u + "mid"], [u + "mid"])
        O(lambda e: e.tensor_tensor(out=tp, in0=mid, in1=BS[:, 8 + NSTEP:9 + NSTEP], op=ALU.subtract),
          [u + "mid", u + "dl"], [u + "tp"])
        O(lambda e: e.tensor_scalar(out=mbv[:, 0:n], in0=SC[:, 0:n], scalar1=tp, scalar2=CNEG[:, 0:1],
                                    op0=ALU.is_lt, op1=ALU.mult),
          [sk, u + "tp"], [mk])
        return ops

    def dsa_bis_pair(c, qa, qb):
        la, lb = dsa_bis_ops(c, qa), dsa_bis_ops(c, qb)
        for t in range(max(len(la), len(lb))):
            for lst in (la, lb):
                if t < len(lst):
                    fn, r, w = lst[t]
                    A("dve", fn, reads=r, writes=w)

    def mask_jobs(c, slot):
        if slot == 0:
            dsa_idx_tile(c, 0); dsa_idx_tile(c, 1)
        elif slot == 1:
            dsa_bis_pair(c, 0, 1); dsa_idx_tile(c, 2); dsa_idx_tile(c, 3)
        else:
            dsa_bis_pair(c, 2, 3)

    for s in range(n_seq):
        for qd in range(4):
            A("sp", lambda e, s=s, qd=qd: e.dma_start(
                out=X[:, 4 * qd:4 * qd + 4, :],
                in_=x_d[s].rearrange("(i p) d -> p i d", p=128)[:, 4 * qd:4 * qd + 4, :]),
              writes=[f"X{i}" for i in range(4 * qd, 4 * qd + 4)], dma=True, sem_key=f"XL{qd}")
        for l in range(n_layers):
            if lvl < 1:
                break
            A("sp", lambda e, l=l: e.dma_start(out=GB, in_=ng_d[l:l + 1, :].to_broadcast([128, D])),
              writes=["GB"], dma=True, sem_key="GB")
            for j in range(2):
                A("sp", lambda e, l=l, j=j: e.dma_start(
                    out=PSCL[:, j:j + 1], in_=psc_d[l, j * 128:(j + 1) * 128].rearrange("(p o) -> p o", o=1)),
                  writes=["PSCL"], dma=True, sem_key=f"PSCL{j}")
            A("sp", lambda e, l=l: e.dma_start(out=FBF[:], in_=fbf_d[l:l + 1, :].to_broadcast([128, 6])),
              writes=["FBF"], dma=True, sem_key="FBF")
            A("pool", lambda e: e.memset(POOLW[:], 0.0), writes=["POOLW"])
            for g in range(4):
                j, r0 = g // 2, (g % 2) * 64
                A("pool", lambda e, l=l, g=g, j=j, r0=r0: e.dma_start(
                    out=POOLW[r0:r0 + 64, j, r0:r0 + 64], in_=pw_d[l, g]),
                  reads=[], writes=["POOLW"], dma=True, sem_key=f"POOLW{g}")
            rms_stats()
            for i in range(NT):
                hb = nxt("hb", 2)
                A("dve", lambda e, i=i, hb=hb: e.scalar_tensor_tensor(
                    out=HB[hb], in0=X[:, i, :], scalar=RSTD[:, i:i + 1], in1=GB, op0=ALU.mult, op1=ALU.mult),
                  reads=[f"X{i}", "RSTD", "GB"], writes=[f"HB{hb}"])
                for k in range(8):
                    A("pe", lambda e, hb=hb, k=k: e.transpose(out=PTR[:, k * 128:(k + 1) * 128],
                                                              in_=HB[hb][:, k * 128:(k + 1) * 128],
                                                              identity=ident[:]),
                      reads=[f"HB{hb}", "ident"], writes=["PTR"])
                copy_alt(HT[:, i * 1024:(i + 1) * 1024], PTR[:, :], ["PTR"], [f"HT{i}"])

            if lvl < 2:
                S.barrier()
                continue
            b = load_w(l, [(C_PG, 256, 0)])
            for j in range(2):
                def ev_g(c, ps_ap, key, j=j):
                    A("act", lambda e: e.activation(out=MIXT[:, j, c * 512:(c + 1) * 512], in_=ps_ap[:, :],
                                                    func=AF.Silu), reads=[key], writes=[f"MIXT{j}"])
                proj_cm(b, j * 128, 128, ev_g)
            b = load_w(l, [(C_PV, 256, 0)])
            for j in range(2):
                srcA = (T1, T2)
                def ev_v(c, ps_ap, key, j=j):
                    if c == 0:
                        A("pool", lambda e: e.memset(VBUF[:, 0:16], 0.0), writes=["VB_h"])
                    else:
                        A("pool", lambda e: e.tensor_copy(out=VBUF[:, 0:16], in_=VBUF[:, 512:528]),
                          reads=["VB_m"], writes=["VB_h"])
                    A("act", lambda e: e.activation(out=VBUF[:, 16:528], in_=ps_ap[:, :], func=AF.Copy),
                      reads=[key, "VB_h"], writes=["VB_m"])
                    rk = ["VB_h", "VB_m"]
                    A("pool", lambda e: e.tensor_tensor(out=T1[:, 1:528], in0=VBUF[:, 1:528], in1=VBUF[:, 0:527],
                                                        op=ALU.add), reads=rk, writes=["T1"])
                    if j == 0:
                        A("pool", lambda e: e.tensor_tensor(out=T2[64:128, 3:528], in0=T1[64:128, 3:528],
                                                            in1=T1[64:128, 1:526], op=ALU.add),
                          reads=["T1"], writes=["T2"])
                        fa, fb = T1, T2
                    else:
                        A("pool", lambda e: e.tensor_tensor(out=T2[:, 3:528], in0=T1[:, 3:528], in1=T1[:, 1:526],
                                                            op=ALU.add), reads=["T1"], writes=["T2"])
                        A("pool", lambda e: e.tensor_tensor(out=T1[:, 7:528], in0=T2[:, 7:528], in1=T2[:, 3:524],
                                                            op=ALU.add), reads=["T2"], writes=["T1"])
                        A("pool", lambda e: e.tensor_tensor(out=T2[64:128, 15:528], in0=T1[64:128, 15:528],
                                                            in1=T1[64:128, 7:520], op=ALU.add),
                          reads=["T1"], writes=["T2"])
                        fa, fb = T1, T2
                    o0 = j * 2048 + c * 512
                    for (rs, src) in ((slice(0, 64), fa), (slice(64, 128), fb)):
                        A("pool", lambda e, rs=rs, src=src: e.tensor_scalar(
                            out=TMPF[rs, 0:512], in0=src[rs, 16:528], scalar1=invw[rs, j:j + 1], scalar2=None,
                            op0=ALU.mult),
                          reads=["T1", "T2", "invw"], writes=["TMPF"])
                        A("pool", lambda e, rs=rs: e.tensor_tensor(
                            out=PTP[rs, o0:o0 + 512], in0=TMPF[rs, 0:512], in1=VBUF[rs, 16:528], op=ALU.subtract),
                          reads=["TMPF", "VB_m"], writes=[f"PTP{j}"])
                        if c == 0:
                            A("pool", lambda e, rs=rs, src=src: e.tensor_tensor(
                                out=TMPF[rs, 0:16], in0=src[rs, 16:32], in1=invcnt[rs, j, :], op=ALU.mult),
                              reads=["T1", "T2", "invcnt"], writes=["TMPF"])
                            A("pool", lambda e, rs=rs: e.tensor_tensor(
                                out=PTP[rs, o0:o0 + 16], in0=TMPF[rs, 0:16], in1=VBUF[rs, 16:32], op=ALU.subtract),
                              reads=["TMPF", "VB_m", f"PTP{j}"], writes=[f"PTP{j}"])
                proj_cm(b, j * 128, 128, ev_v)
            for j in range(2):
                for c in range(4):
                    pa = nxt("pa", 2)
                    A("pe", lambda e, pa=pa, j=j, c=c: e.matmul(
                        PA[pa][:, :], lhsT=POOLW[:, j, :], rhs=PTP[:, j * 2048 + c * 512:j * 2048 + (c + 1) * 512],
                        start=True, stop=True),
                      reads=["POOLW", f"PTP{j}"] + [f"POOLW{g}" for g in range(4)], writes=[f"PA{pa}"])
                    A("dve", lambda e, pa=pa, j=j, c=c: e.scalar_tensor_tensor(
                        out=MIXT[:, j, c * 512:(c + 1) * 512], in0=PA[pa][:, :], scalar=PSCL[:, j:j + 1],
                        in1=MIXT[:, j, c * 512:(c + 1) * 512], op0=ALU.mult, op1=ALU.mult),
                      reads=[f"PA{pa}", "PSCL", f"MIXT{j}"], writes=[f"MIXT{j}"])
            out_proj(l, 0, 2)

            if lvl < 3:
                S.barrier()
                continue
            def qk_proj(c_q, c_k):
                bq = load_w(l, [(c_q, 384, 0)])
                for p in range(3):
                    def ev(c, ps_ap, key, p=p):
                        copy_alt(QT[:, p, c * 512:(c + 1) * 512], ps_ap[:, :], [key], [f"QT{p}"], scale=0.125)
                    proj_cm(bq, p * 128, 128, ev)
                bk = load_w(l, [(c_k, 384, 0)])
                for p in range(3):
                    def ev(c, ps_ap, key, p=p):
                        copy_alt(KT[:, p, c * 512:(c + 1) * 512], ps_ap[:, :], [key], [f"KT{p}"])
                    proj_cm(bk, p * 128, 128, ev)

            def gate_proj(c_g):
                bg = load_w(l, [(c_g, 384, 0)])
                for p in range(3):
                    def ev(c, ps_ap, key, p=p):
                        A("act", lambda e: e.activation(out=MIXT[:, p, c * 512:(c + 1) * 512], in_=ps_ap[:, :],
                                                        func=AF.Silu), reads=[key], writes=[f"MIXT{p}"])
                    proj_cm(bg, p * 128, 128, ev)

            if lvl >= 3.01:
                qk_proj(C_FQ, C_FK)
            if lvl < 3.02:
                S.barrier()
                continue
            gate_proj(C_FG)
            if lvl < 3.03:
                S.barrier()
                continue
            bv = load_w(l, [(C_FV, 384, 0)])
            A("pool", lambda e, l=l: e.dma_start(
                out=WFF[:, :, :],
                in_=win_d[l].rearrange("(k p) c -> p k c", p=128)[:, :, IN_COLS - 128:IN_COLS]),
              writes=["WFF"], dma=True, sem_key="WFF")

            def ev_fv(i, ps_ap, key):
                A("dve", lambda e: e.tensor_copy(out=VT[:, i, :], in_=ps_ap[:, 0:384]), reads=[key], writes=["VT"])
                pa = nxt("pa", 2)
                for k in range(8):
                    A("pe", lambda e, pa=pa, k=k: e.matmul(PA[pa][:, 0:6], lhsT=ht(i, k), rhs=WFF[:, k, 122:128],
                                                           start=(k == 0), stop=(k == 7)),
                      reads=["WFF", f"HT{i}"], writes=[f"PA{pa}"])
                A("dve", lambda e, pa=pa: e.tensor_tensor(out=FF[:, i, :], in0=PA[pa][:, 0:6], in1=FBF[:], op=ALU.add),
                  reads=[f"PA{pa}", "FBF"], writes=["FF"])
            proj_tm(bv, 384, ev_fv)
            if lvl < 3.1:
                S.barrier()
                continue
            FFf = FF[:, :, :].rearrange("p i h -> p (i h)")
            A("act", lambda e: e.activation(out=FFf, in_=FFf, func=AF.Exp, scale=-1.0), reads=["FF"], writes=["FF"])
            A("act", lambda e: e.activation(out=FFf, in_=FFf, func=AF.Ln, bias=1.0, scale=1.0),
              reads=["FF"], writes=["FF"])
            pa = nxt("pa", 2)
            A("pe", lambda e, pa=pa: e.matmul(PA[pa][:, 0:96], lhsT=Utri[:], rhs=FFf, start=True, stop=True),
              reads=["Utri", "FF"], writes=[f"PA{pa}"])
            pa2 = nxt("pa", 2)
            A("pe", lambda e, pa2=pa2: e.matmul(PA[pa2][:, 0:96], lhsT=onesf[:], rhs=FFf, start=True, stop=True),
              reads=["onesf", "FF"], writes=[f"PA{pa2}"])
            A("dve", lambda e, pa2=pa2: e.tensor_copy(out=TOT[:, :, :].rearrange("p i h -> p (i h)"),
                                                      in_=PA[pa2][:, 0:96]), reads=[f"PA{pa2}"], writes=["TOT"])
            for i in range(NT):
                A("dve", lambda e, i=i: e.tensor_tensor(out=OFFS[:, i + 1, :], in0=OFFS[:, i, :], in1=TOT[:, i, :],
                                                        op=ALU.add), reads=["TOT", "OFFS"], writes=["OFFS"])
            A("dve", lambda e, pa=pa: e.tensor_tensor(
                out=GG[:, :, :].rearrange("p i h -> p (i h)"), in0=PA[pa][:, 0:96],
                in1=OFFS[:, 0:NT, :].rearrange("p i h -> p (i h)"), op=ALU.add),
              reads=[f"PA{pa}", "OFFS"], writes=["GG"])
            for i in range(1, NT, 2):
                A("dve", lambda e, i=i: e.tensor_tensor(
                    out=BIASALL[:, i * 96:(i + 1) * 96].rearrange("p (j h) -> p j h", j=16),
                    in0=GG[:, :, :],
                    in1=OFFS[:, i + 1:i + 2, :].to_broadcast([128, 16, 6]), op=ALU.subtract),
                  reads=["GG", "OFFS"], writes=["BIASALL"])
            if lvl < 3.2:
                S.barrier()
                continue
            attention("fox")
            out_proj(l, 640, 3)

            if lvl < 4:
                S.barrier()
                continue
            S.barrier()
            qk_proj(C_DQ, C_DK)
            gate_proj(C_DG)
            bi = load_w(l, [(C_IQ, 256, 0)])
            A("pool", lambda e, l=l: e.dma_start(
                out=WFF[:, :, :],
                in_=win_d[l].rearrange("(k p) c -> p k c", p=128)[:, :, C_IK:C_IK + 128]),
              writes=["WFF"], dma=True, sem_key="WFF")
            for rr in range(3):
                A("dve", lambda e, rr=rr, bi=bi: e.tensor_copy(out=WB[bi][:, :, 256 + 32 * rr:288 + 32 * rr],
                                                               in_=WFF[:, :, 0:32]),
                  reads=["WFF"], writes=[f"WB{bi}"])
            for ti, (c0, m) in enumerate(((0, 96), (96, 96), (192, 64))):
                def ev(c, ps_ap, key, ti=ti, m=m):
                    copy_alt(IQT[0:m, ti * 2048 + c * 512:ti * 2048 + (c + 1) * 512], ps_ap[0:m, :], [key], ["IQT"])
                proj_cm(bi, c0, m, ev)

            def ev_ik(c, ps_ap, key):
                copy_alt(IKT[0:96, c * 512:(c + 1) * 512], ps_ap[0:96, :], [key], ["IKT"])
            proj_cm(bi, 256, 96, ev_ik)
            bv = load_w(l, [(C_DV, 384, 0)])

            def ev_dv(i, ps_ap, key):
                A("dve", lambda e: e.tensor_copy(out=VT[:, i, :], in_=ps_ap[:, 0:384]), reads=[key], writes=["VT"])
                pa = nxt("pa", 2)
                for k in range(8):
                    A("pe", lambda e, pa=pa, k=k: e.matmul(PA[pa][:, 0:8], lhsT=ht(i, k), rhs=WFF[:, k, 32:40],
                                                           start=(k == 0), stop=(k == 7)),
                      reads=["WFF", f"HT{i}"], writes=[f"PA{pa}"])
                A("dve", lambda e, pa=pa: e.tensor_scalar(out=IW[:, i, :], in0=PA[pa][:, 0:8], scalar1=8.0 ** -0.5,
                                                          scalar2=None, op0=ALU.mult), reads=[f"PA{pa}"], writes=["IW"])
            proj_tm(bv, 384, ev_dv)
            S.barrier()
            if lvl < 4.1:
                continue
            attention("dsa")
            out_proj(l, 256, 3)
            S.barrier()

        A("sp", lambda e: e.dma_start(out=GB, in_=fg_d[None, :].to_broadcast([128, D])),
          writes=["GB"], dma=True, sem_key="GB")
        rms_stats()
        for i in range(NT):
            yb = i % 2
            A("dve", lambda e, i=i, yb=yb: e.scalar_tensor_tensor(
                out=YST[yb], in0=X[:, i, :], scalar=RSTD[:, i:i + 1], in1=GB, op0=ALU.mult, op1=ALU.mult),
              reads=[f"X{i}", "RSTD", "GB"], writes=[f"YST{yb}"])
            A("sp", lambda e, s=s, i=i, yb=yb: e.dma_start(out=y_d[s, i * 128:(i + 1) * 128, :], in_=YST[yb]),
              reads=[f"YST{yb}"], writes=[], dma=True, sem_key=f"YST{yb}")
        S.barrier()
    S.barrier()
    S.emit(nc)
    es.close()
    return nc


_NC_CACHE = {}


def kernel(x, w_in, norm_g, pool_w, pool_scale, fox_bf, w_out, final_g):
    n_cores = 8
    x = np.ascontiguousarray(np.asarray(x, dtype=np.float32))
    per = x.shape[0] // n_cores
    if "nc" not in _NC_CACHE:
        _NC_CACHE["nc"] = build(n_seq=per, n_layers=4)
    nc = _NC_CACHE["nc"]
    shared = {
        "w_in": np.ascontiguousarray(np.asarray(w_in, dtype=np.float32)),
        "norm_g": np.ascontiguousarray(np.asarray(norm_g, dtype=np.float32)),
        "pool_w": np.ascontiguousarray(np.asarray(pool_w, dtype=np.float32)),
        "pool_scale": np.ascontiguousarray(np.asarray(pool_scale, dtype=np.float32)),
        "fox_bf": np.ascontiguousarray(np.asarray(fox_bf, dtype=np.float32)),
        "w_out": np.ascontiguousarray(np.asarray(w_out, dtype=np.float32)),
        "final_g": np.ascontiguousarray(np.asarray(final_g, dtype=np.float32)),
    }
    in_maps = []
    for c in range(n_cores):
        m = dict(shared)
        m["x"] = np.ascontiguousarray(x[c * per:(c + 1) * per])
        in_maps.append(m)
    res = run_bass_kernel_spmd(nc, in_maps, core_ids=list(range(n_cores)))
    return np.concatenate([np.asarray(r["y"]) for r in res.results], axis=0).astype(np.float32)
```

```python
import contextlib
import numpy as np
import concourse.bass as bass
import concourse.mybir as mybir
from concourse.bass_utils import run_bass_kernel_spmd

F32 = mybir.dt.float32
BF16 = mybir.dt.bfloat16
ALU = mybir.AluOpType
AF = mybir.ActivationFunctionType
AX = mybir.AxisListType

L_SEQ = 2048
D = 1024
NT = 16
IN_COLS = 3886
NEG = -30000.0
NSTEP = 12
TOPK = 256

C_PV, C_PG, C_DQ, C_DK, C_DV, C_DG = 0, 256, 512, 896, 1280, 1664
C_IQ, C_IK, C_IW, C_FQ, C_FK, C_FV, C_FG, C_FF = 2048, 2304, 2336, 2344, 2728, 3112, 3496, 3880

SEM_SPAN = 3000
SAME_ENG_WINDOW = 5


class Op:
    __slots__ = ("id", "eng", "fn", "deps", "dma", "sem_key", "dcount", "gsig",
                 "eidx", "signals")

    def __init__(self, id, eng, fn, dma, sem_key):
        self.id = id
        self.eng = eng
        self.fn = fn
        self.deps = set()
        self.dma = dma
        self.sem_key = sem_key
        self.dcount = 0
        self.gsig = 0
        self.eidx = 0
        self.signals = False


class Sched:
    ENGS = ("pe", "act", "dve", "pool", "sp")

    def __init__(self):
        self.ops = []
        self.last_writer = {}
        self.readers = {}
        self.eng_count = {e: 0 for e in self.ENGS}
        self.dma_counts = {}
        self.last_dma = {}

    def add(self, eng, fn, reads=(), writes=(), dma=False, sem_key=None, extra_deps=()):
        op = Op(len(self.ops), eng, fn, dma, sem_key)
        if dma:
            assert sem_key is not None
            self.dma_counts[sem_key] = self.dma_counts.get(sem_key, 0) + 1
            op.dcount = self.dma_counts[sem_key]
            self.last_dma[sem_key] = op.id
        op.eidx = self.eng_count[eng]
        self.eng_count[eng] += 1
        for k in list(reads) + list(writes):
            w = self.last_writer.get(k)
            if w is not None:
                op.deps.add(w)
        for k in writes:
            for r in self.readers.get(k, ()):
                op.deps.add(r)
        for k in reads:
            self.readers.setdefault(k, []).append(op.id)
        for k in writes:
            self.last_writer[k] = op.id
            self.readers[k] = []
        for d in extra_deps:
            op.deps.add(d)
        op.deps.discard(op.id)
        self.ops.append(op)
        return op

    def barrier(self):
        last = {}
        for op in self.ops:
            if not op.dma and op.fn is not None:
                last[op.eng] = op.id
        deps = list(last.values()) + list(self.last_dma.values())
        for e in self.ENGS:
            self.add(e, None, extra_deps=deps)

    def emit(self, nc):
        ops = self.ops
        for op in ops:
            for d in op.deps:
                p = ops[d]
                if p.dma or p.fn is None:
                    continue
                if p.eng == op.eng:
                    if op.eng == "pe":
                        continue
                    if op.eidx - p.eidx > SAME_ENG_WINDOW:
                        continue
                p.signals = True
        gs = {e: 0 for e in self.ENGS}
        for op in ops:
            if op.signals and not op.dma:
                gs[op.eng] += 1
                op.gsig = gs[op.eng]
        stack = []
        sems = {}
        for e in self.ENGS:
            for i in range((gs[e] + SEM_SPAN - 1) // SEM_SPAN):
                cm = nc.semaphore(f"s_{e}_{i}")
                sems[(e, i)] = cm.__enter__()
                stack.append(cm)
        dsems = {}
        for k in self.dma_counts:
            cm = nc.semaphore(f"d_{len(dsems)}")
            dsems[k] = cm.__enter__()
            stack.append(cm)
        self.n_sems = len(stack)
        per_eng = {e: [op for op in ops if op.eng == e] for e in self.ENGS}

        def run_engine(e, eng):
            waited = {x: 0 for x in self.ENGS}
            dwaited = {}
            for op in per_eng[e]:
                need = {}
                dneed = {}
                for d in op.deps:
                    p = ops[d]
                    if p.fn is None:
                        continue
                    if p.dma:
                        if dwaited.get(p.sem_key, 0) < p.dcount:
                            dneed[p.sem_key] = max(dneed.get(p.sem_key, 0), p.dcount)
                        continue
                    if p.eng == e:
                        if e == "pe" or op.eidx - p.eidx > SAME_ENG_WINDOW:
                            continue
                    if p.gsig > waited[p.eng]:
                        need[p.eng] = max(need.get(p.eng, 0), p.gsig)
                for pe_, g in need.items():
                    si = (g - 1) // SEM_SPAN
                    eng.wait_ge(sems[(pe_, si)], g - si * SEM_SPAN)
                    waited[pe_] = g
                for k, c in dneed.items():
                    eng.wait_ge(dsems[k], 16 * c)
                    dwaited[k] = c
                if op.fn is None:
                    continue
                ins = op.fn(eng)
                if op.dma:
                    ins.then_inc(dsems[op.sem_key], 16)
                elif op.signals:
                    si = (op.gsig - 1) // SEM_SPAN
                    ins.then_inc(sems[(op.eng, si)], 1)

        with nc.Block() as block:
            @block.tensor
            def _(eng):
                run_engine("pe", eng)

            @block.scalar
            def _(eng):
                run_engine("act", eng)

            @block.vector
            def _(eng):
                run_engine("dve", eng)

            @block.gpsimd
            def _(eng):
                run_engine("pool", eng)

            @block.sync
            def _(eng):
                run_engine("sp", eng)
        for cm in reversed(stack):
            cm.__exit__(None, None, None)


def build(n_seq=2, n_layers=4, lvl=9):
    nc = bass.Bass("TRN2", target_bir_lowering=False)
    x_d = nc.dram_tensor("x", [n_seq, L_SEQ, D], F32, kind="ExternalInput").ap()
    win_d = nc.dram_tensor("w_in", [4, D, IN_COLS], F32, kind="ExternalInput").ap()
    ng_d = nc.dram_tensor("norm_g", [4, D], F32, kind="ExternalInput").ap()
    pw_d = nc.dram_tensor("pool_w", [4, 4, 64, 64], F32, kind="ExternalInput").ap()
    psc_d = nc.dram_tensor("pool_scale", [4, 256], F32, kind="ExternalInput").ap()
    fbf_d = nc.dram_tensor("fox_bf", [4, 6], F32, kind="ExternalInput").ap()
    wout_d = nc.dram_tensor("w_out", [4, D, D], F32, kind="ExternalInput").ap()
    fg_d = nc.dram_tensor("final_g", [D], F32, kind="ExternalInput").ap()
    y_d = nc.dram_tensor("y", [n_seq, L_SEQ, D], F32, kind="ExternalOutput").ap()

    S = Sched()
    A = S.add
    es = contextlib.ExitStack()

    def sb(name, shape, dt):
        return es.enter_context(nc.sbuf_tensor(name, shape, dt))

    def pst(name, shape, dt):
        return es.enter_context(nc.psum_tensor(name, shape, dt))

    X = sb("X", [128, NT, D], F32)
    HT = sb("HT", [128, NT * 8 * 128], BF16)
    MIXT = sb("MIXT", [128, 3, L_SEQ], BF16)
    QT = sb("QT", [128, 3, L_SEQ], BF16)
    KT = sb("KT", [128, 3, L_SEQ], BF16)
    VT = sb("VT", [128, NT, 384], BF16)
    WBT = sb("WBT", [128, 2, 8, 392], BF16)
    WB = [WBT[:, 0, :, :], WBT[:, 1, :, :]]
    WOB = sb("WOB", [128, 3, D], BF16)
    WFF = sb("WFF", [128, 8, 128], BF16)
    SCR = sb("SCR", [128, 24576], mybir.dt.uint8)
    PT = [sb(f"PT{i}", [128, 512], BF16) for i in range(4)]
    RB = [sb(f"RB{i}", [128, 512], BF16) for i in range(3)]
    RD = sb("RD", [128, 512], F32)
    TMPF = sb("TMPF", [128, 512], F32)
    ident = sb("ident", [128, 128], BF16)
    tri = sb("tri", [128, 128], BF16)
    Utri = sb("Utri", [128, 128], F32)
    onesf = sb("onesf", [128, 128], F32)
    ones64 = sb("ones64", [128, 64], BF16)
    dmaskf = sb("dmaskf", [128, 128], F32)
    dmaskb = sb("dmaskb", [128, 128], BF16)
    dmaskg = sb("dmaskg", [128, 128], BF16)
    halves = sb("halves", [128, NSTEP + 1], F32)
    invw = sb("invw", [128, 2], F32)
    invcnt = sb("invcnt", [128, 2, 16], F32)
    SS = sb("SS", [128, NT], F32)
    RSTD = sb("RSTD", [128, NT], F32)
    PSCL = sb("PSCL", [128, 2], F32)
    POOLW = sb("POOLW", [128, 2, 128], BF16)
    FBF = sb("FBF", [128, 6], F32)
    IW = sb("IW", [128, NT, 8], F32)
    FF = sb("FF", [128, NT, 6], F32)
    TOT = sb("TOT", [128, NT, 6], F32)
    OFFS = sb("OFFS", [128, NT + 1, 6], F32)
    GG = sb("GG", [128, NT, 6], F32)
    DW = sb("DW", [128, 8, 128], BF16)
    BSS = [sb("BS0", [128, 8 + NSTEP + 1], F32), sb("BS1", [128, 8 + NSTEP + 1], F32)]

    def view(t, dt, byte_off, n_elem):
        ap = t[:, :]
        apb = ap.bitcast(dt)
        esz = mybir.dt.size(dt)
        tsz = mybir.dt.size(t.dtype)
        e0 = byte_off // esz
        return apb[:, e0:e0 + n_elem]

    HB = [view(SCR, BF16, 16384, 1024), view(SCR, BF16, 18432, 1024)]
    GB = view(SCR, F32, 20480, 1024)
    JUNKN = view(SCR, BF16, 8192, 1024)
    VBUF = view(SCR, F32, 0, 528)
    T1 = view(SCR, F32, 2112, 528)
    T2 = view(SCR, F32, 4224, 528)
    PTP = view(SCR, BF16, 8192, 4096)
    BIASALL = view(SCR, F32, 16384, 16 * 16 * 6)
    IQT = view(SCR, BF16, 0, 3 * 2048)
    IKT = view(SCR, BF16, 12288, 2048)
    SCORES = [view(SCR, F32, 16384, 2048), WBT[:, :, :, :].rearrange('p a k c -> p (a k c)').bitcast(F32)[:, 0:2048]]
    YST = [view(HT, F32, 0, 1024), view(HT, F32, 4096, 1024)]

    def ht(i, k):
        o = (i * 8 + k) * 128
        return HT[:, o:o + 128]

    HT4 = HT[:, :].rearrange("p (i k t) -> p i k t", i=NT, k=8)

    PTR = pst("PTR", [128, 1024], BF16)
    PA = [pst("PA0", [128, 512], F32), pst("PA1", [128, 512], F32)]
    PS = [pst("PS0", [128, 512], F32), pst("PS1", [128, 512], F32)]
    PO = [pst("PO0", [128, 512], F32), pst("PO1", [128, 512], F32)]
    PD = [pst("PD0", [128, 512], F32)]
    rot = {"pa": 0, "ps": 0, "po": 0, "pt": 0, "rb": 0, "wb": 0, "hb": 0}

    def nxt(name, n):
        v = rot[name]
        rot[name] = (v + 1) % n
        return v

    A("pool", lambda e: e.memset(TMPF[:, 0:128], 0.0), writes=["TMPF"])
    A("pool", lambda e: e.affine_select(out=TMPF[:, 0:128], in_=TMPF[:, 0:128], pattern=[[-1, 128]],
                                        compare_op=ALU.not_equal, fill=1.0, base=0,
                                        channel_multiplier=1), reads=["TMPF"], writes=["TMPF"])
    A("dve", lambda e: e.tensor_copy(out=ident[:], in_=TMPF[:, 0:128]), reads=["TMPF"], writes=["ident"])
    A("pool", lambda e: e.memset(TMPF[:, 0:128], 0.0), writes=["TMPF"])
    A("pool", lambda e: e.affine_select(out=TMPF[:, 0:128], in_=TMPF[:, 0:128], pattern=[[1, 128]],
                                        compare_op=ALU.is_ge, fill=NEG, base=0,
                                        channel_multiplier=-1), reads=["TMPF"], writes=["TMPF"])
    A("dve", lambda e: e.tensor_copy(out=tri[:], in_=TMPF[:, 0:128]), reads=["TMPF"], writes=["tri"])
    A("pool", lambda e: e.memset(Utri[:], 1.0), writes=["Utri"])
    A("pool", lambda e: e.affine_select(out=Utri[:], in_=Utri[:], pattern=[[1, 128]],
                                        compare_op=ALU.is_ge, fill=0.0, base=0,
                                        channel_multiplier=-1), reads=["Utri"], writes=["Utri"])
    A("pool", lambda e: e.memset(onesf[:], 1.0), writes=["onesf"])
    A("pool", lambda e: e.memset(ones64[:], 1.0), writes=["ones64"])
    A("pool", lambda e: e.memset(dmaskf[:], 0.0), writes=["dmaskf"])
    A("pool", lambda e: e.memset(dmaskf[0:64, 64:128], -1.0e30), reads=["dmaskf"], writes=["dmaskf"])
    A("pool", lambda e: e.memset(dmaskg[:], 0.0), writes=["dmaskg"])
    A("pool", lambda e: e.memset(dmaskg[0:64, 64:128], -1.0e30), reads=["dmaskg"], writes=["dmaskg"])
    A("pool", lambda e: e.memset(dmaskb[:], 0.0), writes=["dmaskb"])
    A("pool", lambda e: e.memset(dmaskb[0:64, 64:128], NEG), reads=["dmaskb"], writes=["dmaskb"])
    for k in range(NSTEP + 1):
        A("pool", lambda e, k=k: e.memset(halves[:, k:k + 1], 0.5 ** (k + 1)), writes=["halves"])
    for j, (wa, wb_) in enumerate(((2, 4), (8, 16))):
        A("pool", lambda e, j=j, wa=wa: e.memset(invw[0:64, j:j + 1], 1.0 / wa), writes=["invw"])
        A("pool", lambda e, j=j, wb_=wb_: e.memset(invw[64:128, j:j + 1], 1.0 / wb_), writes=["invw"])
        for t in range(16):
            A("pool", lambda e, j=j, t=t, wa=wa: e.memset(invcnt[0:64, j, t:t + 1], 1.0 / min(t + 1, wa)),
              writes=["invcnt"])
            A("pool", lambda e, j=j, t=t, wb_=wb_: e.memset(invcnt[64:128, j, t:t + 1], 1.0 / min(t + 1, wb_)),
              writes=["invcnt"])
    A("pool", lambda e: e.memset(OFFS[:, 0, :], 0.0), writes=["OFFS"])

    def load_w(l, pieces):
        b = nxt("wb", 2)
        for (c0, n, s0) in pieces:
            A("pool", lambda e, b=b, c0=c0, n=n, s0=s0: e.dma_start(
                out=WB[b][:, :, s0:s0 + n],
                in_=win_d[l].rearrange("(k p) c -> p k c", p=128)[:, :, c0:c0 + n]),
              writes=[f"WB{b}"], dma=True, sem_key=f"WB{b}")
        return b

    def proj_cm(b, col0, M, evac):
        for c in range(4):
            pa = nxt("pa", 2)
            for k in range(8):
                A("pe", lambda e, pa=pa, k=k, c=c: e.matmul(
                    PA[pa][0:M, :], lhsT=WB[b][:, k, col0:col0 + M],
                    rhs=HT4[:, 4 * c:4 * c + 4, k, :], start=(k == 0), stop=(k == 7)),
                  reads=[f"WB{b}"] + [f"HT{i}" for i in range(4 * c, 4 * c + 4)], writes=[f"PA{pa}"])
            evac(c, PA[pa], f"PA{pa}")

    def proj_tm(b, N, evac):
        for i in range(NT):
            pa = nxt("pa", 2)
            for k in range(8):
                A("pe", lambda e, pa=pa, k=k, i=i: e.matmul(
                    PA[pa][:, 0:N], lhsT=ht(i, k), rhs=WB[b][:, k, 0:N],
                    start=(k == 0), stop=(k == 7)),
                  reads=[f"WB{b}", f"HT{i}"], writes=[f"PA{pa}"])
            evac(i, PA[pa], f"PA{pa}")

    cnt = {"alt": 0}

    def copy_alt(out, in_, reads, writes, scale=None):
        cnt["alt"] += 1
        if cnt["alt"] % 2 == 0:
            if scale is None:
                A("act", lambda e: e.activation(out=out, in_=in_, func=AF.Copy), reads=reads, writes=writes)
            else:
                A("act", lambda e: e.activation(out=out, in_=in_, func=AF.Copy, scale=scale),
                  reads=reads, writes=writes)
        else:
            if scale is None:
                A("dve", lambda e: e.tensor_copy(out=out, in_=in_), reads=reads, writes=writes)
            else:
                A("dve", lambda e: e.tensor_scalar(out=out, in0=in_, scalar1=scale, scalar2=None,
                                                   op0=ALU.mult), reads=reads, writes=writes)

    def out_proj(l, row0, nch):
        A("pool", lambda e: e.dma_start(
            out=WOB[:, 0:nch, :],
            in_=wout_d[l, row0:row0 + nch * 128, :].rearrange("(q p) c -> p q c", p=128)),
          writes=["WOB"], dma=True, sem_key="WOB")
        for i in range(NT):
            for n in range(2):
                pa = nxt("pa", 2)
                for q in range(nch):
                    A("pe", lambda e, pa=pa, q=q, i=i, n=n: e.matmul(
                        PA[pa][:, :], lhsT=MIXT[:, q, i * 128:(i + 1) * 128],
                        rhs=WOB[:, q, n * 512:(n + 1) * 512], start=(q == 0), stop=(q == nch - 1)),
                      reads=["WOB", f"MIXT{q}"], writes=[f"PA{pa}"])
                A("dve", lambda e, pa=pa, i=i, n=n: e.tensor_tensor(
                    out=X[:, i, n * 512:(n + 1) * 512], in0=X[:, i, n * 512:(n + 1) * 512],
                    in1=PA[pa][:, :], op=ALU.add),
                  reads=[f"PA{pa}", f"X{i}"], writes=[f"X{i}"])

    def rms_stats():
        for i in range(NT):
            A("act", lambda e, i=i: e.activation(out=JUNKN, in_=X[:, i, :], func=AF.Square,
                                                 accum_out=SS[:, i:i + 1]),
              reads=[f"X{i}"], writes=["JUNKN", "SS"])
        A("act", lambda e: e.activation(out=RSTD[:], in_=SS[:], func=AF.Sqrt, bias=EPS_T[:, 0:1], scale=1.0 / D),
          reads=["SS"], writes=["RSTD"])
        A("dve", lambda e: e.reciprocal(out=RSTD[:], in_=RSTD[:]), reads=["RSTD"], writes=["RSTD"])

    EPS_T = sb("EPS_T", [128, 1], F32)
    A("pool", lambda e: e.memset(EPS_T[:], 1e-6), writes=["EPS_T"])
    C255 = sb("C255", [128, 1], F32)
    A("pool", lambda e: e.memset(C255[:], TOPK - 0.5), writes=["C255"])
    CNEG = sb("CNEG", [128, 1], F32)
    A("pool", lambda e: e.memset(CNEG[:], NEG), writes=["CNEG"])


    def mb_view(c, q):
        base = (c % 2) * 16384
        o = 0
        for qq in range(q):
            o += (4 * c + qq + 1) * 128
        n = (4 * c + q + 1) * 128
        return view(HT, BF16, base + 2 * o, n)

    PSC = PTR[:, :].bitcast(F32)

    def attention(kind):
        if kind == "dsa":
            for slot in range(3):
                mask_jobs(0, slot)
        for c in range(4):
            attn_chunk(kind, c)

    def attn_chunk(kind, c):
        nj = 4 * c + 4
        t0 = 4 * c * 128
        po_of = {}
        pd = 0
        pre = {0: (0, 1), 1: (2,), 2: (3,)}

        def qk(p, j, hh):
            h = 2 * p + hh
            r0 = 64 * hh
            i_lo = max(4 * c, j)
            off = (i_lo - 4 * c) * 128
            ps = nxt("ps", 2)
            psk = f"PS{ps}"
            pt = nxt("pt", 4)
            kslc = KT[r0:r0 + 64, p, j * 128:(j + 1) * 128]
            if kind == "fox":
                if j >= 4 * c:
                    A("pe", lambda e: e.matmul(PS[ps][:, off:off + 128], lhsT=ident[:], rhs=tri[:],
                                               start=True, stop=False),
                      reads=["ident", "tri"], writes=[psk])
                    A("pe", lambda e: e.matmul(PS[ps][:, off:off + 128], lhsT=kslc,
                                               rhs=QT[r0:r0 + 64, p, t0 + off:t0 + off + 128],
                                               start=False, stop=True),
                      reads=[f"KT{p}", f"QT{p}"], writes=[psk])
                    if off + 128 < 512:
                        A("pe", lambda e: e.matmul(PS[ps][:, off + 128:512], lhsT=kslc,
                                                   rhs=QT[r0:r0 + 64, p, t0 + off + 128:t0 + 512],
                                                   start=True, stop=True),
                          reads=[f"KT{p}", f"QT{p}"], writes=[psk])
                else:
                    A("pe", lambda e: e.matmul(PS[ps][:, 0:512], lhsT=kslc, rhs=QT[r0:r0 + 64, p, t0:t0 + 512],
                                               start=True, stop=True),
                      reads=[f"KT{p}", f"QT{p}"], writes=[psk])
                i = i_lo
                while i < 4 * c + 4:
                    wd = 2 if i % 2 == 0 else 1
                    co = (i - 4 * c) * 128
                    bcol = ((i | 1) * 16 + j) * 6 + h
                    A("act", lambda e, co=co, wd=wd, bcol=bcol: e.activation(
                        out=PT[pt][:, co:co + 128 * wd], in_=PS[ps][:, co:co + 128 * wd], func=AF.Exp,
                        bias=BIASALL[:, bcol:bcol + 1], scale=1.0),
                      reads=[psk, "BIASALL"], writes=[f"PT{pt}"])
                    i += wd
            else:
                A("pe", lambda e: e.matmul(
                    PS[ps][:, off:512], lhsT=kslc, rhs=QT[r0:r0 + 64, p, t0 + off:t0 + 512],
                    start=True, stop=False),
                  reads=[f"KT{p}", f"QT{p}"], writes=[psk])
                for i in range(i_lo, 4 * c + 4):
                    co = (i - 4 * c) * 128
                    q = i - 4 * c
                    mbv = mb_view(c, q)
                    A("pe", lambda e, co=co, mbv=mbv: e.matmul(
                        PS[ps][:, co:co + 128], lhsT=mbv[:, j * 128:(j + 1) * 128], rhs=ident[:],
                        start=False, stop=True),
                      reads=[f"MB{c % 2}_{q}", "ident"], writes=[psk])
                A("act", lambda e: e.activation(out=PT[pt][:, off:512], in_=PS[ps][:, off:512], func=AF.Exp),
                  reads=[psk], writes=[f"PT{pt}"])
            return (p, j, hh, pt, off)

        def pv(blk):
            p, j, hh, pt, off = blk
            h = 2 * p + hh
            r0 = 64 * hh
            if p not in po_of:
                po_of[p] = nxt("po", 2)
            po = po_of[p]
            A("pe", lambda e: e.matmul(
                PO[po][r0:r0 + 64, off:512], lhsT=VT[:, j, h * 64:(h + 1) * 64], rhs=PT[pt][:, off:512],
                start=(j == 0), stop=(j == nj - 1), tile_position=(0, r0)),
              reads=[f"PT{pt}", "VT"], writes=[f"PO{po}"])
            A("pe", lambda e: e.matmul(
                PD[pd][r0:r0 + 64, off:512], lhsT=ones64[:], rhs=PT[pt][:, off:512],
                start=(j == 0), stop=(j == nj - 1), tile_position=(0, r0)),
              reads=[f"PT{pt}", "ones64"], writes=[f"PD{pd}"])
            if j == nj - 1 and hh == 1:
                csl = slice(4 * c * 128, (4 * c + 4) * 128)
                A("dve", lambda e: e.reciprocal(out=RD[:], in_=PD[pd][:, :]), reads=[f"PD{pd}"], writes=["RD"])
                A("dve", lambda e: e.tensor_tensor(out=TMPF[:], in0=PO[po][:, :], in1=RD[:], op=ALU.mult),
                  reads=[f"PO{po}", "RD"], writes=["TMPF"])
                A("dve", lambda e: e.tensor_tensor(out=MIXT[:, p, csl], in0=TMPF[:], in1=MIXT[:, p, csl],
                                                   op=ALU.mult),
                  reads=["TMPF", f"MIXT{p}"], writes=[f"MIXT{p}"])

        pend = None
        for p in range(3):
            if kind == "dsa" and c < 3:
                mask_jobs(c + 1, p)
            for j in range(nj):
                for hh in range(2):
                    cur = qk(p, j, hh)
                    if pend is not None:
                        pv(pend)
                    pend = cur
        pv(pend)

    def dsa_idx_tile(c, q):
        i = 4 * c + q
        n = (i + 1) * 128
        mk = f"MB{c % 2}_{q}"
        mbv = mb_view(c, q)
        if i < 2:
            if i == 1:
                A("pool", lambda e: e.memset(mbv[:, 0:128], 0.0), writes=[mk])
            A("pool", lambda e: e.tensor_copy(out=mbv[:, n - 128:n], in_=dmaskb[:]),
              reads=["dmaskb"], writes=[mk])
            return
        SC = SCORES[q % 2]
        sk = f"SCORE{q % 2}"
        for h in range(8):
            A("act", lambda e, h=h: e.activation(out=DW[:, h, :], in_=ident[:], func=AF.Copy,
                                                 scale=IW[:, i, h:h + 1]),
              reads=["ident", "IW"], writes=["DW"])
        nsc = (n + 511) // 512
        for sc in range(nsc):
            w = min(512, n - sc * 512)
            last = (sc == nsc - 1)
            for h in range(8):
                ti, r = h // 3, (h % 3) * 32
                pa = nxt("pa", 2)
                rb = nxt("rb", 3)
                A("pe", lambda e, pa=pa, ti=ti, r=r, w=w, sc=sc: e.matmul(
                    PA[pa][:, 0:w], lhsT=IQT[r:r + 32, ti * 2048 + i * 128:ti * 2048 + (i + 1) * 128],
                    rhs=IKT[r:r + 32, sc * 512:sc * 512 + w], start=True, stop=True),
                  reads=["IQT", "IKT"], writes=[f"PA{pa}"])
                A("act", lambda e, pa=pa, rb=rb, w=w: e.activation(out=RB[rb][:, 0:w], in_=PA[pa][:, 0:w],
                                                                   func=AF.Relu),
                  reads=[f"PA{pa}"], writes=[f"RB{rb}"])
                A("pe", lambda e, rb=rb, h=h, w=w, last=last: e.matmul(
                    PSC[:, 0:w], lhsT=DW[:, h, :], rhs=RB[rb][:, 0:w], start=(h == 0),
                    stop=(h == 7 and not last)),
                  reads=["DW", f"RB{rb}"], writes=["PTR"])
            if last:
                A("pe", lambda e, w=w: e.matmul(PSC[:, w - 128:w], lhsT=ident[:], rhs=dmaskg[:],
                                                start=False, stop=True),
                  reads=["ident", "dmaskg"], writes=["PTR"])
            s0 = sc * 512
            A("act", lambda e, s0=s0, w=w: e.activation(out=SC[:, s0:s0 + w], in_=PSC[:, 0:w], func=AF.Copy),
              reads=["PTR"], writes=[sk])

    def dsa_bis_ops(c, q):
        i = 4 * c + q
        if i < 2:
            return []
        n = (i + 1) * 128
        mk = f"MB{c % 2}_{q}"
        mbv = mb_view(c, q)
        SC = SCORES[q % 2]
        sk = f"SCORE{q % 2}"
        BS = BSS[q % 2]
        u = f"b{q % 2}"
        mx, mn, rg, mid, cn, tp = (BS[:, 0:1], BS[:, 1:2], BS[:, 2:3], BS[:, 3:4], BS[:, 4:5], BS[:, 5:6])
        DL = BS[:, 8:8 + NSTEP + 1]
        ops = []
        O = lambda fn, reads, writes: ops.append((fn, reads, writes))
        O(lambda e: e.tensor_reduce(out=mx, in_=SC[:, 0:n], axis=AX.X, op=ALU.max), [sk], [u + "mx"])
        O(lambda e: e.tensor_reduce(out=mn, in_=SC[:, 0:n - 64], axis=AX.X, op=ALU.min), [sk], [u + "mn"])
        O(lambda e: e.tensor_tensor(out=rg, in0=mx, in1=mn, op=ALU.subtract), [u + "mx", u + "mn"], [u + "rg"])
        O(lambda e: e.tensor_scalar(out=DL, in0=halves[:], scalar1=rg, scalar2=None, op0=ALU.mult),
          [u + "rg", "halves"], [u + "dl"])
        O(lambda e: e.tensor_tensor(out=mid, in0=mn, in1=BS[:, 8:9], op=ALU.add), [u + "mn", u + "dl"], [u + "mid"])
        for k in range(NSTEP):
            O(lambda e: e.tensor_scalar(out=mbv[:, 0:n], in0=SC[:, 0:n], scalar1=mid, scalar2=None,
                                        op0=ALU.is_ge, op1=ALU.add, accum_out=cn),
              [sk, u + "mid"], [mk, u + "cn"])
            O(lambda e, k=k: e.tensor_scalar(out=tp, in0=cn, scalar1=C255[:, 0:1], scalar2=BS[:, 8 + k:9 + k],
                                             op0=ALU.is_ge, op1=ALU.mult),
              [u + "cn", u + "dl"], [u + "tp"])
            O(lambda e, k=k: e.scalar_tensor_tensor(out=mid, in0=tp, scalar=BS[:, 9 + k:10 + k], in1=mid,
                                                    op0=ALU.subtract, op1=ALU.add),
              [u + "tp", u + "dl", u + "mid"], [u + "mid"])
        O(lambda e: e.tensor_tensor(out=tp, in0=mid, in1=BS[:, 8 + NSTEP:9 + NSTEP], op=ALU.subtract),
          [u + "mid", u + "dl"], [u + "tp"])
        O(lambda e: e.tensor_scalar(out=mbv[:, 0:n], in0=SC[:, 0:n], scalar1=tp, scalar2=CNEG[:, 0:1],
                                    op0=ALU.is_lt, op1=ALU.mult),
          [sk, u + "tp"], [mk])
        return ops

    def dsa_bis_pair(c, qa, qb):
        la, lb = dsa_bis_ops(c, qa), dsa_bis_ops(c, qb)
        for t in range(max(len(la), len(lb))):
            for lst in (la, lb):
                if t < len(lst):
                    fn, r, w = lst[t]
                    A("dve", fn, reads=r, writes=w)

    def mask_jobs(c, slot):
        if slot == 0:
            dsa_idx_tile(c, 0); dsa_idx_tile(c, 1)
        elif slot == 1:
            dsa_bis_pair(c, 0, 1); dsa_idx_tile(c, 2); dsa_idx_tile(c, 3)
        else:
            dsa_bis_pair(c, 2, 3)

    for s in range(n_seq):
        for qd in range(4):
            A("sp", lambda e, s=s, qd=qd: e.dma_start(
                out=X[:, 4 * qd:4 * qd + 4, :],
                in_=x_d[s].rearrange("(i p) d -> p i d", p=128)[:, 4 * qd:4 * qd + 4, :]),
              writes=[f"X{i}" for i in range(4 * qd, 4 * qd + 4)], dma=True, sem_key=f"XL{qd}")
        for l in range(n_layers):
            if lvl < 1:
                break
            A("sp", lambda e, l=l: e.dma_start(out=GB, in_=ng_d[l:l + 1, :].to_broadcast([128, D])),
              writes=["GB"], dma=True, sem_key="GB")
            for j in range(2):
                A("sp", lambda e, l=l, j=j: e.dma_start(
                    out=PSCL[:, j:j + 1], in_=psc_d[l, j * 128:(j + 1) * 128].rearrange("(p o) -> p o", o=1)),
                  writes=["PSCL"], dma=True, sem_key=f"PSCL{j}")
            A("sp", lambda e, l=l: e.dma_start(out=FBF[:], in_=fbf_d[l:l + 1, :].to_broadcast([128, 6])),
              writes=["FBF"], dma=True, sem_key="FBF")
            A("pool", lambda e: e.memset(POOLW[:], 0.0), writes=["POOLW"])
            for g in range(4):
                j, r0 = g // 2, (g % 2) * 64
                A("pool", lambda e, l=l, g=g, j=j, r0=r0: e.dma_start(
                    out=POOLW[r0:r0 + 64, j, r0:r0 + 64], in_=pw_d[l, g]),
                  reads=[], writes=["POOLW"], dma=True, sem_key=f"POOLW{g}")
            rms_stats()
            for i in range(NT):
                hb = nxt("hb", 2)
                A("dve", lambda e, i=i, hb=hb: e.scalar_tensor_tensor(
                    out=HB[hb], in0=X[:, i, :], scalar=RSTD[:, i:i + 1], in1=GB, op0=ALU.mult, op1=ALU.mult),
                  reads=[f"X{i}", "RSTD", "GB"], writes=[f"HB{hb}"])
                for k in range(8):
                    A("pe", lambda e, hb=hb, k=k: e.transpose(out=PTR[:, k * 128:(k + 1) * 128],
                                                              in_=HB[hb][:, k * 128:(k + 1) * 128],
                                                              identity=ident[:]),
                      reads=[f"HB{hb}", "ident"], writes=["PTR"])
                copy_alt(HT[:, i * 1024:(i + 1) * 1024], PTR[:, :], ["PTR"], [f"HT{i}"])

            if lvl < 2:
                S.barrier()
                continue
            b = load_w(l, [(C_PG, 256, 0)])
            for j in range(2):
                def ev_g(c, ps_ap, key, j=j):
                    A("act", lambda e: e.activation(out=MIXT[:, j, c * 512:(c + 1) * 512], in_=ps_ap[:, :],
                                                    func=AF.Silu), reads=[key], writes=[f"MIXT{j}"])
                proj_cm(b, j * 128, 128, ev_g)
            b = load_w(l, [(C_PV, 256, 0)])
            for j in range(2):
                srcA = (T1, T2)
                def ev_v(c, ps_ap, key, j=j):
                    if c == 0:
                        A("pool", lambda e: e.memset(VBUF[:, 0:16], 0.0), writes=["VB_h"])
                    else:
                        A("pool", lambda e: e.tensor_copy(out=VBUF[:, 0:16], in_=VBUF[:, 512:528]),
                          reads=["VB_m"], writes=["VB_h"])
                    A("act", lambda e: e.activation(out=VBUF[:, 16:528], in_=ps_ap[:, :], func=AF.Copy),
                      reads=[key, "VB_h"], writes=["VB_m"])
                    rk = ["VB_h", "VB_m"]
                    A("pool", lambda e: e.tensor_tensor(out=T1[:, 1:528], in0=VBUF[:, 1:528], in1=VBUF[:, 0:527],
                                                        op=ALU.add), reads=rk, writes=["T1"])
                    if j == 0:
                        A("pool", lambda e: e.tensor_tensor(out=T2[64:128, 3:528], in0=T1[64:128, 3:528],
                                                            in1=T1[64:128, 1:526], op=ALU.add),
                          reads=["T1"], writes=["T2"])
                        fa, fb = T1, T2
                    else:
                        A("pool", lambda e: e.tensor_tensor(out=T2[:, 3:528], in0=T1[:, 3:528], in1=T1[:, 1:526],
                                                            op=ALU.add), reads=["T1"], writes=["T2"])
                        A("pool", lambda e: e.tensor_tensor(out=T1[:, 7:528], in0=T2[:, 7:528], in1=T2[:, 3:524],
                                                            op=ALU.add), reads=["T2"], writes=["T1"])
                        A("pool", lambda e: e.tensor_tensor(out=T2[64:128, 15:528], in0=T1[64:128, 15:528],
                                                            in1=T1[64:128, 7:520], op=ALU.add),
                          reads=["T1"], writes=["T2"])
                        fa, fb = T1, T2
                    o0 = j * 2048 + c * 512
                    for (rs, src) in ((slice(0, 64), fa), (slice(64, 128), fb)):
                        A("pool", lambda e, rs=rs, src=src: e.tensor_scalar(
                            out=TMPF[rs, 0:512], in0=src[rs, 16:528], scalar1=invw[rs, j:j + 1], scalar2=None,
                            op0=ALU.mult),
                          reads=["T1", "T2", "invw"], writes=["TMPF"])
                        A("pool", lambda e, rs=rs: e.tensor_tensor(
                            out=PTP[rs, o0:o0 + 512], in0=TMPF[rs, 0:512], in1=VBUF[rs, 16:528], op=ALU.subtract),
                          reads=["TMPF", "VB_m"], writes=[f"PTP{j}"])
                        if c == 0:
                            A("pool", lambda e, rs=rs, src=src: e.tensor_tensor(
                                out=TMPF[rs, 0:16], in0=src[rs, 16:32], in1=invcnt[rs, j, :], op=ALU.mult),
                              reads=["T1", "T2", "invcnt"], writes=["TMPF"])
                            A("pool", lambda e, rs=rs: e.tensor_tensor(
                                out=PTP[rs, o0:o0 + 16], in0=TMPF[rs, 0:16], in1=VBUF[rs, 16:32], op=ALU.subtract),
                              reads=["TMPF", "VB_m", f"PTP{j}"], writes=[f"PTP{j}"])
                proj_cm(b, j * 128, 128, ev_v)
            for j in range(2):
                for c in range(4):
                    pa = nxt("pa", 2)
                    A("pe", lambda e, pa=pa, j=j, c=c: e.matmul(
                        PA[pa][:, :], lhsT=POOLW[:, j, :], rhs=PTP[:, j * 2048 + c * 512:j * 2048 + (c + 1) * 512],
                        start=True, stop=True),
                      reads=["POOLW", f"PTP{j}"] + [f"POOLW{g}" for g in range(4)], writes=[f"PA{pa}"])
                    A("dve", lambda e, pa=pa, j=j, c=c: e.scalar_tensor_tensor(
                        out=MIXT[:, j, c * 512:(c + 1) * 512], in0=PA[pa][:, :], scalar=PSCL[:, j:j + 1],
                        in1=MIXT[:, j, c * 512:(c + 1) * 512], op0=ALU.mult, op1=ALU.mult),
                      reads=[f"PA{pa}", "PSCL", f"MIXT{j}"], writes=[f"MIXT{j}"])
            out_proj(l, 0, 2)

            if lvl < 3:
                S.barrier()
                continue
            def qk_proj(c_q, c_k):
                bq = load_w(l, [(c_q, 384, 0)])
                for p in range(3):
                    def ev(c, ps_ap, key, p=p):
                        copy_alt(QT[:, p, c * 512:(c + 1) * 512], ps_ap[:, :], [key], [f"QT{p}"], scale=0.125)
                    proj_cm(bq, p * 128, 128, ev)
                bk = load_w(l, [(c_k, 384, 0)])
                for p in range(3):
                    def ev(c, ps_ap, key, p=p):
                        copy_alt(KT[:, p, c * 512:(c + 1) * 512], ps_ap[:, :], [key], [f"KT{p}"])
                    proj_cm(bk, p * 128, 128, ev)

            def gate_proj(c_g):
                bg = load_w(l, [(c_g, 384, 0)])
                for p in range(3):
                    def ev(c, ps_ap, key, p=p):
                        A("act", lambda e: e.activation(out=MIXT[:, p, c * 512:(c + 1) * 512], in_=ps_ap[:, :],
                                                        func=AF.Silu), reads=[key], writes=[f"MIXT{p}"])
                    proj_cm(bg, p * 128, 128, ev)

            if lvl >= 3.01:
                qk_proj(C_FQ, C_FK)
            if lvl < 3.02:
                S.barrier()
                continue
            gate_proj(C_FG)
            if lvl < 3.03:
                S.barrier()
                continue
            bv = load_w(l, [(C_FV, 384, 0)])
            A("pool", lambda e, l=l: e.dma_start(
                out=WFF[:, :, :],
                in_=win_d[l].rearrange("(k p) c -> p k c", p=128)[:, :, IN_COLS - 128:IN_COLS]),
              writes=["WFF"], dma=True, sem_key="WFF")

            def ev_fv(i, ps_ap, key):
                A("dve", lambda e: e.tensor_copy(out=VT[:, i, :], in_=ps_ap[:, 0:384]), reads=[key], writes=["VT"])
                pa = nxt("pa", 2)
                for k in range(8):
                    A("pe", lambda e, pa=pa, k=k: e.matmul(PA[pa][:, 0:6], lhsT=ht(i, k), rhs=WFF[:, k, 122:128],
                                                           start=(k == 0), stop=(k == 7)),
                      reads=["WFF", f"HT{i}"], writes=[f"PA{pa}"])
                A("dve", lambda e, pa=pa: e.tensor_tensor(out=FF[:, i, :], in0=PA[pa][:, 0:6], in1=FBF[:], op=ALU.add),
                  reads=[f"PA{pa}", "FBF"], writes=["FF"])
            proj_tm(bv, 384, ev_fv)
            if lvl < 3.1:
                S.barrier()
                continue
            FFf = FF[:, :, :].rearrange("p i h -> p (i h)")
            A("act", lambda e: e.activation(out=FFf, in_=FFf, func=AF.Exp, scale=-1.0), reads=["FF"], writes=["FF"])
            A("act", lambda e: e.activation(out=FFf, in_=FFf, func=AF.Ln, bias=1.0, scale=1.0),
              reads=["FF"], writes=["FF"])
            pa = nxt("pa", 2)
            A("pe", lambda e, pa=pa: e.matmul(PA[pa][:, 0:96], lhsT=Utri[:], rhs=FFf, start=True, stop=True),
              reads=["Utri", "FF"], writes=[f"PA{pa}"])
            pa2 = nxt("pa", 2)
            A("pe", lambda e, pa2=pa2: e.matmul(PA[pa2][:, 0:96], lhsT=onesf[:], rhs=FFf, start=True, stop=True),
              reads=["onesf", "FF"], writes=[f"PA{pa2}"])
            A("dve", lambda e, pa2=pa2: e.tensor_copy(out=TOT[:, :, :].rearrange("p i h -> p (i h)"),
                                                      in_=PA[pa2][:, 0:96]), reads=[f"PA{pa2}"], writes=["TOT"])
            for i in range(NT):
                A("dve", lambda e, i=i: e.tensor_tensor(out=OFFS[:, i + 1, :], in0=OFFS[:, i, :], in1=TOT[:, i, :],
                                                        op=ALU.add), reads=["TOT", "OFFS"], writes=["OFFS"])
            A("dve", lambda e, pa=pa: e.tensor_tensor(
                out=GG[:, :, :].rearrange("p i h -> p (i h)"), in0=PA[pa][:, 0:96],
                in1=OFFS[:, 0:NT, :].rearrange("p i h -> p (i h)"), op=ALU.add),
              reads=[f"PA{pa}", "OFFS"], writes=["GG"])
            for i in range(1, NT, 2):
                A("dve", lambda e, i=i: e.tensor_tensor(
                    out=BIASALL[:, i * 96:(i + 1) * 96].rearrange("p (j h) -> p j h", j=16),
                    in0=GG[:, :, :],
                    in1=OFFS[:, i + 1:i + 2, :].to_broadcast([128, 16, 6]), op=ALU.subtract),
                  reads=["GG", "OFFS"], writes=["BIASALL"])
            if lvl < 3.2:
                S.barrier()
                continue
            attention("fox")
            out_proj(l, 640, 3)

            if lvl < 4:
                S.barrier()
                continue
            S.barrier()
            qk_proj(C_DQ, C_DK)
            gate_proj(C_DG)
            bi = load_w(l, [(C_IQ, 256, 0)])
            A("pool", lambda e, l=l: e.dma_start(
                out=WFF[:, :, :],
                in_=win_d[l].rearrange("(k p) c -> p k c", p=128)[:, :, C_IK:C_IK + 128]),
              writes=["WFF"], dma=True, sem_key="WFF")
            for rr in range(3):
                A("dve", lambda e, rr=rr, bi=bi: e.tensor_copy(out=WB[bi][:, :, 256 + 32 * rr:288 + 32 * rr],
                                                               in_=WFF[:, :, 0:32]),
                  reads=["WFF"], writes=[f"WB{bi}"])
            for ti, (c0, m) in enumerate(((0, 96), (96, 96), (192, 64))):
                def ev(c, ps_ap, key, ti=ti, m=m):
                    copy_alt(IQT[0:m, ti * 2048 + c * 512:ti * 2048 + (c + 1) * 512], ps_ap[0:m, :], [key], ["IQT"])
                proj_cm(bi, c0, m, ev)

            def ev_ik(c, ps_ap, key):
                copy_alt(IKT[0:96, c * 512:(c + 1) * 512], ps_ap[0:96, :], [key], ["IKT"])
            proj_cm(bi, 256, 96, ev_ik)
            bv = load_w(l, [(C_DV, 384, 0)])

            def ev_dv(i, ps_ap, key):
                A("dve", lambda e: e.tensor_copy(out=VT[:, i, :], in_=ps_ap[:, 0:384]), reads=[key], writes=["VT"])
                pa = nxt("pa", 2)
                for k in range(8):
                    A("pe", lambda e, pa=pa, k=k: e.matmul(PA[pa][:, 0:8], lhsT=ht(i, k), rhs=WFF[:, k, 32:40],
                                                           start=(k == 0), stop=(k == 7)),
                      reads=["WFF", f"HT{i}"], writes=[f"PA{pa}"])
                A("dve", lambda e, pa=pa: e.tensor_scalar(out=IW[:, i, :], in0=PA[pa][:, 0:8], scalar1=8.0 ** -0.5,
                                                          scalar2=None, op0=ALU.mult), reads=[f"PA{pa}"], writes=["IW"])
            proj_tm(bv, 384, ev_dv)
            S.barrier()
            if lvl < 4.1:
                continue
            attention("dsa")
            out_proj(l, 256, 3)
            S.barrier()

        A("sp", lambda e: e.dma_start(out=GB, in_=fg_d[None, :].to_broadcast([128, D])),
          writes=["GB"], dma=True, sem_key="GB")
        rms_stats()
        for i in range(NT):
            yb = i % 2
            A("dve", lambda e, i=i, yb=yb: e.scalar_tensor_tensor(
                out=YST[yb], in0=X[:, i, :], scalar=RSTD[:, i:i + 1], in1=GB, op0=ALU.mult, op1=ALU.mult),
              reads=[f"X{i}", "RSTD", "GB"], writes=[f"YST{yb}"])
            A("sp", lambda e, s=s, i=i, yb=yb: e.dma_start(out=y_d[s, i * 128:(i + 1) * 128, :], in_=YST[yb]),
              reads=[f"YST{yb}"], writes=[], dma=True, sem_key=f"YST{yb}")
        S.barrier()
    S.barrier()
    S.emit(nc)
    es.close()
    return nc


_NC_CACHE = {}


def kernel(x, w_in, norm_g, pool_w, pool_scale, fox_bf, w_out, final_g):
    n_cores = 8
    x = np.ascontiguousarray(np.asarray(x, dtype=np.float32))
    per = x.shape[0] // n_cores
    if "nc" not in _NC_CACHE:
        _NC_CACHE["nc"] = build(n_seq=per, n_layers=4)
    nc = _NC_CACHE["nc"]
    shared = {
        "w_in": np.ascontiguousarray(np.asarray(w_in, dtype=np.float32)),
        "norm_g": np.ascontiguousarray(np.asarray(norm_g, dtype=np.float32)),
        "pool_w": np.ascontiguousarray(np.asarray(pool_w, dtype=np.float32)),
        "pool_scale": np.ascontiguousarray(np.asarray(pool_scale, dtype=np.float32)),
        "fox_bf": np.ascontiguousarray(np.asarray(fox_bf, dtype=np.float32)),
        "w_out": np.ascontiguousarray(np.asarray(w_out, dtype=np.float32)),
        "final_g": np.ascontiguousarray(np.asarray(final_g, dtype=np.float32)),
    }
    in_maps = []
    for c in range(n_cores):
        m = dict(shared)
        m["x"] = np.ascontiguousarray(x[c * per:(c + 1) * per])
        in_maps.append(m)
    res = run_bass_kernel_spmd(nc, in_maps, core_ids=list(range(n_cores)))
    return np.concatenate([np.asarray(r["y"]) for r in res.results], axis=0).astype(np.float32)
```

```python
import contextlib
import numpy as np
import concourse.bass as bass
import concourse.mybir as mybir
from concourse.bass_utils import run_bass_kernel_spmd

F32 = mybir.dt.float32
BF16 = mybir.dt.bfloat16
ALU = mybir.AluOpType
AF = mybir.ActivationFunctionType
AX = mybir.AxisListType

L_SEQ = 2048
D = 1024
NT = 16
IN_COLS = 3886
NEG = -30000.0
NSTEP = 12
TOPK = 256

C_PV, C_PG, C_DQ, C_DK, C_DV, C_DG = 0, 256, 512, 896, 1280, 1664
C_IQ, C_IK, C_IW, C_FQ, C_FK, C_FV, C_FG, C_FF = 2048, 2304, 2336, 2344, 2728, 3112, 3496, 3880

SEM_SPAN = 3000
SAME_ENG_WINDOW = 5


class Op:
    __slots__ = ("id", "eng", "fn", "deps", "dma", "sem_key", "dcount", "gsig",
                 "eidx", "signals")

    def __init__(self, id, eng, fn, dma, sem_key):
        self.id = id
        self.eng = eng
        self.fn = fn
        self.deps = set()
        self.dma = dma
        self.sem_key = sem_key
        self.dcount = 0
        self.gsig = 0
        self.eidx = 0
        self.signals = False


class Sched:
    ENGS = ("pe", "act", "dve", "pool", "sp")

    def __init__(self):
        self.ops = []
        self.last_writer = {}
        self.readers = {}
        self.eng_count = {e: 0 for e in self.ENGS}
        self.dma_counts = {}
        self.last_dma = {}

    def add(self, eng, fn, reads=(), writes=(), dma=False, sem_key=None, extra_deps=()):
        op = Op(len(self.ops), eng, fn, dma, sem_key)
        if dma:
            assert sem_key is not None
            self.dma_counts[sem_key] = self.dma_counts.get(sem_key, 0) + 1
            op.dcount = self.dma_counts[sem_key]
            self.last_dma[sem_key] = op.id
        op.eidx = self.eng_count[eng]
        self.eng_count[eng] += 1
        for k in list(reads) + list(writes):
            w = self.last_writer.get(k)
            if w is not None:
                op.deps.add(w)
        for k in writes:
            for r in self.readers.get(k, ()):
                op.deps.add(r)
        for k in reads:
            self.readers.setdefault(k, []).append(op.id)
        for k in writes:
            self.last_writer[k] = op.id
            self.readers[k] = []
        for d in extra_deps:
            op.deps.add(d)
        op.deps.discard(op.id)
        self.ops.append(op)
        return op

    def barrier(self):
        last = {}
        for op in self.ops:
            if not op.dma and op.fn is not None:
                last[op.eng] = op.id
        deps = list(last.values()) + list(self.last_dma.values())
        for e in self.ENGS:
            self.add(e, None, extra_deps=deps)

    def emit(self, nc):
        ops = self.ops
        for op in ops:
            for d in op.deps:
                p = ops[d]
                if p.dma or p.fn is None:
                    continue
                if p.eng == op.eng:
                    if op.eng == "pe":
                        continue
                    if op.eidx - p.eidx > SAME_ENG_WINDOW:
                        continue
                p.signals = True
        gs = {e: 0 for e in self.ENGS}
        for op in ops:
            if op.signals and not op.dma:
                gs[op.eng] += 1
                op.gsig = gs[op.eng]
        stack = []
        sems = {}
        for e in self.ENGS:
            for i in range((gs[e] + SEM_SPAN - 1) // SEM_SPAN):
                cm = nc.semaphore(f"s_{e}_{i}")
                sems[(e, i)] = cm.__enter__()
                stack.append(cm)
        dsems = {}
        for k in self.dma_counts:
            cm = nc.semaphore(f"d_{len(dsems)}")
            dsems[k] = cm.__enter__()
            stack.append(cm)
        self.n_sems = len(stack)
        per_eng = {e: [op for op in ops if op.eng == e] for e in self.ENGS}

        def run_engine(e, eng):
            waited = {x: 0 for x in self.ENGS}
            dwaited = {}
            for op in per_eng[e]:
                need = {}
                dneed = {}
                for d in op.deps:
                    p = ops[d]
                    if p.fn is None:
                        continue
                    if p.dma:
                        if dwaited.get(p.sem_key, 0) < p.dcount:
                            dneed[p.sem_key] = max(dneed.get(p.sem_key, 0), p.dcount)
                        continue
                    if p.eng == e:
                        if e == "pe" or op.eidx - p.eidx > SAME_ENG_WINDOW:
                            continue
                    if p.gsig > waited[p.eng]:
                        need[p.eng] = max(need.get(p.eng, 0), p.gsig)
                for pe_, g in need.items():
                    si = (g - 1) // SEM_SPAN
                    eng.wait_ge(sems[(pe_, si)], g - si * SEM_SPAN)
                    waited[pe_] = g
                for k, c in dneed.items():
                    eng.wait_ge(dsems[k], 16 * c)
                    dwaited[k] = c
                if op.fn is None:
                    continue
                ins = op.fn(eng)
                if op.dma:
                    ins.then_inc(dsems[op.sem_key], 16)
                elif op.signals:
                    si = (op.gsig - 1) // SEM_SPAN
                    ins.then_inc(sems[(op.eng, si)], 1)

        with nc.Block() as block:
            @block.tensor
            def _(eng):
                run_engine("pe", eng)

            @block.scalar
            def _(eng):
                run_engine("act", eng)

            @block.vector
            def _(eng):
                run_engine("dve", eng)

            @block.gpsimd
            def _(eng):
                run_engine("pool", eng)

            @block.sync
            def _(eng):
                run_engine("sp", eng)
        for cm in reversed(stack):
            cm.__exit__(None, None, None)


def build(n_seq=2, n_layers=4, lvl=9):
    nc = bass.Bass("TRN2", target_bir_lowering=False)
    x_d = nc.dram_tensor("x", [n_seq, L_SEQ, D], F32, kind="ExternalInput").ap()
    win_d = nc.dram_tensor("w_in", [4, D, IN_COLS], F32, kind="ExternalInput").ap()
    ng_d = nc.dram_tensor("norm_g", [4, D], F32, kind="ExternalInput").ap()
    pw_d = nc.dram_tensor("pool_w", [4, 4, 64, 64], F32, kind="ExternalInput").ap()
    psc_d = nc.dram_tensor("pool_scale", [4, 256], F32, kind="ExternalInput").ap()
    fbf_d = nc.dram_tensor("fox_bf", [4, 6], F32, kind="ExternalInput").ap()
    wout_d = nc.dram_tensor("w_out", [4, D, D], F32, kind="ExternalInput").ap()
    fg_d = nc.dram_tensor("final_g", [D], F32, kind="ExternalInput").ap()
    y_d = nc.dram_tensor("y", [n_seq, L_SEQ, D], F32, kind="ExternalOutput").ap()

    S = Sched()
    A = S.add
    es = contextlib.ExitStack()

    def sb(name, shape, dt):
        return es.enter_context(nc.sbuf_tensor(name, shape, dt))

    def pst(name, shape, dt):
        return es.enter_context(nc.psum_tensor(name, shape, dt))

    X = sb("X", [128, NT, D], F32)
    HT = sb("HT", [128, NT * 8 * 128], BF16)
    MIXT = sb("MIXT", [128, 3, L_SEQ], BF16)
    QT = sb("QT", [128, 3, L_SEQ], BF16)
    KT = sb("KT", [128, 3, L_SEQ], BF16)
    VT = sb("VT", [128, NT, 384], BF16)
    WBT = sb("WBT", [128, 2, 8, 392], BF16)
    WB = [WBT[:, 0, :, :], WBT[:, 1, :, :]]
    WOB = sb("WOB", [128, 3, D], BF16)
    WFF = sb("WFF", [128, 8, 128], BF16)
    SCR = sb("SCR", [128, 24576], mybir.dt.uint8)
    PT = [sb(f"PT{i}", [128, 512], BF16) for i in range(4)]
    RB = [sb(f"RB{i}", [128, 512], BF16) for i in range(3)]
    RD = sb("RD", [128, 512], F32)
    TMPF = sb("TMPF", [128, 512], F32)
    ident = sb("ident", [128, 128], BF16)
    tri = sb("tri", [128, 128], BF16)
    Utri = sb("Utri", [128, 128], F32)
    onesf = sb("onesf", [128, 128], F32)
    ones64 = sb("ones64", [128, 64], BF16)
    dmaskf = sb("dmaskf", [128, 128], F32)
    dmaskb = sb("dmaskb", [128, 128], BF16)
    dmaskg = sb("dmaskg", [128, 128], BF16)
    halves = sb("halves", [128, NSTEP + 1], F32)
    invw = sb("invw", [128, 2], F32)
    invcnt = sb("invcnt", [128, 2, 16], F32)
    SS = sb("SS", [128, NT], F32)
    RSTD = sb("RSTD", [128, NT], F32)
    PSCL = sb("PSCL", [128, 2], F32)
    POOLW = sb("POOLW", [128, 2, 128], BF16)
    FBF = sb("FBF", [128, 6], F32)
    IW = sb("IW", [128, NT, 8], F32)
    FF = sb("FF", [128, NT, 6], F32)
    TOT = sb("TOT", [128, NT, 6], F32)
    OFFS = sb("OFFS", [128, NT + 1, 6], F32)
    GG = sb("GG", [128, NT, 6], F32)
    DW = sb("DW", [128, 8, 128], BF16)
    BSS = [sb("BS0", [128, 8 + NSTEP + 1], F32), sb("BS1", [128, 8 + NSTEP + 1], F32)]

    def view(t, dt, byte_off, n_elem):
        ap = t[:, :]
        apb = ap.bitcast(dt)
        esz = mybir.dt.size(dt)
        tsz = mybir.dt.size(t.dtype)
        e0 = byte_off // esz
        return apb[:, e0:e0 + n_elem]

    HB = [view(SCR, BF16, 16384, 1024), view(SCR, BF16, 18432, 1024)]
    GB = view(SCR, F32, 20480, 1024)
    JUNKN = view(SCR, BF16, 8192, 1024)
    VBUF = view(SCR, F32, 0, 528)
    T1 = view(SCR, F32, 2112, 528)
    T2 = view(SCR, F32, 4224, 528)
    PTP = view(SCR, BF16, 8192, 4096)
    BIASALL = view(SCR, F32, 16384, 16 * 16 * 6)
    IQT = view(SCR, BF16, 0, 3 * 2048)
    IKT = view(SCR, BF16, 12288, 2048)
    SCORES = [view(SCR, F32, 16384, 2048), WBT[:, :, :, :].rearrange('p a k c -> p (a k c)').bitcast(F32)[:, 0:2048]]
    YST = [view(HT, F32, 0, 1024), view(HT, F32, 4096, 1024)]

    def ht(i, k):
        o = (i * 8 + k) * 128
        return HT[:, o:o + 128]

    HT4 = HT[:, :].rearrange("p (i k t) -> p i k t", i=NT, k=8)

    PTR = pst("PTR", [128, 1024], BF16)
    PA = [pst("PA0", [128, 512], F32), pst("PA1", [128, 512], F32)]
    PS = [pst("PS0", [128, 512], F32), pst("PS1", [128, 512], F32)]
    PO = [pst("PO0", [128, 512], F32), pst("PO1", [128, 512], F32)]
    PD = [pst("PD0", [128, 512], F32)]
    rot = {"pa": 0, "ps": 0, "po": 0, "pt": 0, "rb": 0, "wb": 0, "hb": 0}

    def nxt(name, n):
        v = rot[name]
        rot[name] = (v + 1) % n
        return v

    A("pool", lambda e: e.memset(TMPF[:, 0:128], 0.0), writes=["TMPF"])
    A("pool", lambda e: e.affine_select(out=TMPF[:, 0:128], in_=TMPF[:, 0:128], pattern=[[-1, 128]],
                                        compare_op=ALU.not_equal, fill=1.0, base=0,
                                        channel_multiplier=1), reads=["TMPF"], writes=["TMPF"])
    A("dve", lambda e: e.tensor_copy(out=ident[:], in_=TMPF[:, 0:128]), reads=["TMPF"], writes=["ident"])
    A("pool", lambda e: e.memset(TMPF[:, 0:128], 0.0), writes=["TMPF"])
    A("pool", lambda e: e.affine_select(out=TMPF[:, 0:128], in_=TMPF[:, 0:128], pattern=[[1, 128]],
                                        compare_op=ALU.is_ge, fill=NEG, base=0,
                                        channel_multiplier=-1), reads=["TMPF"], writes=["TMPF"])
    A("dve", lambda e: e.tensor_copy(out=tri[:], in_=TMPF[:, 0:128]), reads=["TMPF"], writes=["tri"])
    A("pool", lambda e: e.memset(Utri[:], 1.0), writes=["Utri"])
    A("pool", lambda e: e.affine_select(out=Utri[:], in_=Utri[:], pattern=[[1, 128]],
                                        compare_op=ALU.is_ge, fill=0.0, base=0,
                                        channel_multiplier=-1), reads=["Utri"], writes=["Utri"])
    A("pool", lambda e: e.memset(onesf[:], 1.0), writes=["onesf"])
    A("pool", lambda e: e.memset(ones64[:], 1.0), writes=["ones64"])
    A("pool", lambda e: e.memset(dmaskf[:], 0.0), writes=["dmaskf"])
    A("pool", lambda e: e.memset(dmaskf[0:64, 64:128], -1.0e30), reads=["dmaskf"], writes=["dmaskf"])
    A("pool", lambda e: e.memset(dmaskg[:], 0.0), writes=["dmaskg"])
    A("pool", lambda e: e.memset(dmaskg[0:64, 64:128], -1.0e30), reads=["dmaskg"], writes=["dmaskg"])
    A("pool", lambda e: e.memset(dmaskb[:], 0.0), writes=["dmaskb"])
    A("pool", lambda e: e.memset(dmaskb[0:64, 64:128], NEG), reads=["dmaskb"], writes=["dmaskb"])
    for k in range(NSTEP + 1):
        A("pool", lambda e, k=k: e.memset(halves[:, k:k + 1], 0.5 ** (k + 1)), writes=["halves"])
    for j, (wa, wb_) in enumerate(((2, 4), (8, 16))):
        A("pool", lambda e, j=j, wa=wa: e.memset(invw[0:64, j:j + 1], 1.0 / wa), writes=["invw"])
        A("pool", lambda e, j=j, wb_=wb_: e.memset(invw[64:128, j:j + 1], 1.0 / wb_), writes=["invw"])
        for t in range(16):
            A("pool", lambda e, j=j, t=t, wa=wa: e.memset(invcnt[0:64, j, t:t + 1], 1.0 / min(t + 1, wa)),
              writes=["invcnt"])
            A("pool", lambda e, j=j, t=t, wb_=wb_: e.memset(invcnt[64:128, j, t:t + 1], 1.0 / min(t + 1, wb_)),
              writes=["invcnt"])
    A("pool", lambda e: e.memset(OFFS[:, 0, :], 0.0), writes=["OFFS"])

    def load_w(l, pieces):
        b = nxt("wb", 2)
        for (c0, n, s0) in pieces:
            A("pool", lambda e, b=b, c0=c0, n=n, s0=s0: e.dma_start(
                out=WB[b][:, :, s0:s0 + n],
                in_=win_d[l].rearrange("(k p) c -> p k c", p=128)[:, :, c0:c0 + n]),
              writes=[f"WB{b}"], dma=True, sem_key=f"WB{b}")
        return b

    def proj_cm(b, col0, M, evac):
        for c in range(4):
            pa = nxt("pa", 2)
            for k in range(8):
                A("pe", lambda e, pa=pa, k=k, c=c: e.matmul(
                    PA[pa][0:M, :], lhsT=WB[b][:, k, col0:col0 + M],
                    rhs=HT4[:, 4 * c:4 * c + 4, k, :], start=(k == 0), stop=(k == 7)),
                  reads=[f"WB{b}"] + [f"HT{i}" for i in range(4 * c, 4 * c + 4)], writes=[f"PA{pa}"])
            evac(c, PA[pa], f"PA{pa}")

    def proj_tm(b, N, evac):
        for i in range(NT):
            pa = nxt("pa", 2)
            for k in range(8):
                A("pe", lambda e, pa=pa, k=k, i=i: e.matmul(
                    PA[pa][:, 0:N], lhsT=ht(i, k), rhs=WB[b][:, k, 0:N],
                    start=(k == 0), stop=(k == 7)),
                  reads=[f"WB{b}", f"HT{i}"], writes=[f"PA{pa}"])
            evac(i, PA[pa], f"PA{pa}")

    cnt = {"alt": 0}

    def copy_alt(out, in_, reads, writes, scale=None):
        cnt["alt"] += 1
        if cnt["alt"] % 2 == 0:
            if scale is None:
                A("act", lambda e: e.activation(out=out, in_=in_, func=AF.Copy), reads=reads, writes=writes)
            else:
                A("act", lambda e: e.activation(out=out, in_=in_, func=AF.Copy, scale=scale),
                  reads=reads, writes=writes)
        else:
            if scale is None:
                A("dve", lambda e: e.tensor_copy(out=out, in_=in_), reads=reads, writes=writes)
            else:
                A("dve", lambda e: e.tensor_scalar(out=out, in0=in_, scalar1=scale, scalar2=None,
                                                   op0=ALU.mult), reads=reads, writes=writes)

    def out_proj(l, row0, nch):
        A("pool", lambda e: e.dma_start(
            out=WOB[:, 0:nch, :],
            in_=wout_d[l, row0:row0 + nch * 128, :].rearrange("(q p) c -> p q c", p=128)),
          writes=["WOB"], dma=True, sem_key="WOB")
        for i in range(NT):
            for n in range(2):
                pa = nxt("pa", 2)
                for q in range(nch):
                    A("pe", lambda e, pa=pa, q=q, i=i, n=n: e.matmul(
                        PA[pa][:, :], lhsT=MIXT[:, q, i * 128:(i + 1) * 128],
                        rhs=WOB[:, q, n * 512:(n + 1) * 512], start=(q == 0), stop=(q == nch - 1)),
                      reads=["WOB", f"MIXT{q}"], writes=[f"PA{pa}"])
                A("dve", lambda e, pa=pa, i=i, n=n: e.tensor_tensor(
                    out=X[:, i, n * 512:(n + 1) * 512], in0=X[:, i, n * 512:(n + 1) * 512],
                    in1=PA[pa][:, :], op=ALU.add),
                  reads=[f"PA{pa}", f"X{i}"], writes=[f"X{i}"])

    def rms_stats():
        for i in range(NT):
            A("act", lambda e, i=i: e.activation(out=JUNKN, in_=X[:, i, :], func=AF.Square,
                                                 accum_out=SS[:, i:i + 1]),
              reads=[f"X{i}"], writes=["JUNKN", "SS"])
        A("act", lambda e: e.activation(out=RSTD[:], in_=SS[:], func=AF.Sqrt, bias=EPS_T[:, 0:1], scale=1.0 / D),
          reads=["SS"], writes=["RSTD"])
        A("dve", lambda e: e.reciprocal(out=RSTD[:], in_=RSTD[:]), reads=["RSTD"], writes=["RSTD"])

    EPS_T = sb("EPS_T", [128, 1], F32)
    A("pool", lambda e: e.memset(EPS_T[:], 1e-6), writes=["EPS_T"])
    C255 = sb("C255", [128, 1], F32)
    A("pool", lambda e: e.memset(C255[:], TOPK - 0.5), writes=["C255"])
    CNEG = sb("CNEG", [128, 1], F32)
    A("pool", lambda e: e.memset(CNEG[:], NEG), writes=["CNEG"])


    def mb_view(c, q):
        base = (c % 2) * 16384
        o = 0
        for qq in range(q):
            o += (4 * c + qq + 1) * 128
        n = (4 * c + q + 1) * 128
        return view(HT, BF16, base + 2 * o, n)

    PSC = PTR[:, :].bitcast(F32)

    def attention(kind):
        if kind == "dsa":
            for slot in range(3):
                mask_jobs(0, slot)
        for c in range(4):
            attn_chunk(kind, c)

    def attn_chunk(kind, c):
        nj = 4 * c + 4
        t0 = 4 * c * 128
        po_of = {}
        pd = 0
        pre = {0: (0, 1), 1: (2,), 2: (3,)}

        def qk(p, j, hh):
            h = 2 * p + hh
            r0 = 64 * hh
            i_lo = max(4 * c, j)
            off = (i_lo - 4 * c) * 128
            ps = nxt("ps", 2)
            psk = f"PS{ps}"
            pt = nxt("pt", 4)
            kslc = KT[r0:r0 + 64, p, j * 128:(j + 1) * 128]
            if kind == "fox":
                if j >= 4 * c:
                    A("pe", lambda e: e.matmul(PS[ps][:, off:off + 128], lhsT=ident[:], rhs=tri[:],
                                               start=True, stop=False),
                      reads=["ident", "tri"], writes=[psk])
                    A("pe", lambda e: e.matmul(PS[ps][:, off:off + 128], lhsT=kslc,
                                               rhs=QT[r0:r0 + 64, p, t0 + off:t0 + off + 128],
                                               start=False, stop=True),
                      reads=[f"KT{p}", f"QT{p}"], writes=[psk])
                    if off + 128 < 512:
                        A("pe", lambda e: e.matmul(PS[ps][:, off + 128:512], lhsT=kslc,
                                                   rhs=QT[r0:r0 + 64, p, t0 + off + 128:t0 + 512],
                                                   start=True, stop=True),
                          reads=[f"KT{p}", f"QT{p}"], writes=[psk])
                else:
                    A("pe", lambda e: e.matmul(PS[ps][:, 0:512], lhsT=kslc, rhs=QT[r0:r0 + 64, p, t0:t0 + 512],
                                               start=True, stop=True),
                      reads=[f"KT{p}", f"QT{p}"], writes=[psk])
                i = i_lo
                while i < 4 * c + 4:
                    wd = 2 if i % 2 == 0 else 1
                    co = (i - 4 * c) * 128
                    bcol = ((i | 1) * 16 + j) * 6 + h
                    A("act", lambda e, co=co, wd=wd, bcol=bcol: e.activation(
                        out=PT[pt][:, co:co + 128 * wd], in_=PS[ps][:, co:co + 128 * wd], func=AF.Exp,
                        bias=BIASALL[:, bcol:bcol + 1], scale=1.0),
                      reads=[psk, "BIASALL"], writes=[f"PT{pt}"])
                    i += wd
            else:
                A("pe", lambda e: e.matmul(
                    PS[ps][:, off:512], lhsT=kslc, rhs=QT[r0:r0 + 64, p, t0 + off:t0 + 512],
                    start=True, stop=False),
                  reads=[f"KT{p}", f"QT{p}"], writes=[psk])
                for i in range(i_lo, 4 * c + 4):
                    co = (i - 4 * c) * 128
                    q = i - 4 * c
                    mbv = mb_view(c, q)
                    A("pe", lambda e, co=co, mbv=mbv: e.matmul(
                        PS[ps][:, co:co + 128], lhsT=mbv[:, j * 128:(j + 1) * 128], rhs=ident[:],
                        start=False, stop=True),
                      reads=[f"MB{c % 2}_{q}", "ident"], writes=[psk])
                A("act", lambda e: e.activation(out=PT[pt][:, off:512], in_=PS[ps][:, off:512], func=AF.Exp),
                  reads=[psk], writes=[f"PT{pt}"])
            return (p, j, hh, pt, off)

        def pv(blk):
            p, j, hh, pt, off = blk
            h = 2 * p + hh
            r0 = 64 * hh
            if p not in po_of:
                po_of[p] = nxt("po", 2)
            po = po_of[p]
            A("pe", lambda e: e.matmul(
                PO[po][r0:r0 + 64, off:512], lhsT=VT[:, j, h * 64:(h + 1) * 64], rhs=PT[pt][:, off:512],
                start=(j == 0), stop=(j == nj - 1), tile_position=(0, r0)),
              reads=[f"PT{pt}", "VT"], writes=[f"PO{po}"])
            A("pe", lambda e: e.matmul(
                PD[pd][r0:r0 + 64, off:512], lhsT=ones64[:], rhs=PT[pt][:, off:512],
                start=(j == 0), stop=(j == nj - 1), tile_position=(0, r0)),
              reads=[f"PT{pt}", "ones64"], writes=[f"PD{pd}"])
            if j == nj - 1 and hh == 1:
                csl = slice(4 * c * 128, (4 * c + 4) * 128)
                A("act", lambda e: e.activation(out=RD[:], in_=PD[pd][:, :], func=AF.Copy),
                  reads=[f"PD{pd}"], writes=["RD"])
                A("dve", lambda e: e.reciprocal(out=RD[:], in_=RD[:]), reads=["RD"], writes=["RD"])
                A("dve", lambda e: e.tensor_tensor(out=TMPF[:], in0=PO[po][:, :], in1=RD[:], op=ALU.mult),
                  reads=[f"PO{po}", "RD"], writes=["TMPF"])
                A("dve", lambda e: e.tensor_tensor(out=MIXT[:, p, csl], in0=TMPF[:], in1=MIXT[:, p, csl],
                                                   op=ALU.mult),
                  reads=["TMPF", f"MIXT{p}"], writes=[f"MIXT{p}"])

        pend = None
        for p in range(3):
            if kind == "dsa" and c < 3:
                mask_jobs(c + 1, p)
            for j in range(nj):
                for hh in range(2):
                    cur = qk(p, j, hh)
                    if pend is not None:
                        pv(pend)
                    pend = cur
        pv(pend)

    def dsa_idx_tile(c, q):
        i = 4 * c + q
        n = (i + 1) * 128
        mk = f"MB{c % 2}_{q}"
        mbv = mb_view(c, q)
        if i < 2:
            if i == 1:
                A("pool", lambda e: e.memset(mbv[:, 0:128], 0.0), writes=[mk])
            A("pool", lambda e: e.tensor_copy(out=mbv[:, n - 128:n], in_=dmaskb[:]),
              reads=["dmaskb"], writes=[mk])
            return
        SC = SCORES[q % 2]
        sk = f"SCORE{q % 2}"
        for h in range(8):
            A("act", lambda e, h=h: e.activation(out=DW[:, h, :], in_=ident[:], func=AF.Copy,
                                                 scale=IW[:, i, h:h + 1]),
              reads=["ident", "IW"], writes=["DW"])
        nsc = (n + 511) // 512
        for sc in range(nsc):
            w = min(512, n - sc * 512)
            last = (sc == nsc - 1)
            pend = None
            for h in range(8):
                ti, r = h // 3, (h % 3) * 32
                pa = nxt("pa", 2)
                rb = nxt("rb", 3)
                A("pe", lambda e, pa=pa, ti=ti, r=r, w=w, sc=sc: e.matmul(
                    PA[pa][:, 0:w], lhsT=IQT[r:r + 32, ti * 2048 + i * 128:ti * 2048 + (i + 1) * 128],
                    rhs=IKT[r:r + 32, sc * 512:sc * 512 + w], start=True, stop=True),
                  reads=["IQT", "IKT"], writes=[f"PA{pa}"])
                A("act", lambda e, pa=pa, rb=rb, w=w: e.activation(out=RB[rb][:, 0:w], in_=PA[pa][:, 0:w],
                                                                   func=AF.Relu),
                  reads=[f"PA{pa}"], writes=[f"RB{rb}"])
                for (hp, rbp) in ([pend] if pend is not None else []) + ([(h, rb)] if h == 7 else []):
                    A("pe", lambda e, rbp=rbp, hp=hp, w=w, last=last: e.matmul(
                        PSC[:, 0:w], lhsT=DW[:, hp, :], rhs=RB[rbp][:, 0:w], start=(hp == 0),
                        stop=(hp == 7 and not last)),
                      reads=["DW", f"RB{rbp}"], writes=["PTR"])
                pend = (h, rb)
            if last:
                A("pe", lambda e, w=w: e.matmul(PSC[:, w - 128:w], lhsT=ident[:], rhs=dmaskg[:],
                                                start=False, stop=True),
                  reads=["ident", "dmaskg"], writes=["PTR"])
            s0 = sc * 512
            A("act", lambda e, s0=s0, w=w: e.activation(out=SC[:, s0:s0 + w], in_=PSC[:, 0:w], func=AF.Copy),
              reads=["PTR"], writes=[sk])

    def dsa_bis_ops(c, q):
        i = 4 * c + q
        if i < 2:
            return []
        n = (i + 1) * 128
        mk = f"MB{c % 2}_{q}"
        mbv = mb_view(c, q)
        SC = SCORES[q % 2]
        sk = f"SCORE{q % 2}"
        BS = BSS[q % 2]
        u = f"b{q % 2}"
        mx, mn, rg, mid, cn, tp = (BS[:, 0:1], BS[:, 1:2], BS[:, 2:3], BS[:, 3:4], BS[:, 4:5], BS[:, 5:6])
        DL = BS[:, 8:8 + NSTEP + 1]
        ops = []
        O = lambda fn, reads, writes: ops.append((fn, reads, writes))
        O(lambda e: e.tensor_reduce(out=mx, in_=SC[:, 0:n], axis=AX.X, op=ALU.max), [sk], [u + "mx"])
        O(lambda e: e.tensor_reduce(out=mn, in_=SC[:, 0:n - 64], axis=AX.X, op=ALU.min), [sk], [u + "mn"])
        O(lambda e: e.tensor_tensor(out=rg, in0=mx, in1=mn, op=ALU.subtract), [u + "mx", u + "mn"], [u + "rg"])
        O(lambda e: e.tensor_scalar(out=DL, in0=halves[:], scalar1=rg, scalar2=None, op0=ALU.mult),
          [u + "rg", "halves"], [u + "dl"])
        O(lambda e: e.tensor_tensor(out=mid, in0=mn, in1=BS[:, 8:9], op=ALU.add), [u + "mn", u + "dl"], [u + "mid"])
        for k in range(NSTEP):
            O(lambda e: e.tensor_scalar(out=mbv[:, 0:n], in0=SC[:, 0:n], scalar1=mid, scalar2=None,
                                        op0=ALU.is_ge, op1=ALU.add, accum_out=cn),
              [sk, u + "mid"], [mk, u + "cn"])
            O(lambda e, k=k: e.tensor_scalar(out=tp, in0=cn, scalar1=C255[:, 0:1], scalar2=BS[:, 8 + k:9 + k],
                                             op0=ALU.is_ge, op1=ALU.mult),
              [u + "cn", u + "dl"], [u + "tp"])
            O(lambda e, k=k: e.scalar_tensor_tensor(out=mid, in0=tp, scalar=BS[:, 9 + k:10 + k], in1=mid,
                                                    op0=ALU.subtract, op1=ALU.add),
              [u + "tp", u + "dl", u + "mid"], [u + "mid"])
        O(lambda e: e.tensor_tensor(out=tp, in0=mid, in1=BS[:, 8 + NSTEP:9 + NSTEP], op=ALU.subtract),
          [u + "mid", u + "dl"], [u + "tp"])
        O(lambda e: e.tensor_scalar(out=mbv[:, 0:n], in0=SC[:, 0:n], scalar1=tp, scalar2=CNEG[:, 0:1],
                                    op0=ALU.is_lt, op1=ALU.mult),
          [sk, u + "tp"], [mk])
        return ops

    def dsa_bis_pair(c, qa, qb):
        la, lb = dsa_bis_ops(c, qa), dsa_bis_ops(c, qb)
        for t in range(max(len(la), len(lb))):
            for lst in (la, lb):
                if t < len(lst):
                    fn, r, w = lst[t]
                    A("dve", fn, reads=r, writes=w)

    def mask_jobs(c, slot):
        if slot == 0:
            dsa_idx_tile(c, 0); dsa_idx_tile(c, 1)
        elif slot == 1:
            dsa_bis_pair(c, 0, 1); dsa_idx_tile(c, 2); dsa_idx_tile(c, 3)
        else:
            dsa_bis_pair(c, 2, 3)

    for s in range(n_seq):
        for qd in range(4):
            A("sp", lambda e, s=s, qd=qd: e.dma_start(
                out=X[:, 4 * qd:4 * qd + 4, :],
                in_=x_d[s].rearrange("(i p) d -> p i d", p=128)[:, 4 * qd:4 * qd + 4, :]),
              writes=[f"X{i}" for i in range(4 * qd, 4 * qd + 4)], dma=True, sem_key=f"XL{qd}")
        for l in range(n_layers):
            if lvl < 1:
                break
            A("sp", lambda e, l=l: e.dma_start(out=GB, in_=ng_d[l:l + 1, :].to_broadcast([128, D])),
              writes=["GB"], dma=True, sem_key="GB")
            for j in range(2):
                A("sp", lambda e, l=l, j=j: e.dma_start(
                    out=PSCL[:, j:j + 1], in_=psc_d[l, j * 128:(j + 1) * 128].rearrange("(p o) -> p o", o=1)),
                  writes=["PSCL"], dma=True, sem_key=f"PSCL{j}")
            A("sp", lambda e, l=l: e.dma_start(out=FBF[:], in_=fbf_d[l:l + 1, :].to_broadcast([128, 6])),
              writes=["FBF"], dma=True, sem_key="FBF")
            A("pool", lambda e: e.memset(POOLW[:], 0.0), writes=["POOLW"])
            for g in range(4):
                j, r0 = g // 2, (g % 2) * 64
                A("pool", lambda e, l=l, g=g, j=j, r0=r0: e.dma_start(
                    out=POOLW[r0:r0 + 64, j, r0:r0 + 64], in_=pw_d[l, g]),
                  reads=[], writes=["POOLW"], dma=True, sem_key=f"POOLW{g}")
            rms_stats()
            for i in range(NT):
                hb = nxt("hb", 2)
                A("dve", lambda e, i=i, hb=hb: e.scalar_tensor_tensor(
                    out=HB[hb], in0=X[:, i, :], scalar=RSTD[:, i:i + 1], in1=GB, op0=ALU.mult, op1=ALU.mult),
                  reads=[f"X{i}", "RSTD", "GB"], writes=[f"HB{hb}"])
                for k in range(8):
                    A("pe", lambda e, hb=hb, k=k: e.transpose(out=PTR[:, k * 128:(k + 1) * 128],
                                                              in_=HB[hb][:, k * 128:(k + 1) * 128],
                                                              identity=ident[:]),
                      reads=[f"HB{hb}", "ident"], writes=["PTR"])
                copy_alt(HT[:, i * 1024:(i + 1) * 1024], PTR[:, :], ["PTR"], [f"HT{i}"])

            if lvl < 2:
                S.barrier()
                continue
            b = load_w(l, [(C_PG, 256, 0)])
            for j in range(2):
                def ev_g(c, ps_ap, key, j=j):
                    A("act", lambda e: e.activation(out=MIXT[:, j, c * 512:(c + 1) * 512], in_=ps_ap[:, :],
                                                    func=AF.Silu), reads=[key], writes=[f"MIXT{j}"])
                proj_cm(b, j * 128, 128, ev_g)
            b = load_w(l, [(C_PV, 256, 0)])
            for j in range(2):
                srcA = (T1, T2)
                def ev_v(c, ps_ap, key, j=j):
                    if c == 0:
                        A("pool", lambda e: e.memset(VBUF[:, 0:16], 0.0), writes=["VB_h"])
                    else:
                        A("pool", lambda e: e.tensor_copy(out=VBUF[:, 0:16], in_=VBUF[:, 512:528]),
                          reads=["VB_m"], writes=["VB_h"])
                    A("act", lambda e: e.activation(out=VBUF[:, 16:528], in_=ps_ap[:, :], func=AF.Copy),
                      reads=[key, "VB_h"], writes=["VB_m"])
                    rk = ["VB_h", "VB_m"]
                    A("pool", lambda e: e.tensor_tensor(out=T1[:, 1:528], in0=VBUF[:, 1:528], in1=VBUF[:, 0:527],
                                                        op=ALU.add), reads=rk, writes=["T1"])
                    if j == 0:
                        A("pool", lambda e: e.tensor_tensor(out=T2[64:128, 3:528], in0=T1[64:128, 3:528],
                                                            in1=T1[64:128, 1:526], op=ALU.add),
                          reads=["T1"], writes=["T2"])
                        fa, fb = T1, T2
                    else:
                        A("pool", lambda e: e.tensor_tensor(out=T2[:, 3:528], in0=T1[:, 3:528], in1=T1[:, 1:526],
                                                            op=ALU.add), reads=["T1"], writes=["T2"])
                        A("pool", lambda e: e.tensor_tensor(out=T1[:, 7:528], in0=T2[:, 7:528], in1=T2[:, 3:524],
                                                            op=ALU.add), reads=["T2"], writes=["T1"])
                        A("pool", lambda e: e.tensor_tensor(out=T2[64:128, 15:528], in0=T1[64:128, 15:528],
                                                            in1=T1[64:128, 7:520], op=ALU.add),
                          reads=["T1"], writes=["T2"])
                        fa, fb = T1, T2
                    o0 = j * 2048 + c * 512
                    for (rs, src) in ((slice(0, 64), fa), (slice(64, 128), fb)):
                        A("pool", lambda e, rs=rs, src=src: e.tensor_scalar(
                            out=TMPF[rs, 0:512], in0=src[rs, 16:528], scalar1=invw[rs, j:j + 1], scalar2=None,
                            op0=ALU.mult),
                          reads=["T1", "T2", "invw"], writes=["TMPF"])
                        A("pool", lambda e, rs=rs: e.tensor_tensor(
                            out=PTP[rs, o0:o0 + 512], in0=TMPF[rs, 0:512], in1=VBUF[rs, 16:528], op=ALU.subtract),
                          reads=["TMPF", "VB_m"], writes=[f"PTP{j}"])
                        if c == 0:
                            A("pool", lambda e, rs=rs, src=src: e.tensor_tensor(
                                out=TMPF[rs, 0:16], in0=src[rs, 16:32], in1=invcnt[rs, j, :], op=ALU.mult),
                              reads=["T1", "T2", "invcnt"], writes=["TMPF"])
                            A("pool", lambda e, rs=rs: e.tensor_tensor(
                                out=PTP[rs, o0:o0 + 16], in0=TMPF[rs, 0:16], in1=VBUF[rs, 16:32], op=ALU.subtract),
                              reads=["TMPF", "VB_m", f"PTP{j}"], writes=[f"PTP{j}"])
                proj_cm(b, j * 128, 128, ev_v)
            for j in range(2):
                for c in range(4):
                    pa = nxt("pa", 2)
                    A("pe", lambda e, pa=pa, j=j, c=c: e.matmul(
                        PA[pa][:, :], lhsT=POOLW[:, j, :], rhs=PTP[:, j * 2048 + c * 512:j * 2048 + (c + 1) * 512],
                        start=True, stop=True),
                      reads=["POOLW", f"PTP{j}"] + [f"POOLW{g}" for g in range(4)], writes=[f"PA{pa}"])
                    A("dve", lambda e, pa=pa, j=j, c=c: e.scalar_tensor_tensor(
                        out=MIXT[:, j, c * 512:(c + 1) * 512], in0=PA[pa][:, :], scalar=PSCL[:, j:j + 1],
                        in1=MIXT[:, j, c * 512:(c + 1) * 512], op0=ALU.mult, op1=ALU.mult),
                      reads=[f"PA{pa}", "PSCL", f"MIXT{j}"], writes=[f"MIXT{j}"])
            out_proj(l, 0, 2)

            if lvl < 3:
                S.barrier()
                continue
            def qk_proj(c_q, c_k):
                bq = load_w(l, [(c_q, 384, 0)])
                for p in range(3):
                    def ev(c, ps_ap, key, p=p):
                        copy_alt(QT[:, p, c * 512:(c + 1) * 512], ps_ap[:, :], [key], [f"QT{p}"], scale=0.125)
                    proj_cm(bq, p * 128, 128, ev)
                bk = load_w(l, [(c_k, 384, 0)])
                for p in range(3):
                    def ev(c, ps_ap, key, p=p):
                        copy_alt(KT[:, p, c * 512:(c + 1) * 512], ps_ap[:, :], [key], [f"KT{p}"])
                    proj_cm(bk, p * 128, 128, ev)

            def gate_proj(c_g):
                bg = load_w(l, [(c_g, 384, 0)])
                for p in range(3):
                    def ev(c, ps_ap, key, p=p):
                        A("act", lambda e: e.activation(out=MIXT[:, p, c * 512:(c + 1) * 512], in_=ps_ap[:, :],
                                                        func=AF.Silu), reads=[key], writes=[f"MIXT{p}"])
                    proj_cm(bg, p * 128, 128, ev)

            if lvl >= 3.01:
                qk_proj(C_FQ, C_FK)
            if lvl < 3.02:
                S.barrier()
                continue
            gate_proj(C_FG)
            if lvl < 3.03:
                S.barrier()
                continue
            bv = load_w(l, [(C_FV, 384, 0)])
            A("pool", lambda e, l=l: e.dma_start(
                out=WFF[:, :, :],
                in_=win_d[l].rearrange("(k p) c -> p k c", p=128)[:, :, IN_COLS - 128:IN_COLS]),
              writes=["WFF"], dma=True, sem_key="WFF")

            def ev_fv(i, ps_ap, key):
                A("dve", lambda e: e.tensor_copy(out=VT[:, i, :], in_=ps_ap[:, 0:384]), reads=[key], writes=["VT"])
                pa = nxt("pa", 2)
                for k in range(8):
                    A("pe", lambda e, pa=pa, k=k: e.matmul(PA[pa][:, 0:6], lhsT=ht(i, k), rhs=WFF[:, k, 122:128],
                                                           start=(k == 0), stop=(k == 7)),
                      reads=["WFF", f"HT{i}"], writes=[f"PA{pa}"])
                A("dve", lambda e, pa=pa: e.tensor_tensor(out=FF[:, i, :], in0=PA[pa][:, 0:6], in1=FBF[:], op=ALU.add),
                  reads=[f"PA{pa}", "FBF"], writes=["FF"])
            proj_tm(bv, 384, ev_fv)
            if lvl < 3.1:
                S.barrier()
                continue
            FFf = FF[:, :, :].rearrange("p i h -> p (i h)")
            A("act", lambda e: e.activation(out=FFf, in_=FFf, func=AF.Exp, scale=-1.0), reads=["FF"], writes=["FF"])
            A("act", lambda e: e.activation(out=FFf, in_=FFf, func=AF.Ln, bias=1.0, scale=1.0),
              reads=["FF"], writes=["FF"])
            pa = nxt("pa", 2)
            A("pe", lambda e, pa=pa: e.matmul(PA[pa][:, 0:96], lhsT=Utri[:], rhs=FFf, start=True, stop=True),
              reads=["Utri", "FF"], writes=[f"PA{pa}"])
            pa2 = nxt("pa", 2)
            A("pe", lambda e, pa2=pa2: e.matmul(PA[pa2][:, 0:96], lhsT=onesf[:], rhs=FFf, start=True, stop=True),
              reads=["onesf", "FF"], writes=[f"PA{pa2}"])
            A("dve", lambda e, pa2=pa2: e.tensor_copy(out=TOT[:, :, :].rearrange("p i h -> p (i h)"),
                                                      in_=PA[pa2][:, 0:96]), reads=[f"PA{pa2}"], writes=["TOT"])
            for i in range(NT):
                A("dve", lambda e, i=i: e.tensor_tensor(out=OFFS[:, i + 1, :], in0=OFFS[:, i, :], in1=TOT[:, i, :],
                                                        op=ALU.add), reads=["TOT", "OFFS"], writes=["OFFS"])
            A("dve", lambda e, pa=pa: e.tensor_tensor(
                out=GG[:, :, :].rearrange("p i h -> p (i h)"), in0=PA[pa][:, 0:96],
                in1=OFFS[:, 0:NT, :].rearrange("p i h -> p (i h)"), op=ALU.add),
              reads=[f"PA{pa}", "OFFS"], writes=["GG"])
            for i in range(1, NT, 2):
                A("dve", lambda e, i=i: e.tensor_tensor(
                    out=BIASALL[:, i * 96:(i + 1) * 96].rearrange("p (j h) -> p j h", j=16),
                    in0=GG[:, :, :],
                    in1=OFFS[:, i + 1:i + 2, :].to_broadcast([128, 16, 6]), op=ALU.subtract),
                  reads=["GG", "OFFS"], writes=["BIASALL"])
            if lvl < 3.2:
                S.barrier()
                continue
            attention("fox")
            out_proj(l, 640, 3)

            if lvl < 4:
                S.barrier()
                continue
            S.barrier()
            qk_proj(C_DQ, C_DK)
            gate_proj(C_DG)
            bi = load_w(l, [(C_IQ, 256, 0)])
            A("pool", lambda e, l=l: e.dma_start(
                out=WFF[:, :, :],
                in_=win_d[l].rearrange("(k p) c -> p k c", p=128)[:, :, C_IK:C_IK + 128]),
              writes=["WFF"], dma=True, sem_key="WFF")
            for rr in range(3):
                A("dve", lambda e, rr=rr, bi=bi: e.tensor_copy(out=WB[bi][:, :, 256 + 32 * rr:288 + 32 * rr],
                                                               in_=WFF[:, :, 0:32]),
                  reads=["WFF"], writes=[f"WB{bi}"])
            for ti, (c0, m) in enumerate(((0, 96), (96, 96), (192, 64))):
                def ev(c, ps_ap, key, ti=ti, m=m):
                    copy_alt(IQT[0:m, ti * 2048 + c * 512:ti * 2048 + (c + 1) * 512], ps_ap[0:m, :], [key], ["IQT"])
                proj_cm(bi, c0, m, ev)

            def ev_ik(c, ps_ap, key):
                copy_alt(IKT[0:96, c * 512:(c + 1) * 512], ps_ap[0:96, :], [key], ["IKT"])
            proj_cm(bi, 256, 96, ev_ik)
            bv = load_w(l, [(C_DV, 384, 0)])

            def ev_dv(i, ps_ap, key):
                A("dve", lambda e: e.tensor_copy(out=VT[:, i, :], in_=ps_ap[:, 0:384]), reads=[key], writes=["VT"])
                pa = nxt("pa", 2)
                for k in range(8):
                    A("pe", lambda e, pa=pa, k=k: e.matmul(PA[pa][:, 0:8], lhsT=ht(i, k), rhs=WFF[:, k, 32:40],
                                                           start=(k == 0), stop=(k == 7)),
                      reads=["WFF", f"HT{i}"], writes=[f"PA{pa}"])
                A("dve", lambda e, pa=pa: e.tensor_scalar(out=IW[:, i, :], in0=PA[pa][:, 0:8], scalar1=8.0 ** -0.5,
                                                          scalar2=None, op0=ALU.mult), reads=[f"PA{pa}"], writes=["IW"])
            proj_tm(bv, 384, ev_dv)
            S.barrier()
            if lvl < 4.1:
                continue
            attention("dsa")
            out_proj(l, 256, 3)
            S.barrier()

        A("sp", lambda e: e.dma_start(out=GB, in_=fg_d[None, :].to_broadcast([128, D])),
          writes=["GB"], dma=True, sem_key="GB")
        rms_stats()
        for i in range(NT):
            yb = i % 2
            A("dve", lambda e, i=i, yb=yb: e.scalar_tensor_tensor(
                out=YST[yb], in0=X[:, i, :], scalar=RSTD[:, i:i + 1], in1=GB, op0=ALU.mult, op1=ALU.mult),
              reads=[f"X{i}", "RSTD", "GB"], writes=[f"YST{yb}"])
            A("sp", lambda e, s=s, i=i, yb=yb: e.dma_start(out=y_d[s, i * 128:(i + 1) * 128, :], in_=YST[yb]),
              reads=[f"YST{yb}"], writes=[], dma=True, sem_key=f"YST{yb}")
        S.barrier()
    S.barrier()
    S.emit(nc)
    es.close()
    return nc


_NC_CACHE = {}


def kernel(x, w_in, norm_g, pool_w, pool_scale, fox_bf, w_out, final_g):
    n_cores = 8
    x = np.ascontiguousarray(np.asarray(x, dtype=np.float32))
    per = x.shape[0] // n_cores
    if "nc" not in _NC_CACHE:
        _NC_CACHE["nc"] = build(n_seq=per, n_layers=4)
    nc = _NC_CACHE["nc"]
    shared = {
        "w_in": np.ascontiguousarray(np.asarray(w_in, dtype=np.float32)),
        "norm_g": np.ascontiguousarray(np.asarray(norm_g, dtype=np.float32)),
        "pool_w": np.ascontiguousarray(np.asarray(pool_w, dtype=np.float32)),
        "pool_scale": np.ascontiguousarray(np.asarray(pool_scale, dtype=np.float32)),
        "fox_bf": np.ascontiguousarray(np.asarray(fox_bf, dtype=np.float32)),
        "w_out": np.ascontiguousarray(np.asarray(w_out, dtype=np.float32)),
        "final_g": np.ascontiguousarray(np.asarray(final_g, dtype=np.float32)),
    }
    in_maps = []
    for c in range(n_cores):
        m = dict(shared)
        m["x"] = np.ascontiguousarray(x[c * per:(c + 1) * per])
        in_maps.append(m)
    res = run_bass_kernel_spmd(nc, in_maps, core_ids=list(range(n_cores)))
    return np.concatenate([np.asarray(r["y"]) for r in res.results], axis=0).astype(np.float32)
```

```python
import contextlib
import numpy as np
import concourse.bass as bass
import concourse.mybir as mybir
from concourse.bass_utils import run_bass_kernel_spmd

F32 = mybir.dt.float32
BF16 = mybir.dt.bfloat16
ALU = mybir.AluOpType
AF = mybir.ActivationFunctionType
AX = mybir.AxisListType

L_SEQ = 2048
D = 1024
NT = 16
IN_COLS = 3886
NEG = -30000.0
NSTEP = 12
TOPK = 256

C_PV, C_PG, C_DQ, C_DK, C_DV, C_DG = 0, 256, 512, 896, 1280, 1664
C_IQ, C_IK, C_IW, C_FQ, C_FK, C_FV, C_FG, C_FF = 2048, 2304, 2336, 2344, 2728, 3112, 3496, 3880

SEM_SPAN = 3000
SAME_ENG_WINDOW = 5


class Op:
    __slots__ = ("id", "eng", "fn", "deps", "dma", "sem_key", "dcount", "gsig",
                 "eidx", "signals")

    def __init__(self, id, eng, fn, dma, sem_key):
        self.id = id
        self.eng = eng
        self.fn = fn
        self.deps = set()
        self.dma = dma
        self.sem_key = sem_key
        self.dcount = 0
        self.gsig = 0
        self.eidx = 0
        self.signals = False


class Sched:
    ENGS = ("pe", "act", "dve", "pool", "sp")

    def __init__(self):
        self.ops = []
        self.last_writer = {}
        self.readers = {}
        self.eng_count = {e: 0 for e in self.ENGS}
        self.dma_counts = {}
        self.last_dma = {}

    def add(self, eng, fn, reads=(), writes=(), dma=False, sem_key=None, extra_deps=()):
        op = Op(len(self.ops), eng, fn, dma, sem_key)
        if dma:
            assert sem_key is not None
            self.dma_counts[sem_key] = self.dma_counts.get(sem_key, 0) + 1
            op.dcount = self.dma_counts[sem_key]
            self.last_dma[sem_key] = op.id
        op.eidx = self.eng_count[eng]
        self.eng_count[eng] += 1
        for k in list(reads) + list(writes):
            w = self.last_writer.get(k)
            if w is not None:
                op.deps.add(w)
        for k in writes:
            for r in self.readers.get(k, ()):
                op.deps.add(r)
        for k in reads:
            self.readers.setdefault(k, []).append(op.id)
        for k in writes:
            self.last_writer[k] = op.id
            self.readers[k] = []
        for d in extra_deps:
            op.deps.add(d)
        op.deps.discard(op.id)
        self.ops.append(op)
        return op

    def barrier(self):
        last = {}
        for op in self.ops:
            if not op.dma and op.fn is not None:
                last[op.eng] = op.id
        deps = list(last.values()) + list(self.last_dma.values())
        for e in self.ENGS:
            self.add(e, None, extra_deps=deps)

    def emit(self, nc):
        ops = self.ops
        for op in ops:
            for d in op.deps:
                p = ops[d]
                if p.dma or p.fn is None:
                    continue
                if p.eng == op.eng:
                    if op.eng == "pe":
                        continue
                    if op.eidx - p.eidx > SAME_ENG_WINDOW:
                        continue
                p.signals = True
        gs = {e: 0 for e in self.ENGS}
        for op in ops:
            if op.signals and not op.dma:
                gs[op.eng] += 1
                op.gsig = gs[op.eng]
        stack = []
        sems = {}
        for e in self.ENGS:
            for i in range((gs[e] + SEM_SPAN - 1) // SEM_SPAN):
                cm = nc.semaphore(f"s_{e}_{i}")
                sems[(e, i)] = cm.__enter__()
                stack.append(cm)
        dsems = {}
        for k in self.dma_counts:
            cm = nc.semaphore(f"d_{len(dsems)}")
            dsems[k] = cm.__enter__()
            stack.append(cm)
        self.n_sems = len(stack)
        per_eng = {e: [op for op in ops if op.eng == e] for e in self.ENGS}

        def run_engine(e, eng):
            waited = {x: 0 for x in self.ENGS}
            dwaited = {}
            for op in per_eng[e]:
                need = {}
                dneed = {}
                for d in op.deps:
                    p = ops[d]
                    if p.fn is None:
                        continue
                    if p.dma:
                        if dwaited.get(p.sem_key, 0) < p.dcount:
                            dneed[p.sem_key] = max(dneed.get(p.sem_key, 0), p.dcount)
                        continue
                    if p.eng == e:
                        if e == "pe" or op.eidx - p.eidx > SAME_ENG_WINDOW:
                            continue
                    if p.gsig > waited[p.eng]:
                        need[p.eng] = max(need.get(p.eng, 0), p.gsig)
                for pe_, g in need.items():
                    si = (g - 1) // SEM_SPAN
                    eng.wait_ge(sems[(pe_, si)], g - si * SEM_SPAN)
                    waited[pe_] = g
                for k, c in dneed.items():
                    eng.wait_ge(dsems[k], 16 * c)
                    dwaited[k] = c
                if op.fn is None:
                    continue
                ins = op.fn(eng)
                if op.dma:
                    ins.then_inc(dsems[op.sem_key], 16)
                elif op.signals:
                    si = (op.gsig - 1) // SEM_SPAN
                    ins.then_inc(sems[(op.eng, si)], 1)

        with nc.Block() as block:
            @block.tensor
            def _(eng):
                run_engine("pe", eng)

            @block.scalar
            def _(eng):
                run_engine("act", eng)

            @block.vector
            def _(eng):
                run_engine("dve", eng)

            @block.gpsimd
            def _(eng):
                run_engine("pool", eng)

            @block.sync
            def _(eng):
                run_engine("sp", eng)
        for cm in reversed(stack):
            cm.__exit__(None, None, None)


def build(n_seq=2, n_layers=4, lvl=9):
    nc = bass.Bass("TRN2", target_bir_lowering=False)
    x_d = nc.dram_tensor("x", [n_seq, L_SEQ, D], F32, kind="ExternalInput").ap()
    win_d = nc.dram_tensor("w_in", [4, D, IN_COLS], F32, kind="ExternalInput").ap()
    ng_d = nc.dram_tensor("norm_g", [4, D], F32, kind="ExternalInput").ap()
    pw_d = nc.dram_tensor("pool_w", [4, 4, 64, 64], F32, kind="ExternalInput").ap()
    psc_d = nc.dram_tensor("pool_scale", [4, 256], F32, kind="ExternalInput").ap()
    fbf_d = nc.dram_tensor("fox_bf", [4, 6], F32, kind="ExternalInput").ap()
    wout_d = nc.dram_tensor("w_out", [4, D, D], F32, kind="ExternalInput").ap()
    fg_d = nc.dram_tensor("final_g", [D], F32, kind="ExternalInput").ap()
    y_d = nc.dram_tensor("y", [n_seq, L_SEQ, D], F32, kind="ExternalOutput").ap()

    S = Sched()
    A = S.add
    es = contextlib.ExitStack()

    def sb(name, shape, dt):
        return es.enter_context(nc.sbuf_tensor(name, shape, dt))

    def pst(name, shape, dt):
        return es.enter_context(nc.psum_tensor(name, shape, dt))

    X = sb("X", [128, NT, D], F32)
    HT = sb("HT", [128, NT * 8 * 128], BF16)
    MIXT = sb("MIXT", [128, 3, L_SEQ], BF16)
    QT = sb("QT", [128, 3, L_SEQ], BF16)
    KT = sb("KT", [128, 3, L_SEQ], BF16)
    VT = sb("VT", [128, NT, 384], BF16)
    WBT = sb("WBT", [128, 2, 8, 392], BF16)
    WB = [WBT[:, 0, :, :], WBT[:, 1, :, :]]
    WOB = sb("WOB", [128, 3, D], BF16)
    WFF = sb("WFF", [128, 8, 128], BF16)
    SCR = sb("SCR", [128, 24576], mybir.dt.uint8)
    PT = [sb(f"PT{i}", [128, 512], BF16) for i in range(4)]
    RB = [sb(f"RB{i}", [128, 512], BF16) for i in range(3)]
    RD = sb("RD", [128, 512], F32)
    TMPF = sb("TMPF", [128, 512], F32)
    ident = sb("ident", [128, 128], BF16)
    tri = sb("tri", [128, 128], BF16)
    Utri = sb("Utri", [128, 128], F32)
    onesf = sb("onesf", [128, 128], F32)
    ones64 = sb("ones64", [128, 64], BF16)
    dmaskf = sb("dmaskf", [128, 128], F32)
    dmaskb = sb("dmaskb", [128, 128], BF16)
    dmaskg = sb("dmaskg", [128, 128], BF16)
    halves = sb("halves", [128, NSTEP + 1], F32)
    invw = sb("invw", [128, 2], F32)
    invcnt = sb("invcnt", [128, 2, 16], F32)
    SS = sb("SS", [128, NT], F32)
    RSTD = sb("RSTD", [128, NT], F32)
    PSCL = sb("PSCL", [128, 2], F32)
    POOLW = sb("POOLW", [128, 2, 128], BF16)
    FBF = sb("FBF", [128, 6], F32)
    IW = sb("IW", [128, NT, 8], F32)
    FF = sb("FF", [128, NT, 6], F32)
    TOT = sb("TOT", [128, NT, 6], F32)
    OFFS = sb("OFFS", [128, NT + 1, 6], F32)
    GG = sb("GG", [128, NT, 6], F32)
    DW = sb("DW", [128, 8, 128], BF16)
    BSS = [sb("BS0", [128, 8 + NSTEP + 1], F32), sb("BS1", [128, 8 + NSTEP + 1], F32)]

    def view(t, dt, byte_off, n_elem):
        ap = t[:, :]
        apb = ap.bitcast(dt)
        esz = mybir.dt.size(dt)
        tsz = mybir.dt.size(t.dtype)
        e0 = byte_off // esz
        return apb[:, e0:e0 + n_elem]

    HB = [view(SCR, BF16, 16384, 1024), view(SCR, BF16, 18432, 1024)]
    GB = view(SCR, F32, 20480, 1024)
    JUNKN = view(SCR, BF16, 8192, 1024)
    VBUF = view(SCR, F32, 0, 528)
    T1 = view(SCR, F32, 2112, 528)
    T2 = view(SCR, F32, 4224, 528)
    PTP = view(SCR, BF16, 8192, 4096)
    BIASALL = view(SCR, F32, 16384, 16 * 16 * 6)
    IQT = view(SCR, BF16, 0, 3 * 2048)
    IKT = view(SCR, BF16, 12288, 2048)
    SCORES = [view(SCR, F32, 16384, 2048), WBT[:, :, :, :].rearrange('p a k c -> p (a k c)').bitcast(F32)[:, 0:2048]]
    YST = [view(HT, F32, 0, 1024), view(HT, F32, 4096, 1024)]

    def ht(i, k):
        o = (i * 8 + k) * 128
        return HT[:, o:o + 128]

    HT4 = HT[:, :].rearrange("p (i k t) -> p i k t", i=NT, k=8)

    PTR = pst("PTR", [128, 1024], BF16)
    PA = [pst("PA0", [128, 512], F32), pst("PA1", [128, 512], F32)]
    PS = [pst("PS0", [128, 512], F32), pst("PS1", [128, 512], F32)]
    PO = [pst("PO0", [128, 512], F32), pst("PO1", [128, 512], F32)]
    PD = [pst("PD0", [128, 512], F32)]
    rot = {"pa": 0, "ps": 0, "po": 0, "pt": 0, "rb": 0, "wb": 0, "hb": 0}

    def nxt(name, n):
        v = rot[name]
        rot[name] = (v + 1) % n
        return v

    A("pool", lambda e: e.memset(TMPF[:, 0:128], 0.0), writes=["TMPF"])
    A("pool", lambda e: e.affine_select(out=TMPF[:, 0:128], in_=TMPF[:, 0:128], pattern=[[-1, 128]],
                                        compare_op=ALU.not_equal, fill=1.0, base=0,
                                        channel_multiplier=1), reads=["TMPF"], writes=["TMPF"])
    A("dve", lambda e: e.tensor_copy(out=ident[:], in_=TMPF[:, 0:128]), reads=["TMPF"], writes=["ident"])
    A("pool", lambda e: e.memset(TMPF[:, 0:128], 0.0), writes=["TMPF"])
    A("pool", lambda e: e.affine_select(out=TMPF[:, 0:128], in_=TMPF[:, 0:128], pattern=[[1, 128]],
                                        compare_op=ALU.is_ge, fill=NEG, base=0,
                                        channel_multiplier=-1), reads=["TMPF"], writes=["TMPF"])
    A("dve", lambda e: e.tensor_copy(out=tri[:], in_=TMPF[:, 0:128]), reads=["TMPF"], writes=["tri"])
    A("pool", lambda e: e.memset(Utri[:], 1.0), writes=["Utri"])
    A("pool", lambda e: e.affine_select(out=Utri[:], in_=Utri[:], pattern=[[1, 128]],
                                        compare_op=ALU.is_ge, fill=0.0, base=0,
                                        channel_multiplier=-1), reads=["Utri"], writes=["Utri"])
    A("pool", lambda e: e.memset(onesf[:], 1.0), writes=["onesf"])
    A("pool", lambda e: e.memset(ones64[:], 1.0), writes=["ones64"])
    A("pool", lambda e: e.memset(dmaskf[:], 0.0), writes=["dmaskf"])
    A("pool", lambda e: e.memset(dmaskf[0:64, 64:128], -1.0e30), reads=["dmaskf"], writes=["dmaskf"])
    A("pool", lambda e: e.memset(dmaskg[:], 0.0), writes=["dmaskg"])
    A("pool", lambda e: e.memset(dmaskg[0:64, 64:128], -1.0e30), reads=["dmaskg"], writes=["dmaskg"])
    A("pool", lambda e: e.memset(dmaskb[:], 0.0), writes=["dmaskb"])
    A("pool", lambda e: e.memset(dmaskb[0:64, 64:128], NEG), reads=["dmaskb"], writes=["dmaskb"])
    for k in range(NSTEP + 1):
        A("pool", lambda e, k=k: e.memset(halves[:, k:k + 1], 0.5 ** (k + 1)), writes=["halves"])
    for j, (wa, wb_) in enumerate(((2, 4), (8, 16))):
        A("pool", lambda e, j=j, wa=wa: e.memset(invw[0:64, j:j + 1], 1.0 / wa), writes=["invw"])
        A("pool", lambda e, j=j, wb_=wb_: e.memset(invw[64:128, j:j + 1], 1.0 / wb_), writes=["invw"])
        for t in range(16):
            A("pool", lambda e, j=j, t=t, wa=wa: e.memset(invcnt[0:64, j, t:t + 1], 1.0 / min(t + 1, wa)),
              writes=["invcnt"])
            A("pool", lambda e, j=j, t=t, wb_=wb_: e.memset(invcnt[64:128, j, t:t + 1], 1.0 / min(t + 1, wb_)),
              writes=["invcnt"])
    A("pool", lambda e: e.memset(OFFS[:, 0, :], 0.0), writes=["OFFS"])

    def load_w(l, pieces):
        b = nxt("wb", 2)
        for (c0, n, s0) in pieces:
            A("pool", lambda e, b=b, c0=c0, n=n, s0=s0: e.dma_start(
                out=WB[b][:, :, s0:s0 + n],
                in_=win_d[l].rearrange("(k p) c -> p k c", p=128)[:, :, c0:c0 + n]),
              writes=[f"WB{b}"], dma=True, sem_key=f"WB{b}")
        return b

    def proj_cm(b, col0, M, evac):
        for c in range(4):
            pa = nxt("pa", 2)
            for k in range(8):
                A("pe", lambda e, pa=pa, k=k, c=c: e.matmul(
                    PA[pa][0:M, :], lhsT=WB[b][:, k, col0:col0 + M],
                    rhs=HT4[:, 4 * c:4 * c + 4, k, :], start=(k == 0), stop=(k == 7)),
                  reads=[f"WB{b}"] + [f"HT{i}" for i in range(4 * c, 4 * c + 4)], writes=[f"PA{pa}"])
            evac(c, PA[pa], f"PA{pa}")

    def proj_tm(b, N, evac):
        for i in range(NT):
            pa = nxt("pa", 2)
            for k in range(8):
                A("pe", lambda e, pa=pa, k=k, i=i: e.matmul(
                    PA[pa][:, 0:N], lhsT=ht(i, k), rhs=WB[b][:, k, 0:N],
                    start=(k == 0), stop=(k == 7)),
                  reads=[f"WB{b}", f"HT{i}"], writes=[f"PA{pa}"])
            evac(i, PA[pa], f"PA{pa}")

    cnt = {"alt": 0}

    def copy_alt(out, in_, reads, writes, scale=None):
        cnt["alt"] += 1
        if cnt["alt"] % 2 == 0:
            if scale is None:
                A("act", lambda e: e.activation(out=out, in_=in_, func=AF.Copy), reads=reads, writes=writes)
            else:
                A("act", lambda e: e.activation(out=out, in_=in_, func=AF.Copy, scale=scale),
                  reads=reads, writes=writes)
        else:
            if scale is None:
                A("dve", lambda e: e.tensor_copy(out=out, in_=in_), reads=reads, writes=writes)
            else:
                A("dve", lambda e: e.tensor_scalar(out=out, in0=in_, scalar1=scale, scalar2=None,
                                                   op0=ALU.mult), reads=reads, writes=writes)

    def out_proj(l, row0, nch):
        A("pool", lambda e: e.dma_start(
            out=WOB[:, 0:nch, :],
            in_=wout_d[l, row0:row0 + nch * 128, :].rearrange("(q p) c -> p q c", p=128)),
          writes=["WOB"], dma=True, sem_key="WOB")
        for i in range(NT):
            for n in range(2):
                pa = nxt("pa", 2)
                for q in range(nch):
                    A("pe", lambda e, pa=pa, q=q, i=i, n=n: e.matmul(
                        PA[pa][:, :], lhsT=MIXT[:, q, i * 128:(i + 1) * 128],
                        rhs=WOB[:, q, n * 512:(n + 1) * 512], start=(q == 0), stop=(q == nch - 1)),
                      reads=["WOB", f"MIXT{q}"], writes=[f"PA{pa}"])
                A("dve", lambda e, pa=pa, i=i, n=n: e.tensor_tensor(
                    out=X[:, i, n * 512:(n + 1) * 512], in0=X[:, i, n * 512:(n + 1) * 512],
                    in1=PA[pa][:, :], op=ALU.add),
                  reads=[f"PA{pa}", f"X{i}"], writes=[f"X{i}"])

    def rms_stats():
        for i in range(NT):
            A("act", lambda e, i=i: e.activation(out=JUNKN, in_=X[:, i, :], func=AF.Square,
                                                 accum_out=SS[:, i:i + 1]),
              reads=[f"X{i}"], writes=["JUNKN", "SS"])
        A("act", lambda e: e.activation(out=RSTD[:], in_=SS[:], func=AF.Sqrt, bias=EPS_T[:, 0:1], scale=1.0 / D),
          reads=["SS", "EPS_T"], writes=["RSTD"])
        A("dve", lambda e: e.reciprocal(out=RSTD[:], in_=RSTD[:]), reads=["RSTD"], writes=["RSTD"])

    EPS_T = sb("EPS_T", [128, 1], F32)
    A("pool", lambda e: e.memset(EPS_T[:], 1e-6), writes=["EPS_T"])
    C255 = sb("C255", [128, 1], F32)
    A("pool", lambda e: e.memset(C255[:], TOPK - 0.5), writes=["C255"])
    CNEG = sb("CNEG", [128, 1], F32)
    A("pool", lambda e: e.memset(CNEG[:], NEG), writes=["CNEG"])


    def mb_view(c, q):
        base = (c % 2) * 16384
        o = 0
        for qq in range(q):
            o += (4 * c + qq + 1) * 128
        n = (4 * c + q + 1) * 128
        return view(HT, BF16, base + 2 * o, n)

    PSC = PTR[:, :].bitcast(F32)

    def attention(kind):
        if kind == "dsa":
            for slot in range(3):
                mask_jobs(0, slot)
        for c in range(4):
            attn_chunk(kind, c)

    def attn_chunk(kind, c):
        nj = 4 * c + 4
        t0 = 4 * c * 128
        po_of = {}
        pd = 0
        pre = {0: (0, 1), 1: (2,), 2: (3,)}

        def qk(p, j, hh):
            h = 2 * p + hh
            r0 = 64 * hh
            i_lo = max(4 * c, j)
            off = (i_lo - 4 * c) * 128
            ps = nxt("ps", 2)
            psk = f"PS{ps}"
            pt = nxt("pt", 4)
            kslc = KT[r0:r0 + 64, p, j * 128:(j + 1) * 128]
            if kind == "fox":
                if j >= 4 * c:
                    A("pe", lambda e: e.matmul(PS[ps][:, off:off + 128], lhsT=ident[:], rhs=tri[:],
                                               start=True, stop=False),
                      reads=["ident", "tri"], writes=[psk])
                    A("pe", lambda e: e.matmul(PS[ps][:, off:off + 128], lhsT=kslc,
                                               rhs=QT[r0:r0 + 64, p, t0 + off:t0 + off + 128],
                                               start=False, stop=True),
                      reads=[f"KT{p}", f"QT{p}"], writes=[psk])
                    if off + 128 < 512:
                        A("pe", lambda e: e.matmul(PS[ps][:, off + 128:512], lhsT=kslc,
                                                   rhs=QT[r0:r0 + 64, p, t0 + off + 128:t0 + 512],
                                                   start=True, stop=True),
                          reads=[f"KT{p}", f"QT{p}"], writes=[psk])
                else:
                    A("pe", lambda e: e.matmul(PS[ps][:, 0:512], lhsT=kslc, rhs=QT[r0:r0 + 64, p, t0:t0 + 512],
                                               start=True, stop=True),
                      reads=[f"KT{p}", f"QT{p}"], writes=[psk])
                i = i_lo
                while i < 4 * c + 4:
                    wd = 2 if i % 2 == 0 else 1
                    co = (i - 4 * c) * 128
                    bcol = ((i | 1) * 16 + j) * 6 + h
                    A("act", lambda e, co=co, wd=wd, bcol=bcol: e.activation(
                        out=PT[pt][:, co:co + 128 * wd], in_=PS[ps][:, co:co + 128 * wd], func=AF.Exp,
                        bias=BIASALL[:, bcol:bcol + 1], scale=1.0),
                      reads=[psk, "BIASALL"], writes=[f"PT{pt}"])
                    i += wd
            else:
                A("pe", lambda e: e.matmul(
                    PS[ps][:, off:512], lhsT=kslc, rhs=QT[r0:r0 + 64, p, t0 + off:t0 + 512],
                    start=True, stop=False),
                  reads=[f"KT{p}", f"QT{p}"], writes=[psk])
                for i in range(i_lo, 4 * c + 4):
                    co = (i - 4 * c) * 128
                    q = i - 4 * c
                    mbv = mb_view(c, q)
                    A("pe", lambda e, co=co, mbv=mbv: e.matmul(
                        PS[ps][:, co:co + 128], lhsT=mbv[:, j * 128:(j + 1) * 128], rhs=ident[:],
                        start=False, stop=True),
                      reads=[f"MB{c % 2}_{q}", "ident"], writes=[psk])
                A("act", lambda e: e.activation(out=PT[pt][:, off:512], in_=PS[ps][:, off:512], func=AF.Exp),
                  reads=[psk], writes=[f"PT{pt}"])
            return (p, j, hh, pt, off)

        def pv(blk):
            p, j, hh, pt, off = blk
            h = 2 * p + hh
            r0 = 64 * hh
            if p not in po_of:
                po_of[p] = nxt("po", 2)
            po = po_of[p]
            A("pe", lambda e: e.matmul(
                PO[po][r0:r0 + 64, off:512], lhsT=VT[:, j, h * 64:(h + 1) * 64], rhs=PT[pt][:, off:512],
                start=(j == 0), stop=(j == nj - 1), tile_position=(0, r0)),
              reads=[f"PT{pt}", "VT"], writes=[f"PO{po}"])
            A("pe", lambda e: e.matmul(
                PD[pd][r0:r0 + 64, off:512], lhsT=ones64[:], rhs=PT[pt][:, off:512],
                start=(j == 0), stop=(j == nj - 1), tile_position=(0, r0)),
              reads=[f"PT{pt}", "ones64"], writes=[f"PD{pd}"])
            if j == nj - 1 and hh == 1:
                csl = slice(4 * c * 128, (4 * c + 4) * 128)
                A("act", lambda e: e.activation(out=RD[:], in_=PD[pd][:, :], func=AF.Copy),
                  reads=[f"PD{pd}"], writes=["RD"])
                A("dve", lambda e: e.reciprocal(out=RD[:], in_=RD[:]), reads=["RD"], writes=["RD"])
                A("dve", lambda e: e.tensor_tensor(out=TMPF[:], in0=PO[po][:, :], in1=RD[:], op=ALU.mult),
                  reads=[f"PO{po}", "RD"], writes=["TMPF"])
                A("dve", lambda e: e.tensor_tensor(out=MIXT[:, p, csl], in0=TMPF[:], in1=MIXT[:, p, csl],
                                                   op=ALU.mult),
                  reads=["TMPF", f"MIXT{p}"], writes=[f"MIXT{p}"])

        pend = None
        for p in range(3):
            if kind == "dsa" and c < 3:
                mask_jobs(c + 1, p)
            for j in range(nj):
                for hh in range(2):
                    cur = qk(p, j, hh)
                    if pend is not None:
                        pv(pend)
                    pend = cur
        pv(pend)

    def dsa_idx_tile(c, q):
        i = 4 * c + q
        n = (i + 1) * 128
        mk = f"MB{c % 2}_{q}"
        mbv = mb_view(c, q)
        if i < 2:
            if i == 1:
                A("pool", lambda e: e.memset(mbv[:, 0:128], 0.0), writes=[mk])
            A("pool", lambda e: e.tensor_copy(out=mbv[:, n - 128:n], in_=dmaskb[:]),
              reads=["dmaskb"], writes=[mk])
            return
        SC = SCORES[q % 2]
        sk = f"SCORE{q % 2}"
        for h in range(8):
            A("act", lambda e, h=h: e.activation(out=DW[:, h, :], in_=ident[:], func=AF.Copy,
                                                 scale=IW[:, i, h:h + 1]),
              reads=["ident", "IW"], writes=["DW"])
        nsc = (n + 511) // 512
        for sc in range(nsc):
            w = min(512, n - sc * 512)
            last = (sc == nsc - 1)
            pend = None
            for h in range(8):
                ti, r = h // 3, (h % 3) * 32
                pa = nxt("pa", 2)
                rb = nxt("rb", 3)
                A("pe", lambda e, pa=pa, ti=ti, r=r, w=w, sc=sc: e.matmul(
                    PA[pa][:, 0:w], lhsT=IQT[r:r + 32, ti * 2048 + i * 128:ti * 2048 + (i + 1) * 128],
                    rhs=IKT[r:r + 32, sc * 512:sc * 512 + w], start=True, stop=True),
                  reads=["IQT", "IKT"], writes=[f"PA{pa}"])
                A("act", lambda e, pa=pa, rb=rb, w=w: e.activation(out=RB[rb][:, 0:w], in_=PA[pa][:, 0:w],
                                                                   func=AF.Relu),
                  reads=[f"PA{pa}"], writes=[f"RB{rb}"])
                for (hp, rbp) in ([pend] if pend is not None else []) + ([(h, rb)] if h == 7 else []):
                    A("pe", lambda e, rbp=rbp, hp=hp, w=w, last=last: e.matmul(
                        PSC[:, 0:w], lhsT=DW[:, hp, :], rhs=RB[rbp][:, 0:w], start=(hp == 0),
                        stop=(hp == 7 and not last)),
                      reads=["DW", f"RB{rbp}"], writes=["PTR"])
                pend = (h, rb)
            if last:
                A("pe", lambda e, w=w: e.matmul(PSC[:, w - 128:w], lhsT=ident[:], rhs=dmaskg[:],
                                                start=False, stop=True),
                  reads=["ident", "dmaskg"], writes=["PTR"])
            s0 = sc * 512
            A("act", lambda e, s0=s0, w=w: e.activation(out=SC[:, s0:s0 + w], in_=PSC[:, 0:w], func=AF.Copy),
              reads=["PTR"], writes=[sk])

    def dsa_bis_ops(c, q):
        i = 4 * c + q
        if i < 2:
            return []
        n = (i + 1) * 128
        mk = f"MB{c % 2}_{q}"
        mbv = mb_view(c, q)
        SC = SCORES[q % 2]
        sk = f"SCORE{q % 2}"
        BS = BSS[q % 2]
        u = f"b{q % 2}"
        mx, mn, rg, mid, cn, tp = (BS[:, 0:1], BS[:, 1:2], BS[:, 2:3], BS[:, 3:4], BS[:, 4:5], BS[:, 5:6])
        DL = BS[:, 8:8 + NSTEP + 1]
        ops = []
        O = lambda fn, reads, writes: ops.append((fn, reads, writes))
        O(lambda e: e.tensor_reduce(out=mx, in_=SC[:, 0:n], axis=AX.X, op=ALU.max), [sk], [u + "mx"])
        O(lambda e: e.tensor_reduce(out=mn, in_=SC[:, 0:n - 64], axis=AX.X, op=ALU.min), [sk], [u + "mn"])
        O(lambda e: e.tensor_tensor(out=rg, in0=mx, in1=mn, op=ALU.subtract), [u + "mx", u + "mn"], [u + "rg"])
        O(lambda e: e.tensor_scalar(out=DL, in0=halves[:], scalar1=rg, scalar2=None, op0=ALU.mult),
          [u + "rg", "halves"], [u + "dl"])
        O(lambda e: e.tensor_tensor(out=mid, in0=mn, in1=BS[:, 8:9], op=ALU.add), [u + "mn", u + "dl"], [u + "mid"])
        for k in range(NSTEP):
            O(lambda e: e.tensor_scalar(out=mbv[:, 0:n], in0=SC[:, 0:n], scalar1=mid, scalar2=None,
                                        op0=ALU.is_ge, op1=ALU.add, accum_out=cn),
              [sk, u + "mid"], [mk, u + "cn"])
            O(lambda e, k=k: e.tensor_scalar(out=tp, in0=cn, scalar1=C255[:, 0:1], scalar2=BS[:, 8 + k:9 + k],
                                             op0=ALU.is_ge, op1=ALU.mult),
              [u + "cn", u + "dl", "C255"], [u + "tp"])
            O(lambda e, k=k: e.scalar_tensor_tensor(out=mid, in0=tp, scalar=BS[:, 9 + k:10 + k], in1=mid,
                                                    op0=ALU.subtract, op1=ALU.add),
              [u + "tp", u + "dl", u + "mid"], [u + "mid"])
        O(lambda e: e.tensor_tensor(out=tp, in0=mid, in1=BS[:, 8 + NSTEP:9 + NSTEP], op=ALU.subtract),
          [u + "mid", u + "dl"], [u + "tp"])
        O(lambda e: e.tensor_scalar(out=mbv[:, 0:n], in0=SC[:, 0:n], scalar1=tp, scalar2=CNEG[:, 0:1],
                                    op0=ALU.is_lt, op1=ALU.mult),
          [sk, u + "tp", "CNEG"], [mk])
        return ops

    def dsa_bis_pair(c, qa, qb):
        la, lb = dsa_bis_ops(c, qa), dsa_bis_ops(c, qb)
        for t in range(max(len(la), len(lb))):
            for lst in (la, lb):
                if t < len(lst):
                    fn, r, w = lst[t]
                    A("dve", fn, reads=r, writes=w)

    def mask_jobs(c, slot):
        if slot == 0:
            dsa_idx_tile(c, 0); dsa_idx_tile(c, 1)
        elif slot == 1:
            dsa_bis_pair(c, 0, 1); dsa_idx_tile(c, 2); dsa_idx_tile(c, 3)
        else:
            dsa_bis_pair(c, 2, 3)

    for s in range(n_seq):
        for qd in range(4):
            A("sp", lambda e, s=s, qd=qd: e.dma_start(
                out=X[:, 4 * qd:4 * qd + 4, :],
                in_=x_d[s].rearrange("(i p) d -> p i d", p=128)[:, 4 * qd:4 * qd + 4, :]),
              writes=[f"X{i}" for i in range(4 * qd, 4 * qd + 4)], dma=True, sem_key=f"XL{qd}")
        for l in range(n_layers):
            if lvl < 1:
                break
            A("sp", lambda e, l=l: e.dma_start(out=GB, in_=ng_d[l:l + 1, :].to_broadcast([128, D])),
              writes=["GB"], dma=True, sem_key="GB")
            for j in range(2):
                A("sp", lambda e, l=l, j=j: e.dma_start(
                    out=PSCL[:, j:j + 1], in_=psc_d[l, j * 128:(j + 1) * 128].rearrange("(p o) -> p o", o=1)),
                  writes=["PSCL"], dma=True, sem_key=f"PSCL{j}")
            A("sp", lambda e, l=l: e.dma_start(out=FBF[:], in_=fbf_d[l:l + 1, :].to_broadcast([128, 6])),
              writes=["FBF"], dma=True, sem_key="FBF")
            A("pool", lambda e: e.memset(POOLW[:], 0.0), writes=["POOLW"])
            for g in range(4):
                j, r0 = g // 2, (g % 2) * 64
                A("pool", lambda e, l=l, g=g, j=j, r0=r0: e.dma_start(
                    out=POOLW[r0:r0 + 64, j, r0:r0 + 64], in_=pw_d[l, g]),
                  reads=[], writes=["POOLW"], dma=True, sem_key=f"POOLW{g}")
            rms_stats()
            for i in range(NT):
                hb = nxt("hb", 2)
                A("dve", lambda e, i=i, hb=hb: e.scalar_tensor_tensor(
                    out=HB[hb], in0=X[:, i, :], scalar=RSTD[:, i:i + 1], in1=GB, op0=ALU.mult, op1=ALU.mult),
                  reads=[f"X{i}", "RSTD", "GB"], writes=[f"HB{hb}"])
                for k in range(8):
                    A("pe", lambda e, hb=hb, k=k: e.transpose(out=PTR[:, k * 128:(k + 1) * 128],
                                                              in_=HB[hb][:, k * 128:(k + 1) * 128],
                                                              identity=ident[:]),
                      reads=[f"HB{hb}", "ident"], writes=["PTR"])
                copy_alt(HT[:, i * 1024:(i + 1) * 1024], PTR[:, :], ["PTR"], [f"HT{i}"])

            if lvl < 2:
                S.barrier()
                continue
            b = load_w(l, [(C_PG, 256, 0)])
            for j in range(2):
                def ev_g(c, ps_ap, key, j=j):
                    A("act", lambda e: e.activation(out=MIXT[:, j, c * 512:(c + 1) * 512], in_=ps_ap[:, :],
                                                    func=AF.Silu), reads=[key], writes=[f"MIXT{j}"])
                proj_cm(b, j * 128, 128, ev_g)
            b = load_w(l, [(C_PV, 256, 0)])
            for j in range(2):
                srcA = (T1, T2)
                def ev_v(c, ps_ap, key, j=j):
                    if c == 0:
                        A("pool", lambda e: e.memset(VBUF[:, 0:16], 0.0), writes=["VB_h"])
                    else:
                        A("pool", lambda e: e.tensor_copy(out=VBUF[:, 0:16], in_=VBUF[:, 512:528]),
                          reads=["VB_m"], writes=["VB_h"])
                    A("act", lambda e: e.activation(out=VBUF[:, 16:528], in_=ps_ap[:, :], func=AF.Copy),
                      reads=[key, "VB_h"], writes=["VB_m"])
                    rk = ["VB_h", "VB_m"]
                    A("pool", lambda e: e.tensor_tensor(out=T1[:, 1:528], in0=VBUF[:, 1:528], in1=VBUF[:, 0:527],
                                                        op=ALU.add), reads=rk, writes=["T1"])
                    if j == 0:
                        A("pool", lambda e: e.tensor_tensor(out=T2[64:128, 3:528], in0=T1[64:128, 3:528],
                                                            in1=T1[64:128, 1:526], op=ALU.add),
                          reads=["T1"], writes=["T2"])
                        fa, fb = T1, T2
                    else:
                        A("pool", lambda e: e.tensor_tensor(out=T2[:, 3:528], in0=T1[:, 3:528], in1=T1[:, 1:526],
                                                            op=ALU.add), reads=["T1"], writes=["T2"])
                        A("pool", lambda e: e.tensor_tensor(out=T1[:, 7:528], in0=T2[:, 7:528], in1=T2[:, 3:524],
                                                            op=ALU.add), reads=["T2"], writes=["T1"])
                        A("pool", lambda e: e.tensor_tensor(out=T2[64:128, 15:528], in0=T1[64:128, 15:528],
                                                            in1=T1[64:128, 7:520], op=ALU.add),
                          reads=["T1"], writes=["T2"])
                        fa, fb = T1, T2
                    o0 = j * 2048 + c * 512
                    for (rs, src) in ((slice(0, 64), fa), (slice(64, 128), fb)):
                        A("pool", lambda e, rs=rs, src=src: e.tensor_scalar(
                            out=TMPF[rs, 0:512], in0=src[rs, 16:528], scalar1=invw[rs, j:j + 1], scalar2=None,
                            op0=ALU.mult),
                          reads=["T1", "T2", "invw"], writes=["TMPF"])
                        A("pool", lambda e, rs=rs: e.tensor_tensor(
                            out=PTP[rs, o0:o0 + 512], in0=TMPF[rs, 0:512], in1=VBUF[rs, 16:528], op=ALU.subtract),
                          reads=["TMPF", "VB_m"], writes=[f"PTP{j}"])
                        if c == 0:
                            A("pool", lambda e, rs=rs, src=src: e.tensor_tensor(
                                out=TMPF[rs, 0:16], in0=src[rs, 16:32], in1=invcnt[rs, j, :], op=ALU.mult),
                              reads=["T1", "T2", "invcnt"], writes=["TMPF"])
                            A("pool", lambda e, rs=rs: e.tensor_tensor(
                                out=PTP[rs, o0:o0 + 16], in0=TMPF[rs, 0:16], in1=VBUF[rs, 16:32], op=ALU.subtract),
                              reads=["TMPF", "VB_m", f"PTP{j}"], writes=[f"PTP{j}"])
                proj_cm(b, j * 128, 128, ev_v)
            for j in range(2):
                for c in range(4):
                    pa = nxt("pa", 2)
                    A("pe", lambda e, pa=pa, j=j, c=c: e.matmul(
                        PA[pa][:, :], lhsT=POOLW[:, j, :], rhs=PTP[:, j * 2048 + c * 512:j * 2048 + (c + 1) * 512],
                        start=True, stop=True),
                      reads=["POOLW", f"PTP{j}"] + [f"POOLW{g}" for g in range(4)], writes=[f"PA{pa}"])
                    A("dve", lambda e, pa=pa, j=j, c=c: e.scalar_tensor_tensor(
                        out=MIXT[:, j, c * 512:(c + 1) * 512], in0=PA[pa][:, :], scalar=PSCL[:, j:j + 1],
                        in1=MIXT[:, j, c * 512:(c + 1) * 512], op0=ALU.mult, op1=ALU.mult),
                      reads=[f"PA{pa}", "PSCL", f"MIXT{j}"], writes=[f"MIXT{j}"])
            out_proj(l, 0, 2)

            if lvl < 3:
                S.barrier()
                continue
            def qk_proj(c_q, c_k):
                bq = load_w(l, [(c_q, 384, 0)])
                for p in range(3):
                    def ev(c, ps_ap, key, p=p):
                        copy_alt(QT[:, p, c * 512:(c + 1) * 512], ps_ap[:, :], [key], [f"QT{p}"], scale=0.125)
                    proj_cm(bq, p * 128, 128, ev)
                bk = load_w(l, [(c_k, 384, 0)])
                for p in range(3):
                    def ev(c, ps_ap, key, p=p):
                        copy_alt(KT[:, p, c * 512:(c + 1) * 512], ps_ap[:, :], [key], [f"KT{p}"])
                    proj_cm(bk, p * 128, 128, ev)

            def gate_proj(c_g):
                bg = load_w(l, [(c_g, 384, 0)])
                for p in range(3):
                    def ev(c, ps_ap, key, p=p):
                        A("act", lambda e: e.activation(out=MIXT[:, p, c * 512:(c + 1) * 512], in_=ps_ap[:, :],
                                                        func=AF.Silu), reads=[key], writes=[f"MIXT{p}"])
                    proj_cm(bg, p * 128, 128, ev)

            if lvl >= 3.01:
                qk_proj(C_FQ, C_FK)
            if lvl < 3.02:
                S.barrier()
                continue
            gate_proj(C_FG)
            if lvl < 3.03:
                S.barrier()
                continue
            bv = load_w(l, [(C_FV, 384, 0)])
            A("pool", lambda e, l=l: e.dma_start(
                out=WFF[:, :, :],
                in_=win_d[l].rearrange("(k p) c -> p k c", p=128)[:, :, IN_COLS - 128:IN_COLS]),
              writes=["WFF"], dma=True, sem_key="WFF")

            def ev_fv(i, ps_ap, key):
                A("dve", lambda e: e.tensor_copy(out=VT[:, i, :], in_=ps_ap[:, 0:384]), reads=[key], writes=["VT"])
                pa = nxt("pa", 2)
                for k in range(8):
                    A("pe", lambda e, pa=pa, k=k: e.matmul(PA[pa][:, 0:6], lhsT=ht(i, k), rhs=WFF[:, k, 122:128],
                                                           start=(k == 0), stop=(k == 7)),
                      reads=["WFF", f"HT{i}"], writes=[f"PA{pa}"])
                A("dve", lambda e, pa=pa: e.tensor_tensor(out=FF[:, i, :], in0=PA[pa][:, 0:6], in1=FBF[:], op=ALU.add),
                  reads=[f"PA{pa}", "FBF"], writes=["FF"])
            proj_tm(bv, 384, ev_fv)
            if lvl < 3.1:
                S.barrier()
                continue
            FFf = FF[:, :, :].rearrange("p i h -> p (i h)")
            A("act", lambda e: e.activation(out=FFf, in_=FFf, func=AF.Exp, scale=-1.0), reads=["FF"], writes=["FF"])
            A("act", lambda e: e.activation(out=FFf, in_=FFf, func=AF.Ln, bias=1.0, scale=1.0),
              reads=["FF"], writes=["FF"])
            pa = nxt("pa", 2)
            A("pe", lambda e, pa=pa: e.matmul(PA[pa][:, 0:96], lhsT=Utri[:], rhs=FFf, start=True, stop=True),
              reads=["Utri", "FF"], writes=[f"PA{pa}"])
            pa2 = nxt("pa", 2)
            A("pe", lambda e, pa2=pa2: e.matmul(PA[pa2][:, 0:96], lhsT=onesf[:], rhs=FFf, start=True, stop=True),
              reads=["onesf", "FF"], writes=[f"PA{pa2}"])
            A("dve", lambda e, pa2=pa2: e.tensor_copy(out=TOT[:, :, :].rearrange("p i h -> p (i h)"),
                                                      in_=PA[pa2][:, 0:96]), reads=[f"PA{pa2}"], writes=["TOT"])
            for i in range(NT):
                A("dve", lambda e, i=i: e.tensor_tensor(out=OFFS[:, i + 1, :], in0=OFFS[:, i, :], in1=TOT[:, i, :],
                                                        op=ALU.add), reads=["TOT", "OFFS"], writes=["OFFS"])
            A("dve", lambda e, pa=pa: e.tensor_tensor(
                out=GG[:, :, :].rearrange("p i h -> p (i h)"), in0=PA[pa][:, 0:96],
                in1=OFFS[:, 0:NT, :].rearrange("p i h -> p (i h)"), op=ALU.add),
              reads=[f"PA{pa}", "OFFS"], writes=["GG"])
            for i in range(1, NT, 2):
                A("dve", lambda e, i=i: e.tensor_tensor(
                    out=BIASALL[:, i * 96:(i + 1) * 96].rearrange("p (j h) -> p j h", j=16),
                    in0=GG[:, :, :],
                    in1=OFFS[:, i + 1:i + 2, :].to_broadcast([128, 16, 6]), op=ALU.subtract),
                  reads=["GG", "OFFS"], writes=["BIASALL"])
            if lvl < 3.2:
                S.barrier()
                continue
            attention("fox")
            out_proj(l, 640, 3)

            if lvl < 4:
                S.barrier()
                continue
            S.barrier()
            qk_proj(C_DQ, C_DK)
            gate_proj(C_DG)
            bi = load_w(l, [(C_IQ, 256, 0)])
            A("pool", lambda e, l=l: e.dma_start(
                out=WFF[:, :, :],
                in_=win_d[l].rearrange("(k p) c -> p k c", p=128)[:, :, C_IK:C_IK + 128]),
              writes=["WFF"], dma=True, sem_key="WFF")
            for rr in range(3):
                A("dve", lambda e, rr=rr, bi=bi: e.tensor_copy(out=WB[bi][:, :, 256 + 32 * rr:288 + 32 * rr],
                                                               in_=WFF[:, :, 0:32]),
                  reads=["WFF"], writes=[f"WB{bi}"])
            for ti, (c0, m) in enumerate(((0, 96), (96, 96), (192, 64))):
                def ev(c, ps_ap, key, ti=ti, m=m):
                    copy_alt(IQT[0:m, ti * 2048 + c * 512:ti * 2048 + (c + 1) * 512], ps_ap[0:m, :], [key], ["IQT"])
                proj_cm(bi, c0, m, ev)

            def ev_ik(c, ps_ap, key):
                copy_alt(IKT[0:96, c * 512:(c + 1) * 512], ps_ap[0:96, :], [key], ["IKT"])
            proj_cm(bi, 256, 96, ev_ik)
            bv = load_w(l, [(C_DV, 384, 0)])

            def ev_dv(i, ps_ap, key):
                A("dve", lambda e: e.tensor_copy(out=VT[:, i, :], in_=ps_ap[:, 0:384]), reads=[key], writes=["VT"])
                pa = nxt("pa", 2)
                for k in range(8):
                    A("pe", lambda e, pa=pa, k=k: e.matmul(PA[pa][:, 0:8], lhsT=ht(i, k), rhs=WFF[:, k, 32:40],
                                                           start=(k == 0), stop=(k == 7)),
                      reads=["WFF", f"HT{i}"], writes=[f"PA{pa}"])
                A("dve", lambda e, pa=pa: e.tensor_scalar(out=IW[:, i, :], in0=PA[pa][:, 0:8], scalar1=8.0 ** -0.5,
                                                          scalar2=None, op0=ALU.mult), reads=[f"PA{pa}"], writes=["IW"])
            proj_tm(bv, 384, ev_dv)
            S.barrier()
            if lvl < 4.1:
                continue
            attention("dsa")
            out_proj(l, 256, 3)
            S.barrier()

        A("sp", lambda e: e.dma_start(out=GB, in_=fg_d[None, :].to_broadcast([128, D])),
          writes=["GB"], dma=True, sem_key="GB")
        rms_stats()
        for i in range(NT):
            yb = i % 2
            A("dve", lambda e, i=i, yb=yb: e.scalar_tensor_tensor(
                out=YST[yb], in0=X[:, i, :], scalar=RSTD[:, i:i + 1], in1=GB, op0=ALU.mult, op1=ALU.mult),
              reads=[f"X{i}", "RSTD", "GB"], writes=[f"YST{yb}"])
            A("sp", lambda e, s=s, i=i, yb=yb: e.dma_start(out=y_d[s, i * 128:(i + 1) * 128, :], in_=YST[yb]),
              reads=[f"YST{yb}"], writes=[], dma=True, sem_key=f"YST{yb}")
        S.barrier()
    S.barrier()
    S.emit(nc)
    es.close()
    return nc


_NC_CACHE = {}


def kernel(x, w_in, norm_g, pool_w, pool_scale, fox_bf, w_out, final_g):
    n_cores = 8
    x = np.ascontiguousarray(np.asarray(x, dtype=np.float32))
    per = x.shape[0] // n_cores
    if "nc" not in _NC_CACHE:
        _NC_CACHE["nc"] = build(n_seq=per, n_layers=4)
    nc = _NC_CACHE["nc"]
    shared = {
        "w_in": np.ascontiguousarray(np.asarray(w_in, dtype=np.float32)),
        "norm_g": np.ascontiguousarray(np.asarray(norm_g, dtype=np.float32)),
        "pool_w": np.ascontiguousarray(np.asarray(pool_w, dtype=np.float32)),
        "pool_scale": np.ascontiguousarray(np.asarray(pool_scale, dtype=np.float32)),
        "fox_bf": np.ascontiguousarray(np.asarray(fox_bf, dtype=np.float32)),
        "w_out": np.ascontiguousarray(np.asarray(w_out, dtype=np.float32)),
        "final_g": np.ascontiguousarray(np.asarray(final_g, dtype=np.float32)),
    }
    in_maps = []
    for c in range(n_cores):
        m = dict(shared)
        m["x"] = np.ascontiguousarray(x[c * per:(c + 1) * per])
        in_maps.append(m)
    res = run_bass_kernel_spmd(nc, in_maps, core_ids=list(range(n_cores)))
    return np.concatenate([np.asarray(r["y"]) for r in res.results], axis=0).astype(np.float32)
```

```python
import contextlib
import numpy as np
import concourse.bass as bass
import concourse.mybir as mybir
from concourse.bass_utils import run_bass_kernel_spmd

F32 = mybir.dt.float32
BF16 = mybir.dt.bfloat16
ALU = mybir.AluOpType
AF = mybir.ActivationFunctionType
AX = mybir.AxisListType

L_SEQ = 2048
D = 1024
NT = 16
IN_COLS = 3886
NEG = -30000.0
NSTEP = 12
TOPK = 256

C_PV, C_PG, C_DQ, C_DK, C_DV, C_DG = 0, 256, 512, 896, 1280, 1664
C_IQ, C_IK, C_IW, C_FQ, C_FK, C_FV, C_FG, C_FF = 2048, 2304, 2336, 2344, 2728, 3112, 3496, 3880

SEM_SPAN = 3000
SAME_ENG_WINDOW = 5


class Op:
    __slots__ = ("id", "eng", "fn", "deps", "dma", "sem_key", "dcount", "gsig",
                 "eidx", "signals")

    def __init__(self, id, eng, fn, dma, sem_key):
        self.id = id
        self.eng = eng
        self.fn = fn
        self.deps = set()
        self.dma = dma
        self.sem_key = sem_key
        self.dcount = 0
        self.gsig = 0
        self.eidx = 0
        self.signals = False


class Sched:
    ENGS = ("pe", "act", "dve", "pool", "sp")

    def __init__(self):
        self.ops = []
        self.last_writer = {}
        self.readers = {}
        self.eng_count = {e: 0 for e in self.ENGS}
        self.dma_counts = {}
        self.last_dma = {}

    def add(self, eng, fn, reads=(), writes=(), dma=False, sem_key=None, extra_deps=()):
        op = Op(len(self.ops), eng, fn, dma, sem_key)
        if dma:
            assert sem_key is not None
            self.dma_counts[sem_key] = self.dma_counts.get(sem_key, 0) + 1
            op.dcount = self.dma_counts[sem_key]
            self.last_dma[sem_key] = op.id
        op.eidx = self.eng_count[eng]
        self.eng_count[eng] += 1
        for k in list(reads) + list(writes):
            w = self.last_writer.get(k)
            if w is not None:
                op.deps.add(w)
        for k in writes:
            for r in self.readers.get(k, ()):
                op.deps.add(r)
        for k in reads:
            self.readers.setdefault(k, []).append(op.id)
        for k in writes:
            self.last_writer[k] = op.id
            self.readers[k] = []
        for d in extra_deps:
            op.deps.add(d)
        op.deps.discard(op.id)
        self.ops.append(op)
        return op

    def barrier(self):
        last = {}
        for op in self.ops:
            if not op.dma and op.fn is not None:
                last[op.eng] = op.id
        deps = list(last.values()) + list(self.last_dma.values())
        for e in self.ENGS:
            self.add(e, None, extra_deps=deps)

    def emit(self, nc):
        ops = self.ops
        for op in ops:
            for d in op.deps:
                p = ops[d]
                if p.dma or p.fn is None:
                    continue
                if p.eng == op.eng:
                    if op.eng == "pe":
                        continue
                    if op.eidx - p.eidx > SAME_ENG_WINDOW:
                        continue
                p.signals = True
        gs = {e: 0 for e in self.ENGS}
        for op in ops:
            if op.signals and not op.dma:
                gs[op.eng] += 1
                op.gsig = gs[op.eng]
        stack = []
        sems = {}
        for e in self.ENGS:
            for i in range((gs[e] + SEM_SPAN - 1) // SEM_SPAN):
                cm = nc.semaphore(f"s_{e}_{i}")
                sems[(e, i)] = cm.__enter__()
                stack.append(cm)
        dsems = {}
        for k in self.dma_counts:
            cm = nc.semaphore(f"d_{len(dsems)}")
            dsems[k] = cm.__enter__()
            stack.append(cm)
        self.n_sems = len(stack)
        per_eng = {e: [op for op in ops if op.eng == e] for e in self.ENGS}

        def run_engine(e, eng):
            waited = {x: 0 for x in self.ENGS}
            dwaited = {}
            for op in per_eng[e]:
                need = {}
                dneed = {}
                for d in op.deps:
                    p = ops[d]
                    if p.fn is None:
                        continue
                    if p.dma:
                        if dwaited.get(p.sem_key, 0) < p.dcount:
                            dneed[p.sem_key] = max(dneed.get(p.sem_key, 0), p.dcount)
                        continue
                    if p.eng == e:
                        if e == "pe" or op.eidx - p.eidx > SAME_ENG_WINDOW:
                            continue
                    if p.gsig > waited[p.eng]:
                        need[p.eng] = max(need.get(p.eng, 0), p.gsig)
                for pe_, g in need.items():
                    si = (g - 1) // SEM_SPAN
                    eng.wait_ge(sems[(pe_, si)], g - si * SEM_SPAN)
                    waited[pe_] = g
                for k, c in dneed.items():
                    eng.wait_ge(dsems[k], 16 * c)
                    dwaited[k] = c
                if op.fn is None:
                    continue
                ins = op.fn(eng)
                if op.dma:
                    ins.then_inc(dsems[op.sem_key], 16)
                elif op.signals:
                    si = (op.gsig - 1) // SEM_SPAN
                    ins.then_inc(sems[(op.eng, si)], 1)

        with nc.Block() as block:
            @block.tensor
            def _(eng):
                run_engine("pe", eng)

            @block.scalar
            def _(eng):
                run_engine("act", eng)

            @block.vector
            def _(eng):
                run_engine("dve", eng)

            @block.gpsimd
            def _(eng):
                run_engine("pool", eng)

            @block.sync
            def _(eng):
                run_engine("sp", eng)
        for cm in reversed(stack):
            cm.__exit__(None, None, None)


def build(n_seq=2, n_layers=4, lvl=9):
    nc = bass.Bass("TRN2", target_bir_lowering=False)
    x_d = nc.dram_tensor("x", [n_seq, L_SEQ, D], F32, kind="ExternalInput").ap()
    win_d = nc.dram_tensor("w_in", [4, D, IN_COLS], F32, kind="ExternalInput").ap()
    ng_d = nc.dram_tensor("norm_g", [4, D], F32, kind="ExternalInput").ap()
    pw_d = nc.dram_tensor("pool_w", [4, 4, 64, 64], F32, kind="ExternalInput").ap()
    psc_d = nc.dram_tensor("pool_scale", [4, 256], F32, kind="ExternalInput").ap()
    fbf_d = nc.dram_tensor("fox_bf", [4, 6], F32, kind="ExternalInput").ap()
    wout_d = nc.dram_tensor("w_out", [4, D, D], F32, kind="ExternalInput").ap()
    fg_d = nc.dram_tensor("final_g", [D], F32, kind="ExternalInput").ap()
    y_d = nc.dram_tensor("y", [n_seq, L_SEQ, D], F32, kind="ExternalOutput").ap()

    S = Sched()
    A = S.add
    es = contextlib.ExitStack()

    def sb(name, shape, dt):
        return es.enter_context(nc.sbuf_tensor(name, shape, dt))

    def pst(name, shape, dt):
        return es.enter_context(nc.psum_tensor(name, shape, dt))

    X = sb("X", [128, NT, D], F32)
    HT = sb("HT", [128, NT * 8 * 128], BF16)
    MIXT = sb("MIXT", [128, 3, L_SEQ], BF16)
    QT = sb("QT", [128, 3, L_SEQ], BF16)
    KT = sb("KT", [128, 3, L_SEQ], BF16)
    VT = sb("VT", [128, NT, 384], BF16)
    WBT = sb("WBT", [128, 2, 8, 392], BF16)
    WB = [WBT[:, 0, :, :], WBT[:, 1, :, :]]
    WOB = sb("WOB", [128, 3, D], BF16)
    WFF = sb("WFF", [128, 8, 128], BF16)
    SCR = sb("SCR", [128, 24576], mybir.dt.uint8)
    PT = [sb(f"PT{i}", [128, 512], BF16) for i in range(4)]
    RB = [sb(f"RB{i}", [128, 512], BF16) for i in range(3)]
    RD = sb("RD", [128, 512], F32)
    TMPF = sb("TMPF", [128, 512], F32)
    ident = sb("ident", [128, 128], BF16)
    tri = sb("tri", [128, 128], BF16)
    Utri = sb("Utri", [128, 128], F32)
    onesf = sb("onesf", [128, 128], F32)
    ones64 = sb("ones64", [128, 64], BF16)
    dmaskf = sb("dmaskf", [128, 128], F32)
    dmaskb = sb("dmaskb", [128, 128], BF16)
    dmaskg = sb("dmaskg", [128, 128], BF16)
    halves = sb("halves", [128, NSTEP + 1], F32)
    invw = sb("invw", [128, 2], F32)
    invcnt = sb("invcnt", [128, 2, 16], F32)
    SS = sb("SS", [128, NT], F32)
    RSTD = sb("RSTD", [128, NT], F32)
    PSCL = sb("PSCL", [128, 2], F32)
    POOLW = sb("POOLW", [128, 2, 128], BF16)
    FBF = sb("FBF", [128, 6], F32)
    IW = sb("IW", [128, NT, 8], F32)
    FF = sb("FF", [128, NT, 6], F32)
    TOT = sb("TOT", [128, NT, 6], F32)
    OFFS = sb("OFFS", [128, NT + 1, 6], F32)
    GG = sb("GG", [128, NT, 6], F32)
    DW = sb("DW", [128, 8, 128], BF16)
    BSS = [sb("BS0", [128, 8 + NSTEP + 1], F32), sb("BS1", [128, 8 + NSTEP + 1], F32)]

    def view(t, dt, byte_off, n_elem):
        ap = t[:, :]
        apb = ap.bitcast(dt)
        esz = mybir.dt.size(dt)
        tsz = mybir.dt.size(t.dtype)
        e0 = byte_off // esz
        return apb[:, e0:e0 + n_elem]

    HB = [view(SCR, BF16, 16384, 1024), view(SCR, BF16, 18432, 1024)]
    GB = view(SCR, F32, 20480, 1024)
    JUNKN = view(SCR, BF16, 8192, 1024)
    VBUF = view(SCR, F32, 0, 528)
    T1 = view(SCR, F32, 2112, 528)
    T2 = view(SCR, F32, 4224, 528)
    PTP = view(SCR, BF16, 8192, 4096)
    BIASALL = view(SCR, F32, 16384, 16 * 16 * 6)
    IQT = view(SCR, BF16, 0, 3 * 2048)
    IKT = view(SCR, BF16, 12288, 2048)
    SCORES = [view(SCR, F32, 16384, 2048), WBT[:, :, :, :].rearrange('p a k c -> p (a k c)').bitcast(F32)[:, 0:2048]]
    YST = [view(HT, F32, 0, 1024), view(HT, F32, 4096, 1024)]

    def ht(i, k):
        o = (i * 8 + k) * 128
        return HT[:, o:o + 128]

    HT4 = HT[:, :].rearrange("p (i k t) -> p i k t", i=NT, k=8)

    PTR = pst("PTR", [128, 1024], BF16)
    PA = [pst("PA0", [128, 512], F32), pst("PA1", [128, 512], F32)]
    PS = [pst("PS0", [128, 512], F32), pst("PS1", [128, 512], F32)]
    PO = [pst("PO0", [128, 512], F32), pst("PO1", [128, 512], F32)]
    PD = [pst("PD0", [128, 512], F32)]
    rot = {"pa": 0, "ps": 0, "po": 0, "pt": 0, "rb": 0, "wb": 0, "hb": 0}

    def nxt(name, n):
        v = rot[name]
        rot[name] = (v + 1) % n
        return v

    A("pool", lambda e: e.memset(TMPF[:, 0:128], 0.0), writes=["TMPF"])
    A("pool", lambda e: e.affine_select(out=TMPF[:, 0:128], in_=TMPF[:, 0:128], pattern=[[-1, 128]],
                                        compare_op=ALU.not_equal, fill=1.0, base=0,
                                        channel_multiplier=1), reads=["TMPF"], writes=["TMPF"])
    A("dve", lambda e: e.tensor_copy(out=ident[:], in_=TMPF[:, 0:128]), reads=["TMPF"], writes=["ident"])
    A("pool", lambda e: e.memset(TMPF[:, 0:128], 0.0), writes=["TMPF"])
    A("pool", lambda e: e.affine_select(out=TMPF[:, 0:128], in_=TMPF[:, 0:128], pattern=[[1, 128]],
                                        compare_op=ALU.is_ge, fill=NEG, base=0,
                                        channel_multiplier=-1), reads=["TMPF"], writes=["TMPF"])
    A("dve", lambda e: e.tensor_copy(out=tri[:], in_=TMPF[:, 0:128]), reads=["TMPF"], writes=["tri"])
    A("pool", lambda e: e.memset(Utri[:], 1.0), writes=["Utri"])
    A("pool", lambda e: e.affine_select(out=Utri[:], in_=Utri[:], pattern=[[1, 128]],
                                        compare_op=ALU.is_ge, fill=0.0, base=0,
                                        channel_multiplier=-1), reads=["Utri"], writes=["Utri"])
    A("pool", lambda e: e.memset(onesf[:], 1.0), writes=["onesf"])
    A("pool", lambda e: e.memset(ones64[:], 1.0), writes=["ones64"])
    A("pool", lambda e: e.memset(dmaskf[:], 0.0), writes=["dmaskf"])
    A("pool", lambda e: e.memset(dmaskf[0:64, 64:128], -1.0e30), reads=["dmaskf"], writes=["dmaskf"])
    A("pool", lambda e: e.memset(dmaskg[:], 0.0), writes=["dmaskg"])
    A("pool", lambda e: e.memset(dmaskg[0:64, 64:128], -1.0e30), reads=["dmaskg"], writes=["dmaskg"])
    A("pool", lambda e: e.memset(dmaskb[:], 0.0), writes=["dmaskb"])
    A("pool", lambda e: e.memset(dmaskb[0:64, 64:128], NEG), reads=["dmaskb"], writes=["dmaskb"])
    for k in range(NSTEP + 1):
        A("pool", lambda e, k=k: e.memset(halves[:, k:k + 1], 0.5 ** (k + 1)), writes=["halves"])
    for j, (wa, wb_) in enumerate(((2, 4), (8, 16))):
        A("pool", lambda e, j=j, wa=wa: e.memset(invw[0:64, j:j + 1], 1.0 / wa), writes=["invw"])
        A("pool", lambda e, j=j, wb_=wb_: e.memset(invw[64:128, j:j + 1], 1.0 / wb_), writes=["invw"])
        for t in range(16):
            A("pool", lambda e, j=j, t=t, wa=wa: e.memset(invcnt[0:64, j, t:t + 1], 1.0 / min(t + 1, wa)),
              writes=["invcnt"])
            A("pool", lambda e, j=j, t=t, wb_=wb_: e.memset(invcnt[64:128, j, t:t + 1], 1.0 / min(t + 1, wb_)),
              writes=["invcnt"])
    A("pool", lambda e: e.memset(OFFS[:, 0, :], 0.0), writes=["OFFS"])

    def load_w(l, pieces):
        b = nxt("wb", 2)
        for (c0, n, s0) in pieces:
            A("pool", lambda e, b=b, c0=c0, n=n, s0=s0: e.dma_start(
                out=WB[b][:, :, s0:s0 + n],
                in_=win_d[l].rearrange("(k p) c -> p k c", p=128)[:, :, c0:c0 + n]),
              writes=[f"WB{b}"], dma=True, sem_key=f"WB{b}")
        return b

    def proj_cm(b, col0, M, evac):
        for c in range(4):
            pa = nxt("pa", 2)
            for k in range(8):
                A("pe", lambda e, pa=pa, k=k, c=c: e.matmul(
                    PA[pa][0:M, :], lhsT=WB[b][:, k, col0:col0 + M],
                    rhs=HT4[:, 4 * c:4 * c + 4, k, :], start=(k == 0), stop=(k == 7)),
                  reads=[f"WB{b}"] + [f"HT{i}" for i in range(4 * c, 4 * c + 4)], writes=[f"PA{pa}"])
            evac(c, PA[pa], f"PA{pa}")

    def proj_tm(b, N, evac):
        for i in range(NT):
            pa = nxt("pa", 2)
            for k in range(8):
                A("pe", lambda e, pa=pa, k=k, i=i: e.matmul(
                    PA[pa][:, 0:N], lhsT=ht(i, k), rhs=WB[b][:, k, 0:N],
                    start=(k == 0), stop=(k == 7)),
                  reads=[f"WB{b}", f"HT{i}"], writes=[f"PA{pa}"])
            evac(i, PA[pa], f"PA{pa}")

    cnt = {"alt": 0}

    def copy_alt(out, in_, reads, writes, scale=None):
        cnt["alt"] += 1
        if cnt["alt"] % 2 == 0:
            if scale is None:
                A("act", lambda e: e.activation(out=out, in_=in_, func=AF.Copy), reads=reads, writes=writes)
            else:
                A("act", lambda e: e.activation(out=out, in_=in_, func=AF.Copy, scale=scale),
                  reads=reads, writes=writes)
        else:
            if scale is None:
                A("dve", lambda e: e.tensor_copy(out=out, in_=in_), reads=reads, writes=writes)
            else:
                A("dve", lambda e: e.tensor_scalar(out=out, in0=in_, scalar1=scale, scalar2=None,
                                                   op0=ALU.mult), reads=reads, writes=writes)

    def out_proj(l, row0, nch):
        A("pool", lambda e: e.dma_start(
            out=WOB[:, 0:nch, :],
            in_=wout_d[l, row0:row0 + nch * 128, :].rearrange("(q p) c -> p q c", p=128)),
          writes=["WOB"], dma=True, sem_key="WOB")
        for i in range(NT):
            for n in range(2):
                pa = nxt("pa", 2)
                for q in range(nch):
                    A("pe", lambda e, pa=pa, q=q, i=i, n=n: e.matmul(
                        PA[pa][:, :], lhsT=MIXT[:, q, i * 128:(i + 1) * 128],
                        rhs=WOB[:, q, n * 512:(n + 1) * 512], start=(q == 0), stop=(q == nch - 1)),
                      reads=["WOB", f"MIXT{q}"], writes=[f"PA{pa}"])
                A("dve", lambda e, pa=pa, i=i, n=n: e.tensor_tensor(
                    out=X[:, i, n * 512:(n + 1) * 512], in0=X[:, i, n * 512:(n + 1) * 512],
                    in1=PA[pa][:, :], op=ALU.add),
                  reads=[f"PA{pa}", f"X{i}"], writes=[f"X{i}"])

    def rms_stats():
        for i in range(NT):
            A("act", lambda e, i=i: e.activation(out=JUNKN, in_=X[:, i, :], func=AF.Square,
                                                 accum_out=SS[:, i:i + 1]),
              reads=[f"X{i}"], writes=["JUNKN", "SS"])
        A("act", lambda e: e.activation(out=RSTD[:], in_=SS[:], func=AF.Sqrt, bias=EPS_T[:, 0:1], scale=1.0 / D),
          reads=["SS", "EPS_T"], writes=["RSTD"])
        A("dve", lambda e: e.reciprocal(out=RSTD[:], in_=RSTD[:]), reads=["RSTD"], writes=["RSTD"])

    EPS_T = sb("EPS_T", [128, 1], F32)
    A("pool", lambda e: e.memset(EPS_T[:], 1e-6), writes=["EPS_T"])
    C255 = sb("C255", [128, 1], F32)
    A("pool", lambda e: e.memset(C255[:], TOPK - 0.5), writes=["C255"])
    CNEG = sb("CNEG", [128, 1], F32)
    A("pool", lambda e: e.memset(CNEG[:], NEG), writes=["CNEG"])


    def mb_view(c, q):
        base = (c % 2) * 16384
        o = 0
        for qq in range(q):
            o += (4 * c + qq + 1) * 128
        n = (4 * c + q + 1) * 128
        return view(HT, BF16, base + 2 * o, n)

    PSC = PTR[:, :].bitcast(F32)

    def attention(kind):
        if kind == "dsa":
            for slot in range(3):
                mask_jobs(0, slot)
        for c in range(4):
            attn_chunk(kind, c)

    def attn_chunk(kind, c):
        nj = 4 * c + 4
        t0 = 4 * c * 128
        po_of = {}
        pd = 0
        pre = {0: (0, 1), 1: (2,), 2: (3,)}

        def qk(p, j, hh):
            h = 2 * p + hh
            r0 = 64 * hh
            i_lo = max(4 * c, j)
            off = (i_lo - 4 * c) * 128
            ps = nxt("ps", 2)
            psk = f"PS{ps}"
            pt = nxt("pt", 4)
            kslc = KT[r0:r0 + 64, p, j * 128:(j + 1) * 128]
            if kind == "fox":
                if j >= 4 * c:
                    A("pe", lambda e: e.matmul(PS[ps][:, off:off + 128], lhsT=ident[:], rhs=tri[:],
                                               start=True, stop=False),
                      reads=["ident", "tri"], writes=[psk])
                    A("pe", lambda e: e.matmul(PS[ps][:, off:off + 128], lhsT=kslc,
                                               rhs=QT[r0:r0 + 64, p, t0 + off:t0 + off + 128],
                                               start=False, stop=True),
                      reads=[f"KT{p}", f"QT{p}"], writes=[psk])
                    if off + 128 < 512:
                        A("pe", lambda e: e.matmul(PS[ps][:, off + 128:512], lhsT=kslc,
                                                   rhs=QT[r0:r0 + 64, p, t0 + off + 128:t0 + 512],
                                                   start=True, stop=True),
                          reads=[f"KT{p}", f"QT{p}"], writes=[psk])
                else:
                    A("pe", lambda e: e.matmul(PS[ps][:, 0:512], lhsT=kslc, rhs=QT[r0:r0 + 64, p, t0:t0 + 512],
                                               start=True, stop=True),
                      reads=[f"KT{p}", f"QT{p}"], writes=[psk])
                i = i_lo
                while i < 4 * c + 4:
                    wd = 2 if i % 2 == 0 else 1
                    co = (i - 4 * c) * 128
                    bcol = ((i | 1) * 16 + j) * 6 + h
                    A("act", lambda e, co=co, wd=wd, bcol=bcol: e.activation(
                        out=PT[pt][:, co:co + 128 * wd], in_=PS[ps][:, co:co + 128 * wd], func=AF.Exp,
                        bias=BIASALL[:, bcol:bcol + 1], scale=1.0),
                      reads=[psk, "BIASALL"], writes=[f"PT{pt}"])
                    i += wd
            else:
                A("pe", lambda e: e.matmul(
                    PS[ps][:, off:512], lhsT=kslc, rhs=QT[r0:r0 + 64, p, t0 + off:t0 + 512],
                    start=True, stop=False),
                  reads=[f"KT{p}", f"QT{p}"], writes=[psk])
                for i in range(i_lo, 4 * c + 4):
                    co = (i - 4 * c) * 128
                    q = i - 4 * c
                    mbv = mb_view(c, q)
                    A("pe", lambda e, co=co, mbv=mbv: e.matmul(
                        PS[ps][:, co:co + 128], lhsT=mbv[:, j * 128:(j + 1) * 128], rhs=ident[:],
                        start=False, stop=True),
                      reads=[f"MB{c % 2}_{q}", "ident"], writes=[psk])
                A("act", lambda e: e.activation(out=PT[pt][:, off:512], in_=PS[ps][:, off:512], func=AF.Exp),
                  reads=[psk], writes=[f"PT{pt}"])
            return (p, j, hh, pt, off)

        def pv(blk):
            p, j, hh, pt, off = blk
            h = 2 * p + hh
            r0 = 64 * hh
            if p not in po_of:
                po_of[p] = nxt("po", 2)
            po = po_of[p]
            A("pe", lambda e: e.matmul(
                PO[po][r0:r0 + 64, off:512], lhsT=VT[:, j, h * 64:(h + 1) * 64], rhs=PT[pt][:, off:512],
                start=(j == 0), stop=(j == nj - 1), tile_position=(0, r0)),
              reads=[f"PT{pt}", "VT"], writes=[f"PO{po}"])
            A("pe", lambda e: e.matmul(
                PD[pd][r0:r0 + 64, off:512], lhsT=ones64[:], rhs=PT[pt][:, off:512],
                start=(j == 0), stop=(j == nj - 1), tile_position=(0, r0)),
              reads=[f"PT{pt}", "ones64"], writes=[f"PD{pd}"])
            if j == nj - 1 and hh == 1:
                csl = slice(4 * c * 128, (4 * c + 4) * 128)
                A("act", lambda e: e.activation(out=RD[:], in_=PD[pd][:, :], func=AF.Copy),
                  reads=[f"PD{pd}"], writes=["RD"])
                A("dve", lambda e: e.reciprocal(out=RD[:], in_=RD[:]), reads=["RD"], writes=["RD"])
                A("dve", lambda e: e.tensor_tensor(out=TMPF[:], in0=PO[po][:, :], in1=RD[:], op=ALU.mult),
                  reads=[f"PO{po}", "RD"], writes=["TMPF"])
                A("dve", lambda e: e.tensor_tensor(out=MIXT[:, p, csl], in0=TMPF[:], in1=MIXT[:, p, csl],
                                                   op=ALU.mult),
                  reads=["TMPF", f"MIXT{p}"], writes=[f"MIXT{p}"])

        pend = None
        for p in range(3):
            if kind == "dsa" and c < 3:
                mask_jobs(c + 1, p)
            for j in range(nj):
                for hh in range(2):
                    cur = qk(p, j, hh)
                    if pend is not None:
                        pv(pend)
                    pend = cur
        pv(pend)

    def dsa_idx_tile(c, q):
        i = 4 * c + q
        n = (i + 1) * 128
        mk = f"MB{c % 2}_{q}"
        mbv = mb_view(c, q)
        if i < 2:
            if i == 1:
                A("pool", lambda e: e.memset(mbv[:, 0:128], 0.0), writes=[mk])
            A("pool", lambda e: e.tensor_copy(out=mbv[:, n - 128:n], in_=dmaskb[:]),
              reads=["dmaskb"], writes=[mk])
            return
        SC = SCORES[q % 2]
        sk = f"SCORE{q % 2}"
        for h in range(8):
            A("act", lambda e, h=h: e.activation(out=DW[:, h, :], in_=ident[:], func=AF.Copy,
                                                 scale=IW[:, i, h:h + 1]),
              reads=["ident", "IW"], writes=["DW"])
        nsc = (n + 511) // 512
        for sc in range(nsc):
            w = min(512, n - sc * 512)
            last = (sc == nsc - 1)
            pend = None
            for h in range(8):
                ti, r = h // 3, (h % 3) * 32
                pa = nxt("pa", 2)
                rb = nxt("rb", 3)
                A("pe", lambda e, pa=pa, ti=ti, r=r, w=w, sc=sc: e.matmul(
                    PA[pa][:, 0:w], lhsT=IQT[r:r + 32, ti * 2048 + i * 128:ti * 2048 + (i + 1) * 128],
                    rhs=IKT[r:r + 32, sc * 512:sc * 512 + w], start=True, stop=True),
                  reads=["IQT", "IKT"], writes=[f"PA{pa}"])
                A("act", lambda e, pa=pa, rb=rb, w=w: e.activation(out=RB[rb][:, 0:w], in_=PA[pa][:, 0:w],
                                                                   func=AF.Relu),
                  reads=[f"PA{pa}"], writes=[f"RB{rb}"])
                for (hp, rbp) in ([pend] if pend is not None else []) + ([(h, rb)] if h == 7 else []):
                    A("pe", lambda e, rbp=rbp, hp=hp, w=w, last=last: e.matmul(
                        PSC[:, 0:w], lhsT=DW[:, hp, :], rhs=RB[rbp][:, 0:w], start=(hp == 0),
                        stop=(hp == 7 and not last)),
                      reads=["DW", f"RB{rbp}"], writes=["PTR"])
                pend = (h, rb)
            if last:
                A("pe", lambda e, w=w: e.matmul(PSC[:, w - 128:w], lhsT=ident[:], rhs=dmaskg[:],
                                                start=False, stop=True),
                  reads=["ident", "dmaskg"], writes=["PTR"])
            s0 = sc * 512
            A("act", lambda e, s0=s0, w=w: e.activation(out=SC[:, s0:s0 + w], in_=PSC[:, 0:w], func=AF.Copy),
              reads=["PTR"], writes=[sk])

    def dsa_bis_ops(c, q):
        i = 4 * c + q
        if i < 2:
            return []
        n = (i + 1) * 128
        mk = f"MB{c % 2}_{q}"
        mbv = mb_view(c, q)
        SC = SCORES[q % 2]
        sk = f"SCORE{q % 2}"
        BS = BSS[q % 2]
        u = f"b{q % 2}"
        mx, mn, rg, mid, cn, tp = (BS[:, 0:1], BS[:, 1:2], BS[:, 2:3], BS[:, 3:4], BS[:, 4:5], BS[:, 5:6])
        DL = BS[:, 8:8 + NSTEP + 1]
        ops = []
        O = lambda fn, reads, writes: ops.append((fn, reads, writes))
        O(lambda e: e.tensor_reduce(out=mx, in_=SC[:, 0:n], axis=AX.X, op=ALU.max), [sk], [u + "mx"])
        O(lambda e: e.tensor_reduce(out=mn, in_=SC[:, 0:n - 64], axis=AX.X, op=ALU.min), [sk], [u + "mn"])
        O(lambda e: e.tensor_tensor(out=rg, in0=mx, in1=mn, op=ALU.subtract), [u + "mx", u + "mn"], [u + "rg"])
        O(lambda e: e.tensor_scalar(out=DL, in0=halves[:], scalar1=rg, scalar2=None, op0=ALU.mult),
          [u + "rg", "halves"], [u + "dl"])
        O(lambda e: e.tensor_tensor(out=mid, in0=mn, in1=BS[:, 8:9], op=ALU.add), [u + "mn", u + "dl"], [u + "mid"])
        for k in range(NSTEP):
            O(lambda e: e.tensor_scalar(out=mbv[:, 0:n], in0=SC[:, 0:n], scalar1=mid, scalar2=None,
                                        op0=ALU.is_ge, op1=ALU.add, accum_out=cn),
              [sk, u + "mid"], [mk, u + "cn"])
            O(lambda e, k=k: e.tensor_scalar(out=tp, in0=cn, scalar1=C255[:, 0:1], scalar2=BS[:, 8 + k:9 + k],
                                             op0=ALU.is_ge, op1=ALU.mult),
              [u + "cn", u + "dl", "C255"], [u + "tp"])
            O(lambda e, k=k: e.scalar_tensor_tensor(out=mid, in0=tp, scalar=BS[:, 9 + k:10 + k], in1=mid,
                                                    op0=ALU.subtract, op1=ALU.add),
              [u + "tp", u + "dl", u + "mid"], [u + "mid"])
        O(lambda e: e.tensor_tensor(out=tp, in0=mid, in1=BS[:, 8 + NSTEP:9 + NSTEP], op=ALU.subtract),
          [u + "mid", u + "dl"], [u + "tp"])
        O(lambda e: e.tensor_scalar(out=mbv[:, 0:n], in0=SC[:, 0:n], scalar1=tp, scalar2=CNEG[:, 0:1],
                                    op0=ALU.is_lt, op1=ALU.mult),
          [sk, u + "tp", "CNEG"], [mk])
        return ops

    def dsa_bis_pair(c, qa, qb):
        la, lb = dsa_bis_ops(c, qa), dsa_bis_ops(c, qb)
        for t in range(max(len(la), len(lb))):
            for lst in (la, lb):
                if t < len(lst):
                    fn, r, w = lst[t]
                    A("dve", fn, reads=r, writes=w)

    def mask_jobs(c, slot):
        if slot == 0:
            dsa_idx_tile(c, 0); dsa_idx_tile(c, 1)
        elif slot == 1:
            dsa_bis_pair(c, 0, 1); dsa_idx_tile(c, 2); dsa_idx_tile(c, 3)
        else:
            dsa_bis_pair(c, 2, 3)

    for s in range(n_seq):
        for qd in range(4):
            A("sp", lambda e, s=s, qd=qd: e.dma_start(
                out=X[:, 4 * qd:4 * qd + 4, :],
                in_=x_d[s].rearrange("(i p) d -> p i d", p=128)[:, 4 * qd:4 * qd + 4, :]),
              writes=[f"X{i}" for i in range(4 * qd, 4 * qd + 4)], dma=True, sem_key=f"XL{qd}")
        for l in range(n_layers):
            if lvl < 1:
                break
            A("sp", lambda e, l=l: e.dma_start(out=GB, in_=ng_d[l:l + 1, :].to_broadcast([128, D])),
              writes=["GB"], dma=True, sem_key="GB")
            for j in range(2):
                A("sp", lambda e, l=l, j=j: e.dma_start(
                    out=PSCL[:, j:j + 1], in_=psc_d[l, j * 128:(j + 1) * 128].rearrange("(p o) -> p o", o=1)),
                  writes=["PSCL"], dma=True, sem_key=f"PSCL{j}")
            A("sp", lambda e, l=l: e.dma_start(out=FBF[:], in_=fbf_d[l:l + 1, :].to_broadcast([128, 6])),
              writes=["FBF"], dma=True, sem_key="FBF")
            A("pool", lambda e: e.memset(POOLW[:], 0.0), writes=["POOLW"])
            for g in range(4):
                j, r0 = g // 2, (g % 2) * 64
                A("pool", lambda e, l=l, g=g, j=j, r0=r0: e.dma_start(
                    out=POOLW[r0:r0 + 64, j, r0:r0 + 64], in_=pw_d[l, g]),
                  reads=[], writes=["POOLW"], dma=True, sem_key=f"POOLW{g}")
            rms_stats()
            for i in range(NT):
                hb = nxt("hb", 2)
                A("dve", lambda e, i=i, hb=hb: e.scalar_tensor_tensor(
                    out=HB[hb], in0=X[:, i, :], scalar=RSTD[:, i:i + 1], in1=GB, op0=ALU.mult, op1=ALU.mult),
                  reads=[f"X{i}", "RSTD", "GB"], writes=[f"HB{hb}"])
                for k in range(8):
                    A("pe", lambda e, hb=hb, k=k: e.transpose(out=PTR[:, k * 128:(k + 1) * 128],
                                                              in_=HB[hb][:, k * 128:(k + 1) * 128],
                                                              identity=ident[:]),
                      reads=[f"HB{hb}", "ident"], writes=["PTR"])
                copy_alt(HT[:, i * 1024:(i + 1) * 1024], PTR[:, :], ["PTR"], [f"HT{i}"])

            if lvl < 2:
                S.barrier()
                continue
            b = load_w(l, [(C_PG, 256, 0)])
            for j in range(2):
                def ev_g(c, ps_ap, key, j=j):
                    A("act", lambda e: e.activation(out=MIXT[:, j, c * 512:(c + 1) * 512], in_=ps_ap[:, :],
                                                    func=AF.Silu), reads=[key], writes=[f"MIXT{j}"])
                proj_cm(b, j * 128, 128, ev_g)
            b = load_w(l, [(C_PV, 256, 0)])
            for j in range(2):
                srcA = (T1, T2)
                def ev_v(c, ps_ap, key, j=j):
                    if c == 0:
                        A("dve", lambda e: e.memset(VBUF[:, 0:16], 0.0), writes=["VB_h"])
                    else:
                        A("dve", lambda e: e.tensor_copy(out=VBUF[:, 0:16], in_=VBUF[:, 512:528]),
                          reads=["VB_m"], writes=["VB_h"])
                    A("act", lambda e: e.activation(out=VBUF[:, 16:528], in_=ps_ap[:, :], func=AF.Copy),
                      reads=[key, "VB_h"], writes=["VB_m"])
                    rk = ["VB_h", "VB_m"]
                    A("dve", lambda e: e.tensor_tensor(out=T1[:, 1:528], in0=VBUF[:, 1:528], in1=VBUF[:, 0:527],
                                                        op=ALU.add), reads=rk, writes=["T1"])
                    if j == 0:
                        A("dve", lambda e: e.tensor_tensor(out=T2[64:128, 3:528], in0=T1[64:128, 3:528],
                                                            in1=T1[64:128, 1:526], op=ALU.add),
                          reads=["T1"], writes=["T2"])
                        fa, fb = T1, T2
                    else:
                        A("dve", lambda e: e.tensor_tensor(out=T2[:, 3:528], in0=T1[:, 3:528], in1=T1[:, 1:526],
                                                            op=ALU.add), reads=["T1"], writes=["T2"])
                        A("dve", lambda e: e.tensor_tensor(out=T1[:, 7:528], in0=T2[:, 7:528], in1=T2[:, 3:524],
                                                            op=ALU.add), reads=["T2"], writes=["T1"])
                        A("dve", lambda e: e.tensor_tensor(out=T2[64:128, 15:528], in0=T1[64:128, 15:528],
                                                            in1=T1[64:128, 7:520], op=ALU.add),
                          reads=["T1"], writes=["T2"])
                        fa, fb = T1, T2
                    o0 = j * 2048 + c * 512
                    for (rs, src) in ((slice(0, 64), fa), (slice(64, 128), fb)):
                        A("dve", lambda e, rs=rs, src=src: e.tensor_scalar(
                            out=TMPF[rs, 0:512], in0=src[rs, 16:528], scalar1=invw[rs, j:j + 1], scalar2=None,
                            op0=ALU.mult),
                          reads=["T1", "T2", "invw"], writes=["TMPF"])
                        A("dve", lambda e, rs=rs: e.tensor_tensor(
                            out=PTP[rs, o0:o0 + 512], in0=TMPF[rs, 0:512], in1=VBUF[rs, 16:528], op=ALU.subtract),
                          reads=["TMPF", "VB_m"], writes=[f"PTP{j}"])
                        if c == 0:
                            A("dve", lambda e, rs=rs, src=src: e.tensor_tensor(
                                out=TMPF[rs, 0:16], in0=src[rs, 16:32], in1=invcnt[rs, j, :], op=ALU.mult),
                              reads=["T1", "T2", "invcnt"], writes=["TMPF"])
                            A("dve", lambda e, rs=rs: e.tensor_tensor(
                                out=PTP[rs, o0:o0 + 16], in0=TMPF[rs, 0:16], in1=VBUF[rs, 16:32], op=ALU.subtract),
                              reads=["TMPF", "VB_m", f"PTP{j}"], writes=[f"PTP{j}"])
                proj_cm(b, j * 128, 128, ev_v)
            for j in range(2):
                for c in range(4):
                    pa = nxt("pa", 2)
                    A("pe", lambda e, pa=pa, j=j, c=c: e.matmul(
                        PA[pa][:, :], lhsT=POOLW[:, j, :], rhs=PTP[:, j * 2048 + c * 512:j * 2048 + (c + 1) * 512],
                        start=True, stop=True),
                      reads=["POOLW", f"PTP{j}"] + [f"POOLW{g}" for g in range(4)], writes=[f"PA{pa}"])
                    A("dve", lambda e, pa=pa, j=j, c=c: e.scalar_tensor_tensor(
                        out=MIXT[:, j, c * 512:(c + 1) * 512], in0=PA[pa][:, :], scalar=PSCL[:, j:j + 1],
                        in1=MIXT[:, j, c * 512:(c + 1) * 512], op0=ALU.mult, op1=ALU.mult),
                      reads=[f"PA{pa}", "PSCL", f"MIXT{j}"], writes=[f"MIXT{j}"])
            out_proj(l, 0, 2)

            if lvl < 3:
                S.barrier()
                continue
            def qk_proj(c_q, c_k, bq=None):
                if bq is None:
                    bq = load_w(l, [(c_q, 384, 0)])
                for p in range(3):
                    def ev(c, ps_ap, key, p=p):
                        copy_alt(QT[:, p, c * 512:(c + 1) * 512], ps_ap[:, :], [key], [f"QT{p}"], scale=0.125)
                    proj_cm(bq, p * 128, 128, ev)
                bk = load_w(l, [(c_k, 384, 0)])
                for p in range(3):
                    def ev(c, ps_ap, key, p=p):
                        copy_alt(KT[:, p, c * 512:(c + 1) * 512], ps_ap[:, :], [key], [f"KT{p}"])
                    proj_cm(bk, p * 128, 128, ev)

            def gate_proj(c_g):
                bg = load_w(l, [(c_g, 384, 0)])
                for p in range(3):
                    def ev(c, ps_ap, key, p=p):
                        A("act", lambda e: e.activation(out=MIXT[:, p, c * 512:(c + 1) * 512], in_=ps_ap[:, :],
                                                        func=AF.Silu), reads=[key], writes=[f"MIXT{p}"])
                    proj_cm(bg, p * 128, 128, ev)

            if lvl >= 3.01:
                qk_proj(C_FQ, C_FK)
            if lvl < 3.02:
                S.barrier()
                continue
            gate_proj(C_FG)
            if lvl < 3.03:
                S.barrier()
                continue
            bv = load_w(l, [(C_FV, 384, 0)])
            A("pool", lambda e, l=l: e.dma_start(
                out=WFF[:, :, :],
                in_=win_d[l].rearrange("(k p) c -> p k c", p=128)[:, :, IN_COLS - 128:IN_COLS]),
              writes=["WFF"], dma=True, sem_key="WFF")

            def ev_fv(i, ps_ap, key):
                A("dve", lambda e: e.tensor_copy(out=VT[:, i, :], in_=ps_ap[:, 0:384]), reads=[key], writes=["VT"])
                pa = nxt("pa", 2)
                for k in range(8):
                    A("pe", lambda e, pa=pa, k=k: e.matmul(PA[pa][:, 0:6], lhsT=ht(i, k), rhs=WFF[:, k, 122:128],
                                                           start=(k == 0), stop=(k == 7)),
                      reads=["WFF", f"HT{i}"], writes=[f"PA{pa}"])
                A("dve", lambda e, pa=pa: e.tensor_tensor(out=FF[:, i, :], in0=PA[pa][:, 0:6], in1=FBF[:], op=ALU.add),
                  reads=[f"PA{pa}", "FBF"], writes=["FF"])
            proj_tm(bv, 384, ev_fv)
            if lvl < 3.1:
                S.barrier()
                continue
            FFf = FF[:, :, :].rearrange("p i h -> p (i h)")
            A("act", lambda e: e.activation(out=FFf, in_=FFf, func=AF.Exp, scale=-1.0), reads=["FF"], writes=["FF"])
            A("act", lambda e: e.activation(out=FFf, in_=FFf, func=AF.Ln, bias=1.0, scale=1.0),
              reads=["FF"], writes=["FF"])
            pa = nxt("pa", 2)
            A("pe", lambda e, pa=pa: e.matmul(PA[pa][:, 0:96], lhsT=Utri[:], rhs=FFf, start=True, stop=True),
              reads=["Utri", "FF"], writes=[f"PA{pa}"])
            pa2 = nxt("pa", 2)
            A("pe", lambda e, pa2=pa2: e.matmul(PA[pa2][:, 0:96], lhsT=onesf[:], rhs=FFf, start=True, stop=True),
              reads=["onesf", "FF"], writes=[f"PA{pa2}"])
            A("dve", lambda e, pa2=pa2: e.tensor_copy(out=TOT[:, :, :].rearrange("p i h -> p (i h)"),
                                                      in_=PA[pa2][:, 0:96]), reads=[f"PA{pa2}"], writes=["TOT"])
            for i in range(NT):
                A("dve", lambda e, i=i: e.tensor_tensor(out=OFFS[:, i + 1, :], in0=OFFS[:, i, :], in1=TOT[:, i, :],
                                                        op=ALU.add), reads=["TOT", "OFFS"], writes=["OFFS"])
            A("dve", lambda e, pa=pa: e.tensor_tensor(
                out=GG[:, :, :].rearrange("p i h -> p (i h)"), in0=PA[pa][:, 0:96],
                in1=OFFS[:, 0:NT, :].rearrange("p i h -> p (i h)"), op=ALU.add),
              reads=[f"PA{pa}", "OFFS"], writes=["GG"])
            for i in range(1, NT, 2):
                A("dve", lambda e, i=i: e.tensor_tensor(
                    out=BIASALL[:, i * 96:(i + 1) * 96].rearrange("p (j h) -> p j h", j=16),
                    in0=GG[:, :, :],
                    in1=OFFS[:, i + 1:i + 2, :].to_broadcast([128, 16, 6]), op=ALU.subtract),
                  reads=["GG", "OFFS"], writes=["BIASALL"])
            if lvl < 3.2:
                S.barrier()
                continue
            attention("fox")
            out_proj(l, 640, 3)

            if lvl < 4:
                S.barrier()
                continue
            bq_pre = load_w(l, [(C_DQ, 384, 0)])
            S.barrier()
            qk_proj(C_DQ, C_DK, bq_pre)
            gate_proj(C_DG)
            bi = load_w(l, [(C_IQ, 256, 0)])
            A("pool", lambda e, l=l: e.dma_start(
                out=WFF[:, :, :],
                in_=win_d[l].rearrange("(k p) c -> p k c", p=128)[:, :, C_IK:C_IK + 128]),
              writes=["WFF"], dma=True, sem_key="WFF")
            for rr in range(3):
                A("dve", lambda e, rr=rr, bi=bi: e.tensor_copy(out=WB[bi][:, :, 256 + 32 * rr:288 + 32 * rr],
                                                               in_=WFF[:, :, 0:32]),
                  reads=["WFF"], writes=[f"WB{bi}"])
            for ti, (c0, m) in enumerate(((0, 96), (96, 96), (192, 64))):
                def ev(c, ps_ap, key, ti=ti, m=m):
                    copy_alt(IQT[0:m, ti * 2048 + c * 512:ti * 2048 + (c + 1) * 512], ps_ap[0:m, :], [key], ["IQT"])
                proj_cm(bi, c0, m, ev)

            def ev_ik(c, ps_ap, key):
                copy_alt(IKT[0:96, c * 512:(c + 1) * 512], ps_ap[0:96, :], [key], ["IKT"])
            proj_cm(bi, 256, 96, ev_ik)
            bv = load_w(l, [(C_DV, 384, 0)])

            def ev_dv(i, ps_ap, key):
                A("dve", lambda e: e.tensor_copy(out=VT[:, i, :], in_=ps_ap[:, 0:384]), reads=[key], writes=["VT"])
                pa = nxt("pa", 2)
                for k in range(8):
                    A("pe", lambda e, pa=pa, k=k: e.matmul(PA[pa][:, 0:8], lhsT=ht(i, k), rhs=WFF[:, k, 32:40],
                                                           start=(k == 0), stop=(k == 7)),
                      reads=["WFF", f"HT{i}"], writes=[f"PA{pa}"])
                A("dve", lambda e, pa=pa: e.tensor_scalar(out=IW[:, i, :], in0=PA[pa][:, 0:8], scalar1=8.0 ** -0.5,
                                                          scalar2=None, op0=ALU.mult), reads=[f"PA{pa}"], writes=["IW"])
            proj_tm(bv, 384, ev_dv)
            S.barrier()
            if lvl < 4.1:
                continue
            attention("dsa")
            out_proj(l, 256, 3)
            S.barrier()

        A("sp", lambda e: e.dma_start(out=GB, in_=fg_d[None, :].to_broadcast([128, D])),
          writes=["GB"], dma=True, sem_key="GB")
        rms_stats()
        for i in range(NT):
            yb = i % 2
            A("dve", lambda e, i=i, yb=yb: e.scalar_tensor_tensor(
                out=YST[yb], in0=X[:, i, :], scalar=RSTD[:, i:i + 1], in1=GB, op0=ALU.mult, op1=ALU.mult),
              reads=[f"X{i}", "RSTD", "GB"], writes=[f"YST{yb}"])
            A("sp", lambda e, s=s, i=i, yb=yb: e.dma_start(out=y_d[s, i * 128:(i + 1) * 128, :], in_=YST[yb]),
              reads=[f"YST{yb}"], writes=[], dma=True, sem_key=f"YST{yb}")
        S.barrier()
    S.barrier()
    S.emit(nc)
    es.close()
    return nc


_NC_CACHE = {}


def kernel(x, w_in, norm_g, pool_w, pool_scale, fox_bf, w_out, final_g):
    n_cores = 8
    x = np.ascontiguousarray(np.asarray(x, dtype=np.float32))
    per = x.shape[0] // n_cores
    if "nc" not in _NC_CACHE:
        _NC_CACHE["nc"] = build(n_seq=per, n_layers=4)
    nc = _NC_CACHE["nc"]
    shared = {
        "w_in": np.ascontiguousarray(np.asarray(w_in, dtype=np.float32)),
        "norm_g": np.ascontiguousarray(np.asarray(norm_g, dtype=np.float32)),
        "pool_w": np.ascontiguousarray(np.asarray(pool_w, dtype=np.float32)),
        "pool_scale": np.ascontiguousarray(np.asarray(pool_scale, dtype=np.float32)),
        "fox_bf": np.ascontiguousarray(np.asarray(fox_bf, dtype=np.float32)),
        "w_out": np.ascontiguousarray(np.asarray(w_out, dtype=np.float32)),
        "final_g": np.ascontiguousarray(np.asarray(final_g, dtype=np.float32)),
    }
    in_maps = []
    for c in range(n_cores):
        m = dict(shared)
        m["x"] = np.ascontiguousarray(x[c * per:(c + 1) * per])
        in_maps.append(m)
    res = run_bass_kernel_spmd(nc, in_maps, core_ids=list(range(n_cores)))
    return np.concatenate([np.asarray(r["y"]) for r in res.results], axis=0).astype(np.float32)
```
